# Optimizing a Trainium2 kernel written in Bass

```python
import math
import jax, jax.numpy as jnp
from jax import lax
import numpy as np

D_MODEL = 2048
BATCH = 4
SEQ = 4096
DEPTH = 4

GRID_W = 64
CTX_LEN = 256
EPS = 1e-6
CONV_W = 4
ROPE_BASE = 10000.0
N_BRANCH = 3
LRU_WIDTH = 1024
LRU_BLOCKS = 16
LRU_BLOCK = LRU_WIDTH // LRU_BLOCKS
LRU_C = 8.0
SSD_INNER = 1024
SSD_HEADDIM = 64
SSD_HEADS = SSD_INNER // SSD_HEADDIM
SSD_GROUPS = 4
SSD_STATE = 128
SSD_CHUNK = 128
SSD_CONV_DIM = SSD_INNER + 2 * SSD_GROUPS * SSD_STATE
NA_HEADS = 16
NA_HEADDIM = 64
NA_WIDTH = NA_HEADS * NA_HEADDIM
NA_WIN_R = 8
NA_WIN_C = 16
N_EXPERTS = 16
EXPERT_FF = 1024
CAPACITY_FACTOR = 2
PROJ_SIZES = (LRU_WIDTH, LRU_WIDTH, SSD_INNER, SSD_CONV_DIM, 2 * SSD_HEADS, NA_WIDTH, NA_WIDTH, NA_WIDTH, N_BRANCH * D_MODEL)
PROJ_WIDTH = sum(PROJ_SIZES)

kernel_name = "hybrid_lru_ssd_natten_ecmoe_dit"


def rmsnorm(x, g):
    xf = x.astype(jnp.float32)
    y = xf * lax.rsqrt(jnp.mean(xf * xf, axis=-1, keepdims=True) + EPS)
    return (y * g.astype(jnp.float32)).astype(x.dtype)


def split_proj(p):
    idx = tuple(int(i) for i in np.cumsum(PROJ_SIZES)[:-1])
    return jnp.split(p, idx, axis=-1)


def dwconv_centred(x, w, b):
    L = x.shape[1]
    lo = CONV_W // 2
    xp = jnp.pad(x, ((0, 0), (lo, CONV_W - 1 - lo), (0, 0)))
    out = b + w[0] * xp[:, 0:L]
    for k in range(1, CONV_W):
        out = out + w[k] * xp[:, k:k + L]
    return out


def flip_dir(t, d):
    return jnp.flip(t, axis=1) if d else t


def axial_rope(L, dim, dtype):
    pos = jnp.arange(L)
    row = (pos // GRID_W).astype(jnp.float32)
    col = (pos % GRID_W).astype(jnp.float32)
    n_freq = dim // 4
    freqs = ROPE_BASE ** (-jnp.arange(n_freq, dtype=jnp.float32) / n_freq)
    ang = jnp.concatenate([row[:, None] * freqs, col[:, None] * freqs], axis=-1)
    return jnp.cos(ang).astype(dtype), jnp.sin(ang).astype(dtype)


def apply_rope(x, cos, sin):
    x1, x2 = jnp.split(x, 2, axis=-1)
    c, s = cos[:, None, :], sin[:, None, :]
    return jnp.concatenate([x1 * c - x2 * s, x1 * s + x2 * c], axis=-1)


def linear_scan(a, b, h0):
    def combine(l, r):
        al, bl = l
        ar, br = r
        return al * ar, ar * bl + br
    a_cum, h = lax.associative_scan(combine, (a, b), axis=1)
    return h + a_cum * h0[:, None]


def rglru_inputs(x, w_r, b_r, w_i, b_i, lam):
    xb = x.reshape(*x.shape[:-1], LRU_BLOCKS, LRU_BLOCK)
    r = jax.nn.sigmoid(jnp.einsum('blnk,nkj->blnj', xb, w_r).reshape(x.shape) + b_r)
    i = jax.nn.sigmoid(jnp.einsum('blnk,nkj->blnj', xb, w_i).reshape(x.shape) + b_i)
    log_a = -LRU_C * r * jax.nn.softplus(-lam)
    a = jnp.exp(log_a)
    return a, jnp.sqrt(-jnp.expm1(2.0 * log_a)) * (i * x)


def rglru_branch(ax_c, ag_c, ax_l, ag_l, conv_w, conv_b, w_r, b_r, w_i, b_i, lam, ctx_out):
    xc = dwconv_centred(ax_c, conv_w, conv_b)
    xl = dwconv_centred(ax_l, conv_w, conv_b)
    y_c, y_l = None, None
    for d in range(2):
        a_c, u_c = rglru_inputs(flip_dir(xc, d), w_r[d], b_r[d], w_i[d], b_i[d], lam[d])
        h_c = linear_scan(a_c, u_c, jnp.zeros_like(u_c[:, 0]))
        a_l, u_l = rglru_inputs(flip_dir(xl, d), w_r[d], b_r[d], w_i[d], b_i[d], lam[d])
        h_l = flip_dir(linear_scan(a_l, u_l, h_c[:, -1]), d)
        y_l = h_l if y_l is None else y_l + h_l
        if ctx_out:
            h_c = flip_dir(h_c, d)
            y_c = h_c if y_c is None else y_c + h_c
    y_l = y_l * jax.nn.gelu(ag_l)
    if ctx_out:
        y_c = y_c * jax.nn.gelu(ag_c)
    return y_c, y_l


def ssd_chunked(xs, dt, a, bm, cm, h0, need_y):
    bsz, L, H, P = xs.shape
    G, N = bm.shape[2], bm.shape[3]
    R = H // G
    Q = SSD_CHUNK
    nc = L // Q
    f32 = jnp.float32
    x = xs.reshape(bsz, nc, Q, G, R, P).astype(f32)
    dtc = dt.reshape(bsz, nc, Q, G, R)
    bc = bm.reshape(bsz, nc, Q, G, N).astype(f32)
    cc = cm.reshape(bsz, nc, Q, G, N).astype(f32)
    xdt = x * dtc[..., None]
    at = jnp.moveaxis(jnp.cumsum(dtc * a.reshape(G, R), axis=2), 2, -1)
    decay_end = jnp.exp(at[..., -1:] - at)
    states = jnp.einsum('bcjgn,bcgrj,bcjgrp->bcgrpn', bc, decay_end, xdt)
    chunk_decay = jnp.exp(at[..., -1])

    def step(h, inp):
        dec, st = inp
        return dec[..., None, None] * h + st, h

    h_final, h_prev = lax.scan(step, h0.reshape(bsz, G, R, P, N),
                               (jnp.moveaxis(chunk_decay, 1, 0), jnp.moveaxis(states, 1, 0)))
    h_final = h_final.reshape(bsz, H, P, N)
    if not need_y:
        return None, h_final
    h_prev = jnp.moveaxis(h_prev, 0, 1)
    lower = np.tril(np.ones((Q, Q), dtype=bool))
    seg = jnp.where(lower, at[..., :, None] - at[..., None, :], -jnp.inf)
    cb = jnp.einsum('bcign,bcjgn->bcgij', cc, bc)
    m = cb[:, :, :, None] * jnp.exp(seg)
    y_diag = jnp.einsum('bcgrij,bcjgrp->bcigrp', m, xdt)
    y_off = jnp.einsum('bcign,bcgri,bcgrpn->bcigrp', cc, jnp.exp(at), h_prev)
    y = (y_diag + y_off).reshape(bsz, L, H, P).astype(xs.dtype)
    return y, h_final


def ssd_branch(z_c, xbc_c, dt_c, z_l, xbc_l, dt_l, cos, sin, conv_w, conv_b, a_log, dt_bias, d_skip, norm_g, ctx_out):
    gn = SSD_GROUPS * SSD_STATE

    def prep(xbc, dt_raw, rotate):
        xbc = jax.nn.silu(dwconv_centred(xbc, conv_w, conv_b))
        xs, bm, cm = jnp.split(xbc, [SSD_INNER, SSD_INNER + gn], axis=-1)
        bsz, L = xs.shape[:2]
        bm = bm.reshape(bsz, L, SSD_GROUPS, SSD_STATE)
        cm = cm.reshape(bsz, L, SSD_GROUPS, SSD_STATE)
        if rotate:
            bm = apply_rope(bm, cos, sin)
            cm = apply_rope(cm, cos, sin)
        xs = xs.reshape(bsz, L, SSD_HEADS, SSD_HEADDIM)
        dt = jax.nn.softplus(dt_raw.reshape(bsz, L, 2, SSD_HEADS).astype(jnp.float32) + dt_bias.astype(jnp.float32))
        return xs, bm, cm, dt

    xs_c, b_c, c_c, dtc = prep(xbc_c, dt_c, False)
    xs_l, b_l, c_l, dtl = prep(xbc_l, dt_l, True)
    A = -jnp.exp(a_log.astype(jnp.float32))
    bsz = xs_l.shape[0]
    y_c, y_l = None, None
    for d in range(2):
        h0 = jnp.zeros((bsz, SSD_HEADS, SSD_HEADDIM, SSD_STATE), jnp.float32)
        yc_d, h_ctx = ssd_chunked(flip_dir(xs_c, d), flip_dir(dtc[:, :, d], d), A[d],
                                  flip_dir(b_c, d), flip_dir(c_c, d), h0, ctx_out)
        yl_d, _ = ssd_chunked(flip_dir(xs_l, d), flip_dir(dtl[:, :, d], d), A[d],
                              flip_dir(b_l, d), flip_dir(c_l, d), h_ctx, True)
        yl_d = flip_dir(yl_d, d)
        y_l = yl_d if y_l is None else y_l + yl_d
        if ctx_out:
            yc_d = flip_dir(yc_d, d)
            y_c = yc_d if y_c is None else y_c + yc_d

    def finish(y, xs, z):
        y = y + d_skip[:, None] * xs
        y = y.reshape(*y.shape[:2], SSD_INNER)
        return rmsnorm(y * jax.nn.silu(z), norm_g)

    y_l = finish(y_l, xs_l, z_l)
    if ctx_out:
        y_c = finish(y_c, xs_c, z_c)
    return y_c, y_l


def na_branch(q_c, k_c, v_c, q_l, k_l, v_l, rpb, ctx_out):
    bsz, L, _ = q_l.shape
    n_ctx = k_c.shape[1]
    rows = L // GRID_W
    wr = min(NA_WIN_R, rows)
    scale = NA_HEADDIM ** -0.5
    kc = k_c.reshape(bsz, n_ctx, NA_HEADS, NA_HEADDIM)
    vc = v_c.reshape(bsz, n_ctx, NA_HEADS, NA_HEADDIM)
    qg = q_l.reshape(bsz, rows, GRID_W, NA_HEADS, NA_HEADDIM)
    kg = k_l.reshape(bsz, rows, GRID_W, NA_HEADS, NA_HEADDIM)
    vg = v_l.reshape(bsz, rows, GRID_W, NA_HEADS, NA_HEADDIM)
    cols = np.arange(GRID_W)
    col_start = np.clip(cols - NA_WIN_C // 2, 0, GRID_W - NA_WIN_C)
    col_idx = col_start[:, None] + np.arange(NA_WIN_C)[None, :]
    col_off = col_idx - cols[:, None] + (NA_WIN_C - 1)
    rpb_cols = rpb[:, :, col_off]
    n_win = wr * NA_WIN_C

    def row_step(r):
        rs = jnp.clip(r - NA_WIN_R // 2, 0, rows - wr)
        q_row = lax.dynamic_index_in_dim(qg, r, axis=1, keepdims=False)
        k_band = lax.dynamic_slice_in_dim(kg, rs, wr, axis=1)
        v_band = lax.dynamic_slice_in_dim(vg, rs, wr, axis=1)
        k_win = k_band[:, :, col_idx]
        v_win = v_band[:, :, col_idx]
        roff = rs + jnp.arange(wr) - r + (NA_WIN_R - 1)
        bias = jnp.take(rpb_cols, roff, axis=1).transpose(0, 2, 1, 3)
        s_win = jnp.einsum('bjhd,brjwhd->bhjrw', q_row, k_win).astype(jnp.float32) * scale + bias
        s_ctx = jnp.einsum('bjhd,bkhd->bhjk', q_row, kc).astype(jnp.float32) * scale
        s = jnp.concatenate([s_win.reshape(bsz, NA_HEADS, GRID_W, n_win), s_ctx], axis=-1)
        p = jax.nn.softmax(s, axis=-1).astype(v_l.dtype)
        p_win = p[..., :n_win].reshape(bsz, NA_HEADS, GRID_W, wr, NA_WIN_C)
        return (jnp.einsum('bhjrw,brjwhd->bjhd', p_win, v_win)
                + jnp.einsum('bhjk,bkhd->bjhd', p[..., n_win:], vc))

    o = lax.map(row_step, jnp.arange(rows))
    y_l = jnp.moveaxis(o, 0, 1).reshape(bsz, L, NA_WIDTH)
    y_c = None
    if ctx_out:
        qc = q_c.reshape(bsz, n_ctx, NA_HEADS, NA_HEADDIM)
        s = jnp.einsum('bqhd,bkhd->bhqk', qc, kc).astype(jnp.float32) * scale
        p = jax.nn.softmax(s, axis=-1).astype(v_c.dtype)
        y_c = jnp.einsum('bhqk,bkhd->bqhd', p, vc).reshape(bsz, n_ctx, NA_WIDTH)
    return y_c, y_l


def hybrid_mixer(u_c, u_l, cos, sin, w_in, lru_conv_w, lru_conv_b, lru_w_r, lru_b_r, lru_w_i, lru_b_i, lru_lambda,
                 ssd_conv_w, ssd_conv_b, ssd_a_log, ssd_dt_bias, ssd_d, ssd_norm, na_rpb,
                 w_branch_lru, w_branch_ssd, w_branch_na, w_out, ctx_out):
    ax_c, ag_c, z_c, xbc_c, dt_c, q_c, k_c, v_c, g_c = split_proj(u_c @ w_in)
    ax_l, ag_l, z_l, xbc_l, dt_l, q_l, k_l, v_l, g_l = split_proj(u_l @ w_in)
    ya_c, ya_l = rglru_branch(ax_c, ag_c, ax_l, ag_l, lru_conv_w, lru_conv_b, lru_w_r, lru_b_r,
                              lru_w_i, lru_b_i, lru_lambda, ctx_out)
    yb_c, yb_l = ssd_branch(z_c, xbc_c, dt_c, z_l, xbc_l, dt_l, cos, sin, ssd_conv_w, ssd_conv_b,
                            ssd_a_log, ssd_dt_bias, ssd_d, ssd_norm, ctx_out)
    yc_c, yc_l = na_branch(q_c, k_c, v_c, q_l, k_l, v_l, na_rpb, ctx_out)

    def merge(ya, yb, yc, g):
        ga, gb, gc = jnp.split(jax.nn.sigmoid(g), N_BRANCH, axis=-1)
        y = ga * (ya @ w_branch_lru) + gb * (yb @ w_branch_ssd) + gc * (yc @ w_branch_na)
        return y @ w_out

    y_l = merge(ya_l, yb_l, yc_l, g_l)
    y_c = merge(ya_c, yb_c, yc_c, g_c) if ctx_out else None
    return y_c, y_l


def expert_choice_moe(v, w_router, w1, w3, w2):
    bsz, L, _ = v.shape
    cap = CAPACITY_FACTOR * L // N_EXPERTS
    aff = jax.nn.softmax((v @ w_router).astype(jnp.float32), axis=-1)
    g, idx = lax.top_k(jnp.swapaxes(aff, 1, 2), cap)
    bidx = jnp.arange(bsz)[:, None, None]
    xg = v[bidx, idx]
    hdn = jax.nn.silu(jnp.einsum('becd,edf->becf', xg, w1)) * jnp.einsum('becd,edf->becf', xg, w3)
    ye = jnp.einsum('becf,efd->becd', hdn, w2) * g[..., None].astype(v.dtype)
    return jnp.zeros_like(v).at[bidx, idx].add(ye)


def setup_inputs(seed: int = 0) -> dict:
    key = jax.random.key(seed)
    ks = jax.random.split(key, 40)
    f32 = jnp.float32
    D = D_MODEL

    def nrm(k, shape, s):
        return jax.random.normal(k, shape, f32) * s

    def gain(k, shape):
        return 1.0 + nrm(k, shape, 0.02)

    u = jax.random.uniform(ks[20], (DEPTH, 2, LRU_WIDTH), f32, 0.9, 0.999)
    sa = u ** (1.0 / LRU_C)
    dt0 = jnp.exp(jax.random.uniform(ks[22], (DEPTH, 2, SSD_HEADS), f32, math.log(1e-3), math.log(1e-1)))
    return {
        "x": nrm(ks[0], (BATCH, SEQ, D), 1.0),
        "c": nrm(ks[1], (BATCH, D), 1.0),
        "ctx": nrm(ks[2], (BATCH, CTX_LEN, D), 1.0),
        "c_ctx": nrm(ks[3], (D,), 1.0),
        "w_ada": nrm(ks[4], (DEPTH, D, 6 * D), 0.5 * D ** -0.5),
        "b_ada": nrm(ks[5], (DEPTH, 6 * D), 0.02),
        "norm_mix": gain(ks[6], (DEPTH, D)),
        "norm_ffn": gain(ks[7], (DEPTH, D)),
        "w_in": nrm(ks[8], (DEPTH, D, PROJ_WIDTH), D ** -0.5),
        "lru_conv_w": nrm(ks[9], (DEPTH, CONV_W, LRU_WIDTH), 0.5),
        "lru_conv_b": nrm(ks[10], (DEPTH, LRU_WIDTH), 0.02),
        "lru_w_r": nrm(ks[11], (DEPTH, 2, LRU_BLOCKS, LRU_BLOCK, LRU_BLOCK), LRU_BLOCK ** -0.5),
        "lru_b_r": nrm(ks[12], (DEPTH, 2, LRU_WIDTH), 0.02),
        "lru_w_i": nrm(ks[13], (DEPTH, 2, LRU_BLOCKS, LRU_BLOCK, LRU_BLOCK), LRU_BLOCK ** -0.5),
        "lru_b_i": nrm(ks[14], (DEPTH, 2, LRU_WIDTH), 0.02),
        "lru_lambda": jnp.log(sa) - jnp.log1p(-sa),
        "ssd_conv_w": nrm(ks[15], (DEPTH, CONV_W, SSD_CONV_DIM), 0.5),
        "ssd_conv_b": nrm(ks[16], (DEPTH, SSD_CONV_DIM), 0.02),
        "ssd_a_log": jnp.log(jax.random.uniform(ks[21], (DEPTH, 2, SSD_HEADS), f32, 1.0, 16.0)),
        "ssd_dt_bias": dt0 + jnp.log(-jnp.expm1(-dt0)),
        "ssd_d": 1.0 + nrm(ks[17], (DEPTH, SSD_HEADS), 0.1),
        "ssd_norm": gain(ks[18], (DEPTH, SSD_INNER)),
        "na_rpb": nrm(ks[19], (DEPTH, NA_HEADS, 2 * NA_WIN_R - 1, 2 * NA_WIN_C - 1), 0.1),
        "w_branch_lru": nrm(ks[23], (DEPTH, LRU_WIDTH, D), LRU_WIDTH ** -0.5),
        "w_branch_ssd": nrm(ks[24], (DEPTH, SSD_INNER, D), SSD_INNER ** -0.5),
        "w_branch_na": nrm(ks[25], (DEPTH, NA_WIDTH, D), NA_WIDTH ** -0.5),
        "w_out": nrm(ks[26], (DEPTH, D, D), D ** -0.5),
        "w_router": nrm(ks[27], (DEPTH, D, N_EXPERTS), D ** -0.5),
        "w1": nrm(ks[28], (DEPTH, N_EXPERTS, D, EXPERT_FF), D ** -0.5),
        "w3": nrm(ks[29], (DEPTH, N_EXPERTS, D, EXPERT_FF), D ** -0.5),
        "w2": nrm(ks[30], (DEPTH, N_EXPERTS, EXPERT_FF, D), EXPERT_FF ** -0.5),
        "norm_final": gain(ks[31], (D,)),
    }


def reference(x, c, ctx, c_ctx, w_ada, b_ada, norm_mix, norm_ffn, w_in, lru_conv_w, lru_conv_b, lru_w_r, lru_b_r,
              lru_w_i, lru_b_i, lru_lambda, ssd_conv_w, ssd_conv_b, ssd_a_log, ssd_dt_bias, ssd_d, ssd_norm, na_rpb,
              w_branch_lru, w_branch_ssd, w_branch_na, w_out, w_router, w1, w3, w2, norm_final):
    L = x.shape[1]
    cos, sin = axial_rope(L, SSD_STATE, x.dtype)
    silu_c = jax.nn.silu(c)
    silu_cc = jax.nn.silu(c_ctx)
    h_l, h_c = x, ctx
    for l in range(DEPTH):
        ctx_out = l < DEPTH - 1
        mod_l = (silu_c @ w_ada[l] + b_ada[l])[:, None, :]
        mod_c = silu_cc @ w_ada[l] + b_ada[l]
        sh1_l, sc1_l, g1_l, sh2_l, sc2_l, g2_l = jnp.split(mod_l, 6, axis=-1)
        sh1_c, sc1_c, g1_c, sh2_c, sc2_c, g2_c = jnp.split(mod_c, 6, axis=-1)
        u_l = rmsnorm(h_l, norm_mix[l]) * (1.0 + sc1_l) + sh1_l
        u_c = rmsnorm(h_c, norm_mix[l]) * (1.0 + sc1_c) + sh1_c
        y_c, y_l = hybrid_mixer(u_c, u_l, cos, sin, w_in[l], lru_conv_w[l], lru_conv_b[l], lru_w_r[l], lru_b_r[l],
                                lru_w_i[l], lru_b_i[l], lru_lambda[l], ssd_conv_w[l], ssd_conv_b[l], ssd_a_log[l],
                                ssd_dt_bias[l], ssd_d[l], ssd_norm[l], na_rpb[l], w_branch_lru[l], w_branch_ssd[l],
                                w_branch_na[l], w_out[l], ctx_out)
        h_l = h_l + g1_l * y_l
        v_l = rmsnorm(h_l, norm_ffn[l]) * (1.0 + sc2_l) + sh2_l
        h_l = h_l + g2_l * expert_choice_moe(v_l, w_router[l], w1[l], w3[l], w2[l])
        if ctx_out:
            h_c = h_c + g1_c * y_c
            v_c = rmsnorm(h_c, norm_ffn[l]) * (1.0 + sc2_c) + sh2_c
            h_c = h_c + g2_c * expert_choice_moe(v_c, w_router[l], w1[l], w3[l], w2[l])
    return rmsnorm(h_l, norm_final)
```

```python
import contextlib
import numpy as np
import concourse.bass as bass
import concourse.mybir as mybir
from concourse.bass_utils import run_bass_kernel_spmd

F32 = mybir.dt.float32
F32R = mybir.dt.float32r
AF = mybir.ActivationFunctionType
ALU = mybir.AluOpType
AXL = mybir.AxisListType

D = 2048
NCTX = 256
NLAT = 4096
T = NCTX + NLAT
DEPTH = 4
EPS = 1e-6
PROJ = 14368
O_AX, O_AG, O_Z, O_XBC, O_DT, O_Q, O_K, O_V, O_G = 0, 1024, 2048, 3072, 5120, 5152, 6176, 7200, 8224
NEXP = 16
EFF = 1024


class Buf:
    __slots__ = ("t", "w", "r")

    def __init__(self, t=None):
        self.t = t
        self.w = {}
        self.r = {}


class K:
    def __init__(self, nc):
        self.nc = nc
        self.es = contextlib.ExitStack()
        self.eng = dict(pe=nc.tensor, act=nc.scalar, dve=nc.vector, pool=nc.gpsimd, sp=nc.sync)
        self.sem = {}
        self.cnt = {}
        for e in ("pe", "act", "dve", "pool"):
            self.sem[e] = self.es.enter_context(nc.semaphore("s_" + e))
            self.cnt[e] = 0
        self.waited = {e: {} for e in self.eng}
        self.lanes = {}
        for q, n in (("sp", 8), ("pool", 8), ("act", 4)):
            self.lanes[q] = [[self.es.enter_context(nc.semaphore("d_%s%d" % (q, i))), 0] for i in range(n)]
        self.lane_rr = {q: 0 for q in self.lanes}
        self.pst = self.es.enter_context(nc.psum_tensor("pst", [128, 4096], F32))
        self.psum = [Buf(self.pst[:, i * 512:(i + 1) * 512]) for i in range(8)]
        self.ps_rr = 0
        self.n_ins = 0
        self.fill_regs = {}

    def _wait(self, eng, tok):
        key, h, v = tok
        if v <= 0 or self.waited[eng].get(key, 0) >= v:
            return
        self.eng[eng].wait_ge(h, v)
        self.waited[eng][key] = v

    def _deps(self, eng, r, w):
        for b in r:
            for tok in b.w.values():
                if eng == "pe" and tok[0] == "s_pe":
                    continue
                self._wait(eng, tok)
        for b in w:
            for tok in b.w.values():
                if eng == "pe" and tok[0] == "s_pe":
                    continue
                self._wait(eng, tok)
            for tok in b.r.values():
                if eng == "pe" and tok[0] == "s_pe":
                    continue
                self._wait(eng, tok)

    def _mark(self, tok, r, w):
        for b in r:
            b.r[tok[0]] = tok
        for b in w:
            b.w[tok[0]] = tok

    def op(self, eng, name, *args, r=(), w=(), **kw):
        if name == "affine_select" and isinstance(kw.get("fill"), float):
            key = (eng, kw["fill"])
            if key not in self.fill_regs:
                self.fill_regs[key] = self.eng[eng].to_reg(kw["fill"])
            kw["fill"] = self.fill_regs[key]
        self._deps(eng, r, w)
        ins = getattr(self.eng[eng], name)(*args, **kw)
        self.cnt[eng] += 1
        ins.then_inc(self.sem[eng], 1)
        tok = ("s_" + eng, self.sem[eng], self.cnt[eng])
        self._mark(tok, r, w)
        self.n_ins += 1
        return tok

    def dma(self, q, out, in_, r=(), w=(), **kw):
        lanes = self.lanes[q]
        i = self.lane_rr[q]
        self.lane_rr[q] = (i + 1) % len(lanes)
        lane = lanes[i]
        key = "d_%s%d" % (q, i)
        self._deps(q, r, w)
        self._wait(q, (key, lane[0], lane[1] * 16))
        ins = self.eng[q].dma_start(out=out, in_=in_, **kw)
        lane[1] += 1
        ins.then_inc(lane[0], 16)
        tok = (key, lane[0], lane[1] * 16)
        self._mark(tok, r, w)
        self.n_ins += 1
        return tok

    def barrier(self, engines=None):
        toks = [("s_" + e, self.sem[e], self.cnt[e]) for e in self.sem]
        for q, lanes in self.lanes.items():
            for i, lane in enumerate(lanes):
                toks.append(("d_%s%d" % (q, i), lane[0], lane[1] * 16))
        for e in (engines or self.eng):
            for tok in toks:
                if tok[0] == "s_" + e:
                    continue
                self._wait(e, tok)

    def next_psum(self):
        b = self.psum[self.ps_rr]
        self.ps_rr = (self.ps_rr + 1) % 8
        return b

    def psum_group(self, nb):
        if self.ps_rr + nb > 8:
            self.ps_rr = 0
        i = self.ps_rr
        self.ps_rr = (i + nb) % 8
        return self.psum[i:i + nb], self.pst[:, i * 512:(i + nb) * 512]

    @contextlib.contextmanager
    def phase(self):
        ph = Phase(self)
        try:
            yield ph
        finally:
            self.barrier()
            ph.es.close()


_UID = [0]


class Phase:
    def __init__(self, k):
        self.k = k
        self.es = contextlib.ExitStack()
        self.n = 0

    def sb(self, shape, dtype=F32, name=None):
        _UID[0] += 1
        t = self.es.enter_context(self.k.nc.sbuf_tensor("%s_%d" % (name or "t", _UID[0]), list(shape), dtype))
        return Buf(t)


class Ring:
    def __init__(self, ph, n, shape, dtype=F32, name="ring"):
        self.bufs = [ph.sb(shape, dtype, name) for _ in range(n)]
        self.i = 0

    def next(self):
        b = self.bufs[self.i]
        self.i = (self.i + 1) % len(self.bufs)
        return b


def R_(ap):
    return ap.bitcast(F32R)


def F_(ap):
    return ap.bitcast(F32)


def gemm(k, xT, KC, subs, W_ap, blocks, wring, round_eng="pool"):
    Wv = W_ap.rearrange("(kc p) c -> p kc c", p=128)
    loaded = {}

    def load(i):
        c0, ncw, mode, ev = blocks[i]
        wb = wring.next()
        k.dma("pool", wb.t[:, 0:KC, 0:ncw], Wv[:, :, c0:c0 + ncw], w=[wb])
        loaded[i] = wb

    load(0)
    for i, (c0, ncw, mode, ev) in enumerate(blocks):
        if i + 1 < len(blocks):
            load(i + 1)
        wb = loaded.pop(i)
        if mode == "fm":
            for cc in range(0, ncw, 128):
                m = min(128, ncw - cc)
                for (t0, n) in subs["fm"]:
                    ps = k.next_psum()
                    for kc in range(KC):
                        k.op("pe", "matmul", ps.t[0:m, 0:n], wb.t[:, kc, cc:cc + m], xT.t[:, kc, t0:t0 + n],
                             start=(kc == 0), stop=(kc == KC - 1), r=[wb, xT], w=[ps])
                    ev(ps, m, n, c0 + cc, t0)
        else:
            for (t0, n) in subs["tm"]:
                ps = k.next_psum()
                for kc in range(KC):
                    k.op("pe", "matmul", ps.t[0:n, 0:ncw], xT.t[:, kc, t0:t0 + n], wb.t[:, kc, 0:ncw],
                         start=(kc == 0), stop=(kc == KC - 1), r=[wb, xT], w=[ps])
                ev(ps, n, ncw, c0, t0)


def tok_subs(nt):
    return {"fm": [(t, min(512, nt - t)) for t in range(0, nt, 512)],
            "tm": [(t, min(128, nt - t)) for t in range(0, nt, 128)]}


def load_xT(k, xb, src_ap, KC, t0, nt, eng="dve"):
    v = src_ap.rearrange("(kc p) t -> p kc t", p=128)
    k.dma("pool", xb.t[:, 0:KC, 0:nt], v[:, :, t0:t0 + nt], w=[xb])


class Evac:
    def __init__(self, k, ph, n=4):
        self.k = k
        self.ring = Ring(ph, n, [128, 512], F32, "stg")
        self.flip = 0

    def copy(self, st, ps, m, n, func=None):
        k = self.k
        if func is not None:
            k.op("act", "activation", st.t[0:m, 0:n], ps.t[0:m, 0:n], func, r=[ps], w=[st])
        else:
            self.flip ^= 1
            if self.flip:
                k.op("dve", "tensor_copy", st.t[0:m, 0:n], ps.t[0:m, 0:n], r=[ps], w=[st])
            else:
                k.op("act", "activation", st.t[0:m, 0:n], ps.t[0:m, 0:n], AF.Copy, r=[ps], w=[st])

    def fm(self, dst, row0=0, func=None, tbase=0):
        def ev(ps, m, n, c, t0):
            st = self.ring.next()
            self.copy(st, ps, m, n, func)
            self.k.dma("sp", dst[row0 + c:row0 + c + m, tbase + t0:tbase + t0 + n], st.t[0:m, 0:n], r=[st])
        return ev

    def tm(self, dst, col0=0, func=None, tbase=0):
        def ev(ps, n, ncw, c, t0):
            st = self.ring.next()
            self.copy(st, ps, n, ncw, func)
            self.k.dma("sp", dst[tbase + t0:tbase + t0 + n, col0 + c:col0 + c + ncw], st.t[0:n, 0:ncw], r=[st])
        return ev


class Prog:
    def __init__(self, dbg=()):
        self.dbg = set(dbg)
        nc = bass.Bass("TRN2", target_bir_lowering=False)
        self.nc = nc
        self.k = K(nc)
        self.inp = {}
        self.scr = {}
        self.old_moe = False

    def din(self, name, shape):
        ap = self.nc.dram_tensor(name, list(shape), F32, kind="ExternalInput").ap()
        self.inp[name] = ap
        return ap

    def dscr(self, name, shape):
        kind = "ExternalOutput" if name in self.dbg else "Internal"
        ap = self.nc.dram_tensor(name, list(shape), F32, kind=kind).ap()
        self.scr[name] = ap
        return ap

    def dump(self, name, buf, ap, shape):
        if ("dump_" + name) not in self.dbg:
            return
        o = self.nc.dram_tensor("dump_" + name, list(shape), F32, kind="ExternalOutput").ap()
        self.k.dma("sp", o, ap, r=[buf])

    def dout(self, name, shape):
        return self.nc.dram_tensor(name, list(shape), F32, kind="ExternalOutput").ap()


def phase_ada(P, l):
    k = P.k
    with k.phase() as ph:
        cs = ph.sb([128, 16, 2], F32R, "cs")
        ctmp = ph.sb([128, 2, 16], F32, "ctmp")
        bias = ph.sb([2, 6 * D], F32, "adab")
        modrow = ph.sb([2, 6 * D], F32, "modrow")
        wring = Ring(ph, 2, [128, 16, 512], F32R, "wada")
        k.dma("sp", ctmp.t[:, :, :], P.inp["cvec"].rearrange("s (kc p) -> p s kc", p=128), w=[ctmp], allow_slow_non_contiguous=True)
        for s in range(2):
            k.op("act", "activation", cs.t[:, :, s], ctmp.t[:, s, :], AF.Silu, r=[ctmp], w=[cs])
        for s in range(2):
            k.dma("sp", bias.t[s:s + 1, :], P.inp["b_ada"][l:l + 1, :], w=[bias])

        def ev(ps, n, ncw, c, t0):
            k.op("dve", "tensor_tensor", modrow.t[0:2, c:c + ncw], ps.t[0:2, 0:ncw], bias.t[0:2, c:c + ncw], ALU.add,
                 r=[ps, bias], w=[modrow])

        blocks = [(c, 512, "tm", ev) for c in range(0, 6 * D, 512)]
        gemm(k, cs, 16, {"tm": [(0, 2)], "fm": []}, P.inp["w_ada"][l], blocks, wring)
        k.dma("sp", P.scr["MOD"][:, :], modrow.t[:, :], r=[modrow])


def bcast_rows(k, dst, src_row_ap, n=128):
    k.dma("sp", dst.t[0:n, :], src_row_ap.partition_broadcast(n) if hasattr(src_row_ap, "partition_broadcast") else src_row_ap, w=[dst])


def phase_norm(P, l, which, src, gain_row_ap, dstT, dst_tok=None, t_lo=0, t_hi=T):
    k = P.k
    MOD = P.scr["MOD"]
    with k.phase() as ph:
        ident = ph.sb([128, 128], F32, "ident")
        make_ident(k, ident)
        gsc = [ph.sb([128, D], F32, "gsc") for _ in range(2)]
        shb = [ph.sb([128, D], F32, "shb") for _ in range(2)]
        gb = ph.sb([128, D], F32, "gb")
        o_sh = (0 if which == 0 else 3) * D
        o_sc = o_sh + D
        k.dma("sp", gb.t[:, :], gain_row_ap.partition_broadcast(128), w=[gb])
        for s in range(2):
            k.dma("sp", shb[s].t[:, :], MOD[s, o_sh:o_sh + D].partition_broadcast(128), w=[shb[s]])
            k.dma("sp", gsc[s].t[:, :], MOD[s, o_sc:o_sc + D].partition_broadcast(128), w=[gsc[s]])
            k.op("dve", "scalar_tensor_tensor", gsc[s].t[:, :], gsc[s].t[:, :], 1.0, gb.t[:, :], ALU.add, ALU.mult,
                 r=[gsc[s], gb], w=[gsc[s]])
        hring = Ring(ph, 3, [128, D], F32, "h")
        vring = Ring(ph, 2, [128, D], F32, "v")
        junk = ph.sb([128, D], F32, "junk")
        stat = Ring(ph, 4, [128, 4], F32, "stat")
        tring = Ring(ph, 2, [128, 16, 512], F32, "uT")
        tb = None
        tiles = list(range(t_lo, t_hi, 128))
        hq = []

        def ldh(ti):
            hb_ = hring.next()
            k.dma("sp", hb_.t[:, :], src[tiles[ti]:tiles[ti] + 128, :], w=[hb_])
            hq.append(hb_)

        ldh(0)
        for ti, t0 in enumerate(tiles):
            s = 1 if t0 < NCTX else 0
            if ti + 1 < len(tiles):
                ldh(ti + 1)
            hb = hq.pop(0)
            st = stat.next()
            k.op("act", "activation", junk.t[:, :], hb.t[:, :], AF.Square, accum_out=st.t[:, 0:1], r=[hb], w=[junk, st])
            k.op("dve", "tensor_scalar", st.t[:, 1:2], st.t[:, 0:1], 1.0 / D, EPS, ALU.mult, ALU.add, r=[st], w=[st])
            k.op("act", "activation", st.t[:, 2:3], st.t[:, 1:2], AF.Sqrt, r=[st], w=[st])
            k.op("dve", "reciprocal", st.t[:, 3:4], st.t[:, 2:3], r=[st], w=[st])
            vb = vring.next()
            k.op("dve", "scalar_tensor_tensor", vb.t[:, :], hb.t[:, :], st.t[:, 3:4], gsc[s].t[:, :], ALU.mult, ALU.mult,
                 r=[hb, st, gsc[s]], w=[vb])
            k.op("pool", "tensor_tensor", vb.t[:, :], vb.t[:, :], shb[s].t[:, :], ALU.add, r=[vb, shb[s]], w=[vb])
            if dst_tok is not None:
                k.dma("sp", dst_tok[t0:t0 + 128, :], vb.t[:, :], r=[vb])
            j = ti % 4
            if j == 0:
                tb = tring.next()
            for g4 in range(4):
                ps = k.next_psum()
                for q in range(4):
                    kc = g4 * 4 + q
                    k.op("pe", "transpose", ps.t[:, q * 128:(q + 1) * 128], vb.t[:, kc * 128:(kc + 1) * 128], ident.t[:, :],
                         r=[vb, ident], w=[ps])
                eng = "act" if g4 % 2 == 0 else "dve"
                outv = tb.t[:, g4 * 4:(g4 + 1) * 4, j * 128:(j + 1) * 128]
                inv = ps.t[:, :].rearrange("p (q t) -> p q t", q=4)
                if eng == "act":
                    k.op("act", "activation", outv, inv, AF.Copy, r=[ps], w=[tb])
                else:
                    k.op("dve", "tensor_copy", outv, inv, r=[ps], w=[tb])
            if j == 3 or ti == len(tiles) - 1:
                tb0 = tiles[ti - j]
                nt = (j + 1) * 128
                k.dma("sp", dstT.rearrange("(kc p) t -> p kc t", p=128)[:, :, tb0:tb0 + nt], tb.t[:, :, 0:nt], r=[tb])


def make_ident(k, ident):
    k.op("pool", "memset", ident.t[:, :], 0.0, w=[ident])
    k.op("pool", "affine_select", ident.t[:, :], ident.t[:, :], pattern=[[-1, 128]], compare_op=ALU.not_equal, fill=1.0,
         base=0, channel_multiplier=1, r=[ident], w=[ident])


def phase_win(P, l):
    k = P.k
    S = P.scr
    with k.phase() as ph:
        evs = Evac(k, ph, 6)
        wring = Ring(ph, 2, [128, 16, 512], F32R, "win")
        xb = ph.sb([128, 16, 1024], F32R, "xT")
        W = P.inp["w_in"][l]
        for t0 in range(0, T, 1024):
            nt = min(1024, T - t0)
            load_xT(k, xb, S["UT"], 16, t0, nt)
            blocks = []
            for c in range(O_AX, O_AG, 512):
                blocks.append((c, 512, "fm", evs.fm(S["AXT"], -O_AX, None, t0)))
            for c in range(O_AG, O_Z, 512):
                blocks.append((c, 512, "fm", evs.fm(S["AGT"], -O_AG, AF.Gelu, t0)))
            for c in range(O_Z, O_XBC, 512):
                blocks.append((c, 512, "tm", evs.tm(S["Z"], -O_Z, AF.Silu, t0)))
            for c in range(O_XBC, O_DT, 512):
                blocks.append((c, 512, "fm", evs.fm(S["XBCT"], -O_XBC, None, t0)))
            blocks.append((O_DT, 32, "tm", evs.tm(S["DTR"], -O_DT, None, t0)))
            for c in range(O_Q, O_K, 512):
                blocks.append((c, 512, "fm", evs.fm(S["QT"], -O_Q, None, t0)))
            for c in range(O_K, O_V, 512):
                blocks.append((c, 512, "fm", evs.fm(S["KT"], -O_K, None, t0)))
            for c in range(O_V, O_G, 512):
                blocks.append((c, 512, "tm", evs.tm(S["V"], -O_V, None, t0)))
            for c in range(O_G, PROJ, 512):
                blocks.append((c, 512, "fm", evs.fm(S["GT"], -O_G, AF.Sigmoid, t0)))
            gemm(k, xb, 16, tok_subs(nt), W, blocks, wring)


SCRATCH = {
    "MOD": [2, 6 * D], "H": [T, D], "UT": [D, T], "VTOK": [T, D],
    "AXT": [1024, T], "AGT": [1024, T], "XBCT": [2048, T], "QT": [1024, T], "KT": [1024, T], "GT": [6144, T],
    "Z": [T, 1024], "DTR": [T, 32], "V": [T, 1024],
    "YAT": [1024, T], "YBT": [1024, T], "YCT": [1024, T], "YMT": [D, T],
    "XS": [T, 1024], "BTK": [T, 512], "BFT": [512, T], "CFT": [512, T], "YBR": [T, 1024],
    "POSM": [16, T], "PA": [T, 32], "YE": [16, 544, D],
}

NL = DEPTH


def weight_shapes():
    return {
        "w_ada": [NL, D, 6 * D], "b_ada": [NL, 6 * D], "norm_mix": [NL, D], "norm_ffn": [NL, D],
        "w_in": [NL, D, PROJ],
        "lru_conv_w": [NL, 4, 1024], "lru_conv_b": [NL, 1024], "lru_w_r": [NL, 2, 16, 64, 64], "lru_b_r": [NL, 2, 1024],
        "lru_w_i": [NL, 2, 16, 64, 64], "lru_b_i": [NL, 2, 1024], "lru_lambda": [NL, 2, 1024],
        "ssd_conv_w": [NL, 4, 2048], "ssd_conv_b": [NL, 2048], "ssd_a_log": [NL, 2, 16], "ssd_dt_bias": [NL, 2, 16],
        "ssd_d": [NL, 16], "ssd_norm": [NL, 1024],
        "w_branch_lru": [NL, 1024, D], "w_branch_ssd": [NL, 1024, D], "w_branch_na": [NL, 1024, D], "w_out": [NL, D, D],
        "w_router": [NL, D, 16], "w1": [NL, 16, D, EFF], "w3": [NL, 16, D, EFF], "w2": [NL, 16, EFF, D],
        "norm_final": [1, D],
    }


def declare(P, weights):
    P.din("hin", [T, D])
    P.din("cvec", [2, D])
    P.din("ropec", [128, T])
    P.din("ropes", [128, T])
    P.din("negmask", [64, 64])
    if "rpbx" in weights:
        P.din("rpbx", [NL, 16, 64, 15, 64])
    ws = weight_shapes()
    for n in weights:
        if n != "rpbx":
            P.din(n, ws[n])
    for n, s in SCRATCH.items():
        P.dscr(n, s)


def small_T(k, dst, src_ap, pattern, **kw):
    k.dma("sp", dst, src_ap.rearrange(pattern, **kw), allow_slow_non_contiguous=True)


def phase_lru(P, l):
    k = P.k
    S = P.scr
    I = P.inp
    SEGS = [(0, NCTX), (NCTX, T)]
    with k.phase() as ph:
        wg = ph.sb([128, 32, 128], F32R, "wg")
        prm = ph.sb([128, 8, 16], F32, "prm")
        lamt = ph.sb([128, 2, 8], F32, "lam")
        brt = ph.sb([128, 2, 8], F32, "br")
        bit = ph.sb([128, 2, 8], F32, "bi")
        c8 = ph.sb([128, 2, 8], F32, "c8")
        zt = ph.sb([128, 32, 128], F32, "zt")
        k.op("pool", "memset", zt.t[:, :, :], 0.0, w=[zt])
        k.op("dve", "tensor_copy", wg.t[:, :, :], zt.t[:, :, :], r=[zt], w=[wg])
        for c in range(8):
            for d in range(2):
                for g, nm in enumerate(("lru_w_r", "lru_w_i")):
                    idx = (c * 2 + d) * 2 + g
                    for hb in range(2):
                        k.dma("pool", wg.t[hb * 64:(hb + 1) * 64, idx, hb * 64:(hb + 1) * 64], I[nm][l, d, 2 * c + hb, :, :], w=[wg])
        for tap in range(4):
            k.dma("sp", prm.t[:, :, tap], I["lru_conv_w"][l, tap, :].rearrange("(c p) -> p c", p=128), w=[prm], allow_slow_non_contiguous=True)
        k.dma("sp", prm.t[:, :, 4], I["lru_conv_b"][l, :].rearrange("(c p) -> p c", p=128), w=[prm], allow_slow_non_contiguous=True)
        for tl, nm in ((lamt, "lru_lambda"), (brt, "lru_b_r"), (bit, "lru_b_i")):
            for d in range(2):
                k.dma("sp", tl.t[:, d, :], I[nm][l, d, :].rearrange("(c p) -> p c", p=128), w=[tl], allow_slow_non_contiguous=True)
        k.op("act", "activation", c8.t[:, :, :], lamt.t[:, :, :], AF.Exp, scale=-1.0, r=[lamt], w=[c8])
        k.op("act", "activation", c8.t[:, :, :], c8.t[:, :, :], AF.Ln, bias=1.0, r=[c8], w=[c8])
        k.op("dve", "tensor_scalar", c8.t[:, :, :], c8.t[:, :, :], -8.0, None, ALU.mult, r=[c8], w=[c8])

        ax = ph.sb([128, T], F32, "ax")
        xc = ph.sb([128, T], F32R, "xc")
        ag = ph.sb([128, T], F32, "ag")
        at = ph.sb([128, T], F32, "a")
        ut = ph.sb([128, T], F32, "u")
        sq = ph.sb([128, T], F32, "sq")
        hd = [ph.sb([128, T], F32, "hd%d" % d) for d in range(2)]
        for c in range(8):
            rows = slice(c * 128, (c + 1) * 128)
            k.dma("sp", ax.t[:, :], S["AXT"][rows, :], w=[ax])
            k.dma("sp", ag.t[:, :], S["AGT"][rows, :], w=[ag])
            k.op("dve", "tensor_scalar", xc.t[:, :], ax.t[:, :], prm.t[:, c, 2:3], prm.t[:, c, 4:5], ALU.mult, ALU.add,
                 r=[ax, prm], w=[xc])
            for (s0, s1) in SEGS:
                for tap, off in ((0, -2), (1, -1), (3, 1)):
                    if off < 0:
                        o = xc.t[:, s0 - off:s1]
                        i0 = ax.t[:, s0:s1 + off]
                    else:
                        o = xc.t[:, s0:s1 - off]
                        i0 = ax.t[:, s0 + off:s1]
                    k.op("dve", "scalar_tensor_tensor", o, i0, prm.t[:, c, tap:tap + 1], F_(o), ALU.mult, ALU.add,
                         r=[ax, prm, xc], w=[xc])
            for d in range(2):
                for g, (dst, bt) in enumerate(((at, brt), (ut, bit))):
                    idx = (c * 2 + d) * 2 + g
                    for t0 in range(0, T, 512):
                        n = min(512, T - t0)
                        ps = k.next_psum()
                        k.op("pe", "matmul", ps.t[:, 0:n], wg.t[:, idx, :], xc.t[:, t0:t0 + n], start=True, stop=True,
                             r=[wg, xc], w=[ps])
                        k.op("act", "activation", dst.t[:, t0:t0 + n], ps.t[:, 0:n], AF.Sigmoid, bias=bt.t[:, d, c:c + 1],
                             r=[ps, bt], w=[dst])
                k.op("act", "activation", at.t[:, :], at.t[:, :], AF.Exp, scale=c8.t[:, d, c:c + 1], r=[at, c8], w=[at])
                k.op("pool", "tensor_tensor", ut.t[:, :], ut.t[:, :], F_(xc.t[:, :]), ALU.mult, r=[ut, xc], w=[ut])
                k.op("dve", "tensor_tensor", sq.t[:, :], at.t[:, :], at.t[:, :], ALU.mult, r=[at], w=[sq])
                k.op("act", "activation", sq.t[:, :], sq.t[:, :], AF.Sqrt, scale=-1.0, bias=1.0, r=[sq], w=[sq])
                k.op("dve", "tensor_tensor", ut.t[:, :], ut.t[:, :], sq.t[:, :], ALU.mult, r=[ut, sq], w=[ut])
                h = hd[d]
                if d == 0:
                    k.op("dve", "tensor_tensor_scan", h.t[:, :], at.t[:, :], ut.t[:, :], 0.0, ALU.mult, ALU.add,
                         r=[at, ut], w=[h])
                else:
                    k.op("dve", "tensor_tensor_scan", h.t[:, 0:NCTX][:, ::-1], at.t[:, 0:NCTX][:, ::-1], ut.t[:, 0:NCTX][:, ::-1],
                         0.0, ALU.mult, ALU.add, r=[at, ut], w=[h])
                    k.op("dve", "tensor_tensor_scan", h.t[:, NCTX:T][:, ::-1], at.t[:, NCTX:T][:, ::-1], ut.t[:, NCTX:T][:, ::-1],
                         h.t[:, 0:1], ALU.mult, ALU.add, r=[at, ut, h], w=[h])
            k.op("pool", "tensor_tensor", hd[0].t[:, :], hd[0].t[:, :], hd[1].t[:, :], ALU.add, r=[hd[0], hd[1]], w=[hd[0]])
            k.op("dve", "tensor_tensor", hd[0].t[:, :], hd[0].t[:, :], ag.t[:, :], ALU.mult, r=[hd[0], ag], w=[hd[0]])
            k.dma("sp", S["YAT"][rows, :], hd[0].t[:, :], r=[hd[0]])


def make_consts(k, ph):
    c = {}
    c["ident"] = ph.sb([128, 128], F32, "ident")
    make_ident(k, c["ident"])
    for nm, pat, cm in (("trif", [[1, 128]], -1), ("trib", [[-1, 128]], 1)):
        t = ph.sb([128, 128], F32, nm)
        k.op("pool", "memset", t.t[:, :], 1.0, w=[t])
        k.op("pool", "affine_select", t.t[:, :], t.t[:, :], pattern=pat, compare_op=ALU.is_ge, fill=0.0, base=0,
             channel_multiplier=cm, r=[t], w=[t])
        c[nm] = t
    ones = ph.sb([128, 128], F32, "ones")
    k.op("pool", "memset", ones.t[:, :], 1.0, w=[ones])
    c["ones"] = ones
    sw = ph.sb([128, 128], F32, "swapf")
    k.op("pool", "memset", sw.t[:, :], 0.0, w=[sw])
    for base in (-64, 64):
        k.op("pool", "affine_select", sw.t[:, :], sw.t[:, :], pattern=[[-1, 128]], compare_op=ALU.not_equal, fill=1.0,
             base=base, channel_multiplier=1, r=[sw], w=[sw])
    swr = ph.sb([128, 128], F32R, "swap")
    k.op("dve", "tensor_copy", swr.t[:, :], sw.t[:, :], r=[sw], w=[swr])
    c["swap"] = swr
    return c


def dwconv(k, xc, ax, prm, c, segs):
    k.op("dve", "tensor_scalar", xc.t[:, :], ax.t[:, :], prm.t[:, c, 2:3], prm.t[:, c, 4:5], ALU.mult, ALU.add,
         r=[ax, prm], w=[xc])
    for (s0, s1) in segs:
        for tap, off in ((0, -2), (1, -1), (3, 1)):
            if off < 0:
                o = xc.t[:, s0 - off:s1]
                i0 = ax.t[:, s0:s1 + off]
            else:
                o = xc.t[:, s0:s1 - off]
                i0 = ax.t[:, s0 + off:s1]
            k.op("dve", "scalar_tensor_tensor", o, i0, prm.t[:, c, tap:tap + 1], F_(o), ALU.mult, ALU.add,
                 r=[ax, prm, xc], w=[xc])


def to_token_major(k, ident, src_buf, src_view, dst, col0, stg_ring, nt=T):
    dv = dst.rearrange("(tt p) c -> p tt c", p=128)
    ntile = nt // 128
    for tt0 in range(0, ntile, 4):
        nq = min(4, ntile - tt0)
        ps = k.next_psum()
        for q in range(nq):
            t0 = (tt0 + q) * 128
            k.op("pe", "transpose", ps.t[:, q * 128:(q + 1) * 128], src_view[:, t0:t0 + 128], ident.t[:, :], r=[ident, src_buf], w=[ps])
        st = stg_ring.next()
        k.op("act", "activation", st.t[:, 0:nq * 128], ps.t[:, 0:nq * 128], AF.Copy, r=[ps], w=[st])
        k.dma("sp", dv[:, tt0:tt0 + nq, col0:col0 + 128], st.t[:, 0:nq * 128].rearrange("p (q c) -> p q c", q=nq), r=[st])


def phase_ssd_prep(P, l):
    k = P.k
    S = P.scr
    I = P.inp
    SEGS = [(0, NCTX), (NCTX, T)]
    with k.phase() as ph:
        C = make_consts(k, ph)
        prm = ph.sb([128, 16, 8], F32, "prm")
        for tap in range(4):
            k.dma("sp", prm.t[:, :, tap], I["ssd_conv_w"][l, tap, :].rearrange("(c p) -> p c", p=128), w=[prm], allow_slow_non_contiguous=True)
        k.dma("sp", prm.t[:, :, 4], I["ssd_conv_b"][l, :].rearrange("(c p) -> p c", p=128), w=[prm], allow_slow_non_contiguous=True)
        cosf = ph.sb([128, T], F32, "cosf")
        sinf = ph.sb([128, T], F32, "sinf")
        k.dma("sp", cosf.t[:, :], I["ropec"][:, :], w=[cosf])
        k.dma("sp", sinf.t[:, :], I["ropes"][:, :], w=[sinf])
        axr = Ring(ph, 2, [128, T], F32, "ax")
        xcr = Ring(ph, 2, [128, T], F32R, "xc")
        rot = ph.sb([128, T], F32, "rot")
        tmp = ph.sb([128, T], F32, "tmp")
        stg = Ring(ph, 3, [128, 512], F32, "stg")
        for c in range(16):
            ax = axr.next()
            xc = xcr.next()
            k.dma("sp", ax.t[:, :], S["XBCT"][c * 128:(c + 1) * 128, :], w=[ax])
            dwconv(k, xc, ax, prm, c, SEGS)
            k.op("act", "activation", xc.t[:, :], F_(xc.t[:, :]), AF.Silu, r=[xc], w=[xc])
            if c < 8:
                to_token_major(k, C["ident"], xc, F_(xc.t[:, :]), S["XS"], c * 128, stg)
                k_ = None
            else:
                k.op("pool", "tensor_tensor", rot.t[:, :], F_(xc.t[:, :]), cosf.t[:, :], ALU.mult, r=[xc, cosf], w=[rot])
                for t0 in range(0, T, 512):
                    n = min(512, T - t0)
                    ps = k.next_psum()
                    k.op("pe", "matmul", ps.t[:, 0:n], C["swap"].t[:, :], xc.t[:, t0:t0 + n], start=True, stop=True,
                         r=[C["swap"], xc], w=[ps])
                    k.op("dve", "tensor_tensor", tmp.t[:, t0:t0 + n], ps.t[:, 0:n], sinf.t[:, t0:t0 + n], ALU.mult,
                         r=[ps, sinf], w=[tmp])
                k.op("dve", "tensor_tensor", rot.t[:, :], rot.t[:, :], tmp.t[:, :], ALU.add, r=[rot, tmp], w=[rot])
                if c < 12:
                    g = c - 8
                    k.dma("sp", S["BFT"][g * 128:(g + 1) * 128, :], rot.t[:, :], r=[rot])
                    to_token_major(k, C["ident"], rot, rot.t[:, :], S["BTK"], g * 128, stg)
                else:
                    g = c - 12
                    k.dma("sp", S["CFT"][g * 128:(g + 1) * 128, :], rot.t[:, :], r=[rot])


def phase_ssd(P, l):
    k = P.k
    S = P.scr
    I = P.inp
    NCH = T // 128
    with k.phase() as ph:
        C = make_consts(k, ph)
        dtb = ph.sb([128, 32], F32, "dtb")
        alog = ph.sb([128, 32], F32, "alog")
        dsk = ph.sb([128, 16], F32, "dsk")
        gn = ph.sb([128, 1024], F32, "gn")
        k.dma("sp", dtb.t[:, :], I["ssd_dt_bias"][l].rearrange("d h -> (d h)").partition_broadcast(128), w=[dtb])
        k.dma("sp", alog.t[:, :], I["ssd_a_log"][l].rearrange("d h -> (d h)").partition_broadcast(128), w=[alog])
        k.dma("sp", dsk.t[:, :], I["ssd_d"][l, :].partition_broadcast(128), w=[dsk])
        k.dma("sp", gn.t[:, :], I["ssd_norm"][l, :].partition_broadcast(128), w=[gn])
        k.op("act", "activation", alog.t[:, :], alog.t[:, :], AF.Exp, r=[alog], w=[alog])
        k.op("dve", "tensor_scalar", alog.t[:, :], alog.t[:, :], -1.0, None, ALU.mult, r=[alog], w=[alog])
        Hf = ph.sb([128, 1024], F32, "Hf")
        Hr = ph.sb([128, 1024], F32R, "Hr")
        xsr = Ring(ph, 2, [128, 1024], F32, "xs")
        xdtr = Ring(ph, 2, [128, 1024], F32R, "xdt")
        xddr = Ring(ph, 2, [128, 1024], F32R, "xdd")
        btkr = Ring(ph, 2, [128, 512], F32R, "btk")
        bftr = Ring(ph, 2, [128, 4, 128], F32R, "bft")
        cftr = Ring(ph, 2, [128, 4, 128], F32R, "cft")
        Ltr = Ring(ph, 2, [128, 16, 128], F32, "Lt")
        MTr = Ring(ph, 2, [128, 16, 128], F32R, "MT")
        ychr = Ring(ph, 2, [128, 1024], F32, "ych")
        tmpr = Ring(ph, 2, [128, 1024], F32, "ytmp")
        zr = Ring(ph, 2, [128, 1024], F32, "z")
        ypr = Ring(ph, 2, [128, 1024], F32, "yprev")
        junk = ph.sb([128, 1024], F32, "junk")
        stat = Ring(ph, 4, [128, 4], F32, "stat")
        stg = Ring(ph, 3, [128, 512], F32, "stg")
        bft_v = S["BFT"].rearrange("(g n) t -> n g t", n=128)
        cft_v = S["CFT"].rearrange("(g n) t -> n g t", n=128)
        ybt_v = S["YBT"].rearrange("(c p) t -> p c t", p=128)

        def b16(ap, n):
            return ap.unsqueeze(2).broadcast_to([128, 16, n])

        def v3(ap):
            return ap.rearrange("p (h q) -> p h q", h=16)

        SM = ph.sb([128, NCH, 9, 16], F32, "SM")
        cbmr = Ring(ph, 2, [128, 4, 128], F32, "cbm")
        dtr_v = S["DTR"].rearrange("(c p) h -> p c h", p=128)

        def cb16(ap):
            return ap.unsqueeze(1).broadcast_to([128, NCH, 16])

        for d in range(2):
            tri = C["trif"] if d == 0 else C["trib"]
            order = list(range(NCH)) if d == 0 else [1, 0] + list(range(NCH - 1, 1, -1))
            k.dma("sp", SM.t[:, :, 0, :], dtr_v[:, :, d * 16:(d + 1) * 16], w=[SM])
            k.op("dve", "tensor_tensor", SM.t[:, :, 0, :], SM.t[:, :, 0, :], cb16(dtb.t[:, d * 16:(d + 1) * 16]), ALU.add, r=[SM, dtb], w=[SM])
            k.op("act", "activation", SM.t[:, :, 0, :], SM.t[:, :, 0, :], AF.Exp, r=[SM], w=[SM])
            k.op("act", "activation", SM.t[:, :, 0, :], SM.t[:, :, 0, :], AF.Ln, bias=1.0, r=[SM], w=[SM])
            k.op("dve", "tensor_tensor", SM.t[:, :, 1, :], SM.t[:, :, 0, :], cb16(alog.t[:, d * 16:(d + 1) * 16]), ALU.mult, r=[SM, alog], w=[SM])
            HC = NCH // 2
            for (mat, slot) in ((tri, 2), (C["ones"], 3)):
                for half in range(2):
                    ps = k.next_psum()
                    k.op("pe", "matmul", ps.t[:, 0:HC * 16].rearrange("p (c h) -> p c h", h=16), mat.t[:, :], SM.t[:, half * HC:(half + 1) * HC, 1, :],
                         start=True, stop=True, r=[mat, SM], w=[ps])
                    k.op("dve", "tensor_copy", SM.t[:, half * HC:(half + 1) * HC, slot, :], ps.t[:, 0:HC * 16].rearrange("p (c h) -> p c h", h=16),
                         r=[ps], w=[SM])
            k.op("dve", "tensor_tensor", SM.t[:, :, 7, :], SM.t[:, :, 3, :], SM.t[:, :, 2, :], ALU.subtract, r=[SM], w=[SM])
            k.op("act", "activation", SM.t[:, :, 4, :], SM.t[:, :, 2, :], AF.Exp, r=[SM], w=[SM])
            k.op("act", "activation", SM.t[:, :, 5, :], SM.t[:, :, 7, :], AF.Exp, r=[SM], w=[SM])
            k.op("act", "activation", SM.t[:, :, 6, :], SM.t[:, :, 3, :], AF.Exp, r=[SM], w=[SM])
            k.op("dve", "tensor_tensor", SM.t[:, :, 8, :], SM.t[:, :, 0, :], SM.t[:, :, 5, :], ALU.mult, r=[SM], w=[SM])
            k.op("pool", "memset", Hf.t[:, :], 0.0, w=[Hf])
            k.op("dve", "tensor_copy", Hr.t[:, :], Hf.t[:, :], r=[Hf], w=[Hr])
            for ci in order:
                t0 = ci * 128
                xs = xsr.next(); xdt = xdtr.next(); xdd = xddr.next(); btk = btkr.next(); bft = bftr.next(); cft = cftr.next()
                Lt = Ltr.next(); MT = MTr.next()

                def sm(i, ci=ci):
                    return SM.t[:, ci, i, :]

                k.dma("sp", xs.t[:, :], S["XS"][t0:t0 + 128, :], w=[xs])
                k.dma("pool", btk.t[:, :], S["BTK"][t0:t0 + 128, :], w=[btk])
                k.dma("pool", bft.t[:, :, :], bft_v[:, :, t0:t0 + 128], w=[bft])
                k.dma("pool", cft.t[:, :, :], cft_v[:, :, t0:t0 + 128], w=[cft])
                k.op("dve", "tensor_tensor", v3(xdt.t[:, :]), v3(xs.t[:, :]), b16(sm(0), 64), ALU.mult, r=[xs, SM], w=[xdt])
                k.op("dve", "tensor_tensor", v3(xdd.t[:, :]), v3(xs.t[:, :]), b16(sm(8), 64), ALU.mult, r=[xs, SM], w=[xdd])
                ab_, aap = k.psum_group(4)
                for h in range(16):
                    k.op("pe", "matmul", aap[:, h * 128:(h + 1) * 128], SM.t[:, ci, 1, h:h + 1].broadcast_to([128, 128]), tri.t[:, :],
                         start=True, stop=True, r=[SM, tri], w=[ab_[h // 4]])
                cb = k.next_psum()
                for g in range(4):
                    k.op("pe", "matmul", cb.t[:, g * 128:(g + 1) * 128], bft.t[:, g, :], cft.t[:, g, :], start=True, stop=True,
                         r=[bft, cft], w=[cb])
                cbm = cbmr.next()
                k.op("dve", "tensor_tensor", cbm.t[:, :, :], cb.t[:, :].rearrange("p (g i) -> p g i", g=4),
                     tri.t[:, :].unsqueeze(1).broadcast_to([128, 4, 128]), ALU.mult, r=[cb, tri], w=[cbm])
                for h in range(16):
                    k.op("dve", "tensor_scalar", Lt.t[:, h, :], aap[:, h * 128:(h + 1) * 128], SM.t[:, ci, 2, h:h + 1], 0.0,
                         ALU.subtract, ALU.min, r=[ab_[h // 4], SM], w=[Lt])
                k.op("act", "activation", Lt.t[:, :, :], Lt.t[:, :, :], AF.Exp, r=[Lt], w=[Lt])
                k.op("dve", "tensor_tensor", MT.t[:, :, :].rearrange("p (g r) i -> p g r i", g=4), Lt.t[:, :, :].rearrange("p (g r) i -> p g r i", g=4),
                     cbm.t[:, :, :].unsqueeze(2).broadcast_to([128, 4, 4, 128]), ALU.mult, r=[Lt, cbm], w=[MT])
                sb_, sap = k.psum_group(2)
                for g in range(4):
                    k.op("pe", "matmul", sap[:, g * 256:(g + 1) * 256], btk.t[:, g * 128:(g + 1) * 128], xdd.t[:, g * 256:(g + 1) * 256],
                         start=True, stop=True, r=[btk, xdd], w=[sb_[g // 2]])
                yb_, yap = k.psum_group(2)
                for h in range(16):
                    k.op("pe", "matmul", yap[:, h * 64:(h + 1) * 64], MT.t[:, h, :], xdt.t[:, h * 64:(h + 1) * 64], start=True, stop=True,
                         r=[MT, xdt], w=[yb_[h // 8]])
                ob_, oap = k.psum_group(2)
                for g in range(4):
                    k.op("pe", "matmul", oap[:, g * 256:(g + 1) * 256], cft.t[:, g, :], Hr.t[:, g * 256:(g + 1) * 256], start=True, stop=True,
                         r=[cft, Hr], w=[ob_[g // 2]])
                k.op("dve", "tensor_tensor", v3(Hf.t[:, :]), v3(Hf.t[:, :]), b16(sm(6), 64), ALU.mult, r=[Hf, SM], w=[Hf])
                k.op("dve", "tensor_tensor", Hf.t[:, :], Hf.t[:, :], sap, ALU.add, r=[Hf, sb_[0], sb_[1]], w=[Hf])
                k.op("act", "activation", Hr.t[:, :], Hf.t[:, :], AF.Copy, r=[Hf], w=[Hr])
                ych = ychr.next(); ytmp = tmpr.next()
                k.op("dve", "tensor_tensor", v3(ytmp.t[:, :]), v3(oap), b16(sm(4), 64), ALU.mult, r=[ob_[0], ob_[1], SM], w=[ytmp])
                k.op("dve", "tensor_tensor", ych.t[:, :], yap, ytmp.t[:, :], ALU.add, r=[yb_[0], yb_[1], ytmp], w=[ych])
                if d == 0:
                    k.dma("sp", S["YBR"][t0:t0 + 128, :], ych.t[:, :], r=[ych])
                else:
                    yp = ypr.next(); z = zr.next()
                    k.dma("sp", yp.t[:, :], S["YBR"][t0:t0 + 128, :], w=[yp])
                    k.dma("sp", z.t[:, :], S["Z"][t0:t0 + 128, :], w=[z])
                    k.op("pool", "tensor_tensor", ych.t[:, :], ych.t[:, :], yp.t[:, :], ALU.add, r=[ych, yp], w=[ych])
                    k.op("pool", "tensor_tensor", v3(ytmp.t[:, :]), v3(xs.t[:, :]), b16(dsk.t[:, :], 64), ALU.mult, r=[xs, dsk, ytmp], w=[ytmp])
                    k.op("pool", "tensor_tensor", ych.t[:, :], ych.t[:, :], ytmp.t[:, :], ALU.add, r=[ych, ytmp], w=[ych])
                    k.op("dve", "tensor_tensor", ych.t[:, :], ych.t[:, :], z.t[:, :], ALU.mult, r=[ych, z], w=[ych])
                    st = stat.next()
                    k.op("act", "activation", junk.t[:, :], ych.t[:, :], AF.Square, accum_out=st.t[:, 0:1], r=[ych], w=[junk, st])
                    k.op("dve", "tensor_scalar", st.t[:, 1:2], st.t[:, 0:1], 1.0 / 1024, EPS, ALU.mult, ALU.add, r=[st], w=[st])
                    k.op("act", "activation", st.t[:, 2:3], st.t[:, 1:2], AF.Sqrt, r=[st], w=[st])
                    k.op("dve", "reciprocal", st.t[:, 3:4], st.t[:, 2:3], r=[st], w=[st])
                    k.op("dve", "scalar_tensor_tensor", ych.t[:, :], ych.t[:, :], st.t[:, 3:4], gn.t[:, :], ALU.mult, ALU.mult,
                         r=[ych, st, gn], w=[ych])
                    for c0 in range(0, 8, 4):
                        ps = k.next_psum()
                        for q in range(4):
                            k.op("pe", "transpose", ps.t[:, q * 128:(q + 1) * 128], ych.t[:, (c0 + q) * 128:(c0 + q + 1) * 128], C["ident"].t[:, :],
                                 r=[ych, C["ident"]], w=[ps])
                        sg = stg.next()
                        k.op("act", "activation", sg.t[:, :], ps.t[:, :], AF.Copy, r=[ps], w=[sg])
                        k.dma("sp", ybt_v[:, c0:c0 + 4, t0:t0 + 128], sg.t[:, :].rearrange("p (q t) -> p q t", q=4), r=[sg])
            k.barrier()


def rope_tables():
    pos = np.arange(NLAT)
    row = (pos // 64).astype(np.float32)
    col = (pos % 64).astype(np.float32)
    freqs = (np.float32(10000.0) ** (-np.arange(32, dtype=np.float32) / np.float32(32))).astype(np.float32)
    ang = np.concatenate([row[:, None] * freqs, col[:, None] * freqs], axis=-1).astype(np.float32)
    cos = np.cos(ang).astype(np.float32).T
    sin = np.sin(ang).astype(np.float32).T
    c = np.ones((128, T), np.float32)
    s = np.zeros((128, T), np.float32)
    c[0:64, NCTX:] = cos
    c[64:128, NCTX:] = cos
    s[0:64, NCTX:] = -sin
    s[64:128, NCTX:] = sin
    return c, s


def na_tables(rpb):
    Ld = rpb.shape[0]
    kc = np.arange(64)[:, None]
    j = np.arange(64)[None, :]
    ci = np.clip(kc - j + 15, 0, 30)
    s = np.arange(15)
    x = rpb[:, :, 14 - s, :]
    x = x[:, :, :, ci]
    rpbx = np.ascontiguousarray(np.transpose(x, (0, 1, 3, 2, 4))).astype(np.float32)
    cs = np.clip(np.arange(64) - 8, 0, 48)[None, :]
    inside = (kc >= cs) & (kc < cs + 16)
    negmask = np.where(inside, 0.0, -30000.0).astype(np.float32)
    return rpbx, negmask


def phase_na(P, l):
    k = P.k
    S = P.scr
    I = P.inp

    def rs(r):
        return min(max(r - 4, 0), 56)

    with k.phase() as ph:
        C = make_consts(k, ph)
        negm = ph.sb([64, 64], F32, "negm")
        k.dma("sp", negm.t[:, :], I["negmask"][:, :], w=[negm])
        qTr = Ring(ph, 1, [64, T], F32R, "qT")
        kTr = Ring(ph, 1, [64, T], F32R, "kT")
        var = Ring(ph, 2, [64, 64, 66], F32R, "vaug")
        vcr = Ring(ph, 2, [128, 2, 66], F32R, "vc")
        tblr = Ring(ph, 2, [64, 15, 64], F32, "tbl")
        yctr = Ring(ph, 2, [64, T], F32, "yct")
        PTr = Ring(ph, 2, [64, 15, 512], F32R, "PT")
        PTcr = Ring(ph, 2, [128, 2, 512], F32R, "PTc")
        tmpr = Ring(ph, 3, [128, 512], F32, "natmp")
        yblk = Ring(ph, 2, [128, 8, 64], F32, "yblk")
        recr = Ring(ph, 2, [128, 8], F32, "rec")
        heads = {}

        def load_head(h):
            qT = qTr.next(); kT = kTr.next(); va = var.next(); vc = vcr.next(); tbl = tblr.next(); yct = yctr.next()
            k.dma("pool", qT.t[:, :], S["QT"][h * 64:(h + 1) * 64, :], w=[qT])
            k.dma("pool", kT.t[:, :], S["KT"][h * 64:(h + 1) * 64, :], w=[kT])
            k.dma("pool", va.t[:, :, 0:64], S["V"][NCTX:T, h * 64:(h + 1) * 64].rearrange("(r kc) d -> kc r d", kc=64), w=[va])
            k.dma("pool", vc.t[:, :, 0:64], S["V"][0:NCTX, h * 64:(h + 1) * 64].rearrange("(c p) d -> p c d", p=128), w=[vc])
            k.op("dve", "tensor_copy", va.t[:, :, 64:66], C["ones"].t[0:64, 0:128].rearrange("p (a b) -> p a b", b=2), r=[C["ones"]], w=[va])
            k.op("dve", "tensor_copy", vc.t[:, :, 64:66], C["ones"].t[:, 0:4].rearrange("p (a b) -> p a b", b=2), r=[C["ones"]], w=[vc])
            k.dma("sp", tbl.t[:, :, :], I["rpbx"][l, h], w=[tbl])
            k.op("pool", "tensor_tensor", tbl.t[:, :, :], tbl.t[:, :, :], negm.t[:, :].unsqueeze(1).broadcast_to([64, 15, 64]), ALU.add,
                 r=[tbl, negm], w=[tbl])
            heads[h] = dict(qT=qT, kT=kT, va=va, vc=vc, tbl=tbl, yct=yct)

        def stageS(item):
            h, b = item
            if b == "c":
                load_head(h)
            H = heads[h]
            qT, kT, tbl = H["qT"], H["kT"], H["tbl"]
            c = dict(item=item, H=H)
            PTc = c["PTc"] = PTcr.next()
            if b == "c":
                for cc in range(2):
                    ps = k.next_psum()
                    k.op("pe", "matmul", ps.t[:, 0:NCTX], kT.t[:, cc * 128:(cc + 1) * 128], qT.t[:, 0:NCTX], start=True, stop=True, r=[kT, qT], w=[ps])
                    k.op("act", "activation", PTc.t[:, cc, 0:NCTX], ps.t[:, 0:NCTX], AF.Exp, scale=0.125, r=[ps], w=[PTc])
                return c
            r0 = b * 8
            rows = list(range(r0, r0 + 8))
            Rlo = rs(r0); Rhi = rs(r0 + 7) + 7
            PT = c["PT"] = PTr.next()
            q0 = NCTX + r0 * 64
            for cc in range(2):
                ps = k.next_psum()
                k.op("pe", "matmul", ps.t[:, :], kT.t[:, cc * 128:(cc + 1) * 128], qT.t[:, q0:q0 + 512], start=True, stop=True, r=[kT, qT], w=[ps])
                k.op("act", "activation", PTc.t[:, cc, :], ps.t[:, :], AF.Exp, scale=0.125, r=[ps], w=[PTc])
            for R in range(Rlo, Rhi + 1):
                vr = [r for r in rows if rs(r) <= R <= rs(r) + 7]
                ra, rb = vr[0], vr[-1]
                nr = rb - ra + 1
                n = nr * 64
                s0 = 7 - R + ra
                ps = k.next_psum()
                k.op("pe", "matmul", ps.t[0:64, 0:n], kT.t[:, NCTX + R * 64:NCTX + (R + 1) * 64], qT.t[:, NCTX + ra * 64:NCTX + (rb + 1) * 64],
                     start=True, stop=True, r=[kT, qT], w=[ps])
                tm = tmpr.next()
                k.op("dve", "scalar_tensor_tensor", tm.t[0:64, 0:n], ps.t[0:64, 0:n], 0.125, tbl.t[:, s0:s0 + nr, :].rearrange("p a b -> p (a b)"),
                     ALU.mult, ALU.add, r=[ps, tbl], w=[tm])
                o0 = (ra - r0) * 64
                k.op("act", "activation", PT.t[:, R - Rlo, o0:o0 + n], tm.t[0:64, 0:n], AF.Exp, r=[tm], w=[PT])
            return c

        def stageV(c):
            h, b = c["item"]
            H = c["H"]
            va, vc, yct = H["va"], H["vc"], H["yct"]
            PTc = c["PTc"]
            if b == "c":
                ps2 = k.next_psum()
                for qc in range(2):
                    for cc in range(2):
                        k.op("pe", "matmul", ps2.t[:, qc * 66:(qc + 1) * 66], PTc.t[:, cc, qc * 128:(qc + 1) * 128], vc.t[:, cc, :],
                             start=(cc == 0), stop=(cc == 1), r=[PTc, vc], w=[ps2])
                rec = recr.next(); yb = yblk.next()
                p2v = ps2.t[:, 0:132].rearrange("p (a b) -> p a b", b=66)
                k.op("dve", "reciprocal", rec.t[:, 0:2], p2v[:, :, 64], r=[ps2], w=[rec])
                k.op("dve", "tensor_tensor", yb.t[:, 0:2, :], p2v[:, :, 0:64], rec.t[:, 0:2].unsqueeze(2).broadcast_to([128, 2, 64]), ALU.mult,
                     r=[ps2, rec], w=[yb])
                pst = k.next_psum()
                for qc in range(2):
                    k.op("pe", "transpose", pst.t[0:64, qc * 128:(qc + 1) * 128], yb.t[:, qc, :], C["ident"].t[:, :], r=[yb, C["ident"]], w=[pst])
                k.op("act", "activation", yct.t[:, 0:NCTX], pst.t[0:64, 0:NCTX], AF.Copy, r=[pst], w=[yct])
                return
            PT = c["PT"]
            r0 = b * 8
            rows = list(range(r0, r0 + 8))
            Rlo = rs(r0)
            q0 = NCTX + r0 * 64
            pb_, pap = k.psum_group(2)
            for ri, r in enumerate(rows):
                off = (ri // 4) * 512 + (ri % 4) * 66
                band = list(range(rs(r), rs(r) + 8))
                for bi, R in enumerate(band):
                    k.op("pe", "matmul", pap[0:64, off:off + 66], PT.t[:, R - Rlo, ri * 64:(ri + 1) * 64], va.t[:, R, :],
                         start=(bi == 0), stop=False, r=[PT, va], w=[pb_[ri // 4]])
                for cc in range(2):
                    k.op("pe", "matmul", pap[0:64, off:off + 66], PTc.t[:, cc, ri * 64:(ri + 1) * 64], vc.t[:, cc, :],
                         start=False, stop=(cc == 1), r=[PTc, vc], w=[pb_[ri // 4]])
            rec = recr.next(); yb = yblk.next()
            for half in range(2):
                pv = pap[0:64, half * 512:half * 512 + 264].rearrange("p (a b) -> p a b", b=66)
                k.op("dve", "reciprocal", rec.t[0:64, half * 4:half * 4 + 4], pv[:, :, 64], r=[pb_[half]], w=[rec])
                k.op("dve", "tensor_tensor", yb.t[0:64, half * 4:half * 4 + 4, :], pv[:, :, 0:64],
                     rec.t[0:64, half * 4:half * 4 + 4].unsqueeze(2).broadcast_to([64, 4, 64]), ALU.mult, r=[pb_[half], rec], w=[yb])
            pst = k.next_psum()
            for ri in range(8):
                k.op("pe", "transpose", pst.t[0:64, ri * 64:(ri + 1) * 64], yb.t[0:64, ri, :], C["ident"].t[0:64, 0:64], r=[yb, C["ident"]], w=[pst])
            k.op("act", "activation", yct.t[:, q0:q0 + 512], pst.t[0:64, :], AF.Copy, r=[pst], w=[yct])
            if b == 7:
                k.dma("sp", S["YCT"][h * 64:(h + 1) * 64, :], yct.t[:, :], r=[yct])

        items = [(h, b) for h in range(16) for b in (["c"] + list(range(8)))]
        pend = stageS(items[0])
        for i in range(len(items)):
            nxt = stageS(items[i + 1]) if i + 1 < len(items) else None
            stageV(pend)
            pend = nxt


def phase_merge(P, l):
    k = P.k
    S = P.scr
    I = P.inp
    srcs = [(S["YAT"], I["w_branch_lru"][l]), (S["YBT"], I["w_branch_ssd"][l]), (S["YCT"], I["w_branch_na"][l])]
    with k.phase() as ph:
        wring = Ring(ph, 2, [128, 8, 512], F32R, "wbr")
        xb = ph.sb([128, 8, 1024], F32R, "xT")
        acc = ph.sb([128, 16, 1024], F32, "acc")
        gring = Ring(ph, 4, [128, 512], F32, "g")
        tring = Ring(ph, 2, [128, 512], F32, "tmp")
        for tb0 in range(0, T, 1024):
            nt = min(1024, T - tb0)
            subs = tok_subs(nt)
            for br in range(3):
                load_xT(k, xb, srcs[br][0], 8, tb0, nt)
                seq = [(c0 + cc, t0, n) for c0 in range(0, D, 512) for cc in range(0, 512, 128) for (t0, n) in subs["fm"]]
                gq = []

                def ldg(i, br=br, seq=seq, gq=gq, tb0=tb0):
                    if i < len(seq):
                        c, t0, n = seq[i]
                        g = gring.next()
                        k.dma("sp", g.t[:, 0:n], S["GT"][br * D + c:br * D + c + 128, tb0 + t0:tb0 + t0 + n], w=[g])
                        gq.append(g)

                ldg(0); ldg(1)
                cnt = [0]

                def ev(ps, m, n, c, t0, br=br, gq=gq, cnt=cnt, ldg=ldg, tb0=tb0):
                    ldg(cnt[0] + 2)
                    cnt[0] += 1
                    g = gq.pop(0)
                    av = acc.t[:, c // 128, t0:t0 + n]
                    if br == 0:
                        k.op("dve", "tensor_tensor", av, ps.t[:, 0:n], g.t[:, 0:n], ALU.mult, r=[ps, g], w=[acc])
                    else:
                        tm = tring.next()
                        k.op("dve", "tensor_tensor", tm.t[:, 0:n], ps.t[:, 0:n], g.t[:, 0:n], ALU.mult, r=[ps, g], w=[tm])
                        k.op("pool", "tensor_tensor", av, av, tm.t[:, 0:n], ALU.add, r=[acc, tm], w=[acc])
                        if br == 2:
                            k.dma("sp", S["YMT"][c:c + 128, tb0 + t0:tb0 + t0 + n], av, r=[acc])

                blocks = [(c0, 512, "fm", ev) for c0 in range(0, D, 512)]
                gemm(k, xb, 8, subs, srcs[br][1], blocks, wring)


def load_mod_bcast(k, P, tiles, chunk):
    for s in range(2):
        k.dma("sp", tiles[s].t[:, :], P.scr["MOD"][s, chunk * D:(chunk + 1) * D].partition_broadcast(128), w=[tiles[s]])


def phase_wout(P, l, hsrc):
    k = P.k
    S = P.scr
    I = P.inp
    with k.phase() as ph:
        wring = Ring(ph, 2, [128, 16, 512], F32R, "wout")
        xb = ph.sb([128, 16, 1024], F32R, "xT")
        g1 = [ph.sb([128, D], F32, "g1") for _ in range(2)]
        load_mod_bcast(k, P, g1, 2)
        hring = Ring(ph, 4, [128, 512], F32, "h")
        tring = Ring(ph, 3, [128, 512], F32, "tmp")
        for tb0 in range(0, T, 1024):
            nt = min(1024, T - tb0)
            subs = tok_subs(nt)
            load_xT(k, xb, S["YMT"], 16, tb0, nt)
            seq = [(c0, t0, n) for c0 in range(0, D, 512) for (t0, n) in subs["tm"]]
            hq = []

            def ldh(i, seq=seq, hq=hq, tb0=tb0):
                if i < len(seq):
                    c, t0, n = seq[i]
                    h = hring.next()
                    k.dma("sp", h.t[0:n, :], hsrc[tb0 + t0:tb0 + t0 + n, c:c + 512], w=[h])
                    hq.append(h)

            ldh(0); ldh(1)
            cnt = [0]

            def ev(ps, n, ncw, c, t0, hq=hq, cnt=cnt, ldh=ldh, tb0=tb0):
                ldh(cnt[0] + 2)
                cnt[0] += 1
                h = hq.pop(0)
                s = 1 if (tb0 + t0) < NCTX else 0
                tm = tring.next()
                k.op("dve", "tensor_tensor", tm.t[0:n, :], ps.t[0:n, 0:ncw], g1[s].t[0:n, c:c + ncw], ALU.mult, r=[ps, g1[s]], w=[tm])
                k.op("pool", "tensor_tensor", tm.t[0:n, :], tm.t[0:n, :], h.t[0:n, :], ALU.add, r=[tm, h], w=[tm])
                k.dma("sp", S["H"][tb0 + t0:tb0 + t0 + n, c:c + ncw], tm.t[0:n, :], r=[tm])

            blocks = [(c0, 512, "tm", ev) for c0 in range(0, D, 512)]
            gemm(k, xb, 16, subs, I["w_out"][l], blocks, wring)


CAP_L = 512
CAP_C = 32
NSLOT = CAP_L + CAP_C


def phase_router(P, l):
    k = P.k
    S = P.scr
    I = P.inp
    with k.phase() as ph:
        C = make_consts(k, ph)
        wr = ph.sb([128, 16, 16], F32, "wr")
        k.dma("sp", wr.t[:, :, :], I["w_router"][l].rearrange("(kc p) e -> p kc e", p=128), w=[wr])
        affT = ph.sb([16, T], F32, "affT")
        work = ph.sb([16, T], F32, "work")
        mask = ph.sb([16, T], F32, "mask")
        ones16 = ph.sb([16, T], F32, "ones16")
        scan = ph.sb([16, T], F32, "scan")
        k.op("pool", "memset", ones16.t[:, :], 1.0, w=[ones16])
        xr = Ring(ph, 2, [128, 16, 512], F32, "vT")
        sm = Ring(ph, 4, [128, 64], F32, "rsm")
        uv = S["UT"].rearrange("(kc p) t -> p kc t", p=128)
        for tb0 in range(0, T, 512):
            nt = min(512, T - tb0)
            xb = xr.next()
            k.dma("sp", xb.t[:, :, 0:nt], uv[:, :, tb0:tb0 + nt], w=[xb])
            for t0 in range(0, nt, 128):
                ps = k.next_psum()
                for kc in range(16):
                    k.op("pe", "matmul", ps.t[:, 0:16], xb.t[:, kc, t0:t0 + 128], wr.t[:, kc, :], start=(kc == 0), stop=(kc == 15),
                         r=[xb, wr], w=[ps])
                s = sm.next()
                k.op("dve", "tensor_reduce", s.t[:, 0:1], ps.t[:, 0:16], axis=AXL.X, op=ALU.max, r=[ps], w=[s])
                k.op("dve", "tensor_scalar", s.t[:, 1:2], s.t[:, 0:1], -1.0, None, ALU.mult, r=[s], w=[s])
                k.op("act", "activation", s.t[:, 16:32], ps.t[:, 0:16], AF.Exp, bias=s.t[:, 1:2], accum_out=s.t[:, 2:3], r=[ps, s], w=[s])
                k.op("dve", "reciprocal", s.t[:, 3:4], s.t[:, 2:3], r=[s], w=[s])
                k.op("dve", "tensor_scalar", s.t[:, 32:48], s.t[:, 16:32], s.t[:, 3:4], None, ALU.mult, r=[s], w=[s])
                pt = k.next_psum()
                k.op("pe", "transpose", pt.t[0:16, 0:128], s.t[:, 32:48], C["ident"].t[:, :], r=[s, C["ident"]], w=[pt])
                k.op("act", "activation", affT.t[:, tb0 + t0:tb0 + t0 + 128], pt.t[0:16, 0:128], AF.Copy, r=[pt], w=[affT])
        thr = ph.sb([16, 8], F32, "thr")
        mx = Ring(ph, 2, [16, 8], F32, "mx")
        k.op("dve", "tensor_copy", work.t[:, :], affT.t[:, :], r=[affT], w=[work])
        for (s0, s1, cap, ti) in ((0, NCTX, CAP_C, 0), (NCTX, T, CAP_L, 1)):
            nit = cap // 8
            for it in range(nit):
                m = mx.next()
                k.op("dve", "max", m.t[:, :], work.t[:, s0:s1], r=[work], w=[m])
                if it < nit - 1:
                    k.op("dve", "match_replace", work.t[:, s0:s1], m.t[:, :], work.t[:, s0:s1], -1.0, r=[work, m], w=[work])
                else:
                    k.op("dve", "tensor_copy", thr.t[:, ti:ti + 1], m.t[:, 7:8], r=[m], w=[thr])
            k.op("dve", "tensor_scalar", mask.t[:, s0:s1], affT.t[:, s0:s1], thr.t[:, ti:ti + 1], None, ALU.is_ge, r=[affT, thr], w=[mask])
            k.op("dve", "tensor_tensor_scan", scan.t[:, s0:s1], ones16.t[:, s0:s1], mask.t[:, s0:s1], 0.0, ALU.mult, ALU.add,
                 r=[ones16, mask], w=[scan])
        k.op("dve", "tensor_tensor", scan.t[:, :], scan.t[:, :], mask.t[:, :], ALU.mult, r=[scan, mask], w=[scan])
        k.op("dve", "tensor_scalar", scan.t[:, :], scan.t[:, :], -1.0, None, ALU.add, r=[scan], w=[scan])
        k.op("pool", "tensor_tensor", mask.t[:, :], mask.t[:, :], affT.t[:, :], ALU.mult, r=[mask, affT], w=[mask])
        k.dma("sp", S["POSM"][:, :], scan.t[:, :], r=[scan])
        stg = Ring(ph, 3, [128, 32], F32, "pa")
        for t0 in range(0, T, 128):
            pt = k.next_psum()
            k.op("pe", "transpose", pt.t[:, 0:16], scan.t[:, t0:t0 + 128], C["ident"].t[0:16, 0:16], r=[scan, C["ident"]], w=[pt])
            k.op("pe", "transpose", pt.t[:, 16:32], mask.t[:, t0:t0 + 128], C["ident"].t[0:16, 0:16], r=[mask, C["ident"]], w=[pt])
            st = stg.next()
            k.op("act", "activation", st.t[:, :], pt.t[:, 0:32], AF.Copy, r=[pt], w=[st])
            k.dma("sp", S["PA"][t0:t0 + 128, :], st.t[:, :], r=[st])


def phase_moe1(P, l):
    k = P.k
    S = P.scr
    I = P.inp
    NT = T // 128
    with k.phase() as ph:
        pa = ph.sb([128, NT, 32], F32, "pa")
        k.dma("sp", pa.t[:, :, :], S["PA"].rearrange("(tt p) c -> p tt c", p=128), w=[pa])
        iota = ph.sb([128, 512], F32, "iota")
        k.op("pool", "iota", iota.t[:, :], pattern=[[1, 512]], base=0, channel_multiplier=0, allow_small_or_imprecise_dtypes=True, w=[iota])
        vctx = ph.sb([128, 2, D], F32R, "vctx")
        k.dma("pool", vctx.t[:, :, :], S["VTOK"][0:NCTX, :].rearrange("(tt p) c -> p tt c", p=128), w=[vctx])
        vring = Ring(ph, 3, [128, 1024], F32R, "vtok")
        selr = Ring(ph, 3, [128, 512], F32R, "sel")
        xg = ph.sb([128, 16, NSLOT], F32R, "xg")
        h1s = ph.sb([128, 8, NSLOT], F32, "h1s")
        hid = ph.sb([128, 8, NSLOT], F32R, "hid")
        wring = Ring(ph, 2, [128, 16, 512], F32R, "wexp")
        evs = Evac(k, ph, 4)
        subs = {"fm": [(0, 512), (512, CAP_C)], "tm": [(0, 128), (128, 128), (256, 128), (384, 128), (512, CAP_C)]}
        for e in range(NEXP):
            for half in range(2):
                k.ps_rr = 0
                for tt in range(2, NT):
                    vt = vring.next()
                    k.dma("pool", vt.t[:, :], S["VTOK"][tt * 128:(tt + 1) * 128, half * 1024:(half + 1) * 1024], w=[vt])
                    sel = selr.next()
                    k.op("dve", "tensor_scalar", sel.t[:, :], iota.t[:, :], pa.t[:, tt, e:e + 1], None, ALU.is_equal, r=[iota, pa], w=[sel])
                    for dc in range(8):
                        k.op("pe", "matmul", k.psum[dc].t[:, :], vt.t[:, dc * 128:(dc + 1) * 128], sel.t[:, :], start=(tt == 2), stop=(tt == NT - 1),
                             r=[vt, sel], w=[k.psum[dc]])
                for dc in range(8):
                    if dc % 2 == 0:
                        k.op("act", "activation", xg.t[:, half * 8 + dc, 0:512], k.psum[dc].t[:, :], AF.Copy, r=[k.psum[dc]], w=[xg])
                    else:
                        k.op("dve", "tensor_copy", xg.t[:, half * 8 + dc, 0:512], k.psum[dc].t[:, :], r=[k.psum[dc]], w=[xg])
            selc = [selr.next() for _ in range(2)]
            for tt in range(2):
                k.op("dve", "tensor_scalar", selc[tt].t[:, 0:CAP_C], iota.t[:, 0:CAP_C], pa.t[:, tt, e:e + 1], None, ALU.is_equal,
                     r=[iota, pa], w=[selc[tt]])
            ps = k.next_psum()
            for dc in range(16):
                for tt in range(2):
                    k.op("pe", "matmul", ps.t[:, dc * CAP_C:(dc + 1) * CAP_C], vctx.t[:, tt, dc * 128:(dc + 1) * 128], selc[tt].t[:, 0:CAP_C],
                         start=(tt == 0), stop=(tt == 1), r=[vctx, selc[tt]], w=[ps])
            k.op("dve", "tensor_copy", xg.t[:, :, 512:NSLOT], ps.t[:, :].rearrange("p (a b) -> p a b", b=CAP_C), r=[ps], w=[xg])

            def ev1(ps, m, n, c, t0):
                k.op("act", "activation", h1s.t[:, c // 128, t0:t0 + n], ps.t[:, 0:n], AF.Silu, r=[ps], w=[h1s])

            def ev3(ps, m, n, c, t0):
                k.op("dve", "tensor_tensor", hid.t[:, c // 128, t0:t0 + n], ps.t[:, 0:n], h1s.t[:, c // 128, t0:t0 + n], ALU.mult, r=[ps, h1s], w=[hid])

            gemm(k, xg, 16, subs, I["w1"][l, e], [(c0, 512, "fm", ev1) for c0 in range(0, EFF, 512)], wring)
            gemm(k, xg, 16, subs, I["w3"][l, e], [(c0, 512, "fm", ev3) for c0 in range(0, EFF, 512)], wring)
            gemm(k, hid, 8, subs, I["w2"][l, e], [(c0, 512, "tm", evs.tm(S["YE"][e], 0, None, 0)) for c0 in range(0, D, 512)], wring)


def phase_moe2(P, l):
    k = P.k
    S = P.scr
    NT = T // 128
    with k.phase() as ph:
        pa = ph.sb([128, NT, 32], F32, "pa")
        k.dma("sp", pa.t[:, :, :], S["PA"].rearrange("(tt p) c -> p tt c", p=128), w=[pa])
        posm = ph.sb([16, T], F32, "posm")
        k.dma("sp", posm.t[:, :], S["POSM"][:, :], w=[posm])
        iop = ph.sb([128, 4], F32, "iop")
        k.op("pool", "iota", iop.t[:, :], pattern=[[128, 4]], base=0, channel_multiplier=1, allow_small_or_imprecise_dtypes=True, w=[iop])
        sele = ph.sb([16, 16, 128], F32, "sele")
        k.op("pool", "memset", sele.t[:, :, :], 1.0, w=[sele])
        k.op("pool", "affine_select", sele.t[:, :, :], sele.t[:, :, :], pattern=[[-1, 16], [0, 128]], compare_op=ALU.is_equal, fill=0.0,
             base=0, channel_multiplier=1, r=[sele], w=[sele])
        g2 = [ph.sb([128, D], F32, "g2") for _ in range(2)]
        load_mod_bcast(k, P, g2, 5)
        acc = ph.sb([128, 4, D], F32, "acc")
        yer = Ring(ph, 2, [128, 5, D], F32R, "ye")
        selT = Ring(ph, 2, [128, 4, 512], F32R, "selT")
        hring = Ring(ph, 2, [128, D], F32, "h")
        blocks = [(0, 2, True)] + [(tt, 4, False) for tt in range(2, NT, 4)]
        for (tt0, ntile, is_ctx) in blocks:
            nt = ntile * 128
            tb0 = tt0 * 128
            for e in range(NEXP):
                ye = yer.next()
                if is_ctx:
                    k.dma("pool", ye.t[0:CAP_C, 4, :], S["YE"][e, CAP_L:NSLOT, :], w=[ye])
                else:
                    k.dma("pool", ye.t[:, 0:4, :], S["YE"][e, 0:CAP_L, :].rearrange("(sc p) c -> p sc c", p=128), w=[ye])
                pb = k.next_psum()
                k.op("pe", "matmul", pb.t[:, 0:nt], sele.t[:, e, :], posm.t[:, tb0:tb0 + nt], start=True, stop=True, r=[sele, posm], w=[pb])
                st = selT.next()
                if is_ctx:
                    k.op("dve", "tensor_scalar", st.t[0:CAP_C, 0, 0:nt], pb.t[0:CAP_C, 0:nt], iop.t[0:CAP_C, 0:1], None, ALU.is_equal, r=[pb, iop], w=[st])
                else:
                    for sc in range(4):
                        k.op("dve", "tensor_scalar", st.t[:, sc, 0:nt], pb.t[:, 0:nt], iop.t[:, sc:sc + 1], None, ALU.is_equal, r=[pb, iop], w=[st])
                for ti in range(ntile):
                    ob_, oap = k.psum_group(4)
                    for cb in range(4):
                        if is_ctx:
                            k.op("pe", "matmul", oap[:, cb * 512:(cb + 1) * 512], st.t[0:CAP_C, 0, ti * 128:(ti + 1) * 128], ye.t[0:CAP_C, 4, cb * 512:(cb + 1) * 512],
                                 start=True, stop=True, r=[st, ye], w=[ob_[cb]])
                        else:
                            for sc in range(4):
                                k.op("pe", "matmul", oap[:, cb * 512:(cb + 1) * 512], st.t[:, sc, ti * 128:(ti + 1) * 128], ye.t[:, sc, cb * 512:(cb + 1) * 512],
                                     start=(sc == 0), stop=(sc == 3), r=[st, ye], w=[ob_[cb]])
                    gate = pa.t[:, tt0 + ti, 16 + e:17 + e]
                    if e == 0:
                        k.op("dve", "tensor_scalar", acc.t[:, ti, :], oap, gate, None, ALU.mult, r=ob_ + [pa], w=[acc])
                    else:
                        k.op("dve", "scalar_tensor_tensor", acc.t[:, ti, :], oap, gate, acc.t[:, ti, :], ALU.mult, ALU.add, r=ob_ + [pa, acc], w=[acc])
            for ti in range(ntile):
                t0 = tb0 + ti * 128
                s = 1 if is_ctx else 0
                h = hring.next()
                k.dma("sp", h.t[:, :], S["H"][t0:t0 + 128, :], w=[h])
                k.op("pool", "tensor_tensor", acc.t[:, ti, :], acc.t[:, ti, :], g2[s].t[:, :], ALU.mult, r=[acc, g2[s]], w=[acc])
                k.op("dve", "tensor_tensor", h.t[:, :], h.t[:, :], acc.t[:, ti, :], ALU.add, r=[h, acc], w=[h])
                k.dma("sp", S["H"][t0:t0 + 128, :], h.t[:, :], r=[h])


def phase_final(P, out_ap):
    k = P.k
    S = P.scr
    with k.phase() as ph:
        gb = ph.sb([128, D], F32, "gb")
        k.dma("sp", gb.t[:, :], P.inp["norm_final"][0, :].partition_broadcast(128), w=[gb])
        hring = Ring(ph, 3, [128, D], F32, "h")
        junk = ph.sb([128, D], F32, "junk")
        stat = Ring(ph, 4, [128, 4], F32, "stat")
        for t0 in range(NCTX, T, 128):
            hb = hring.next()
            k.dma("sp", hb.t[:, :], S["H"][t0:t0 + 128, :], w=[hb])
            st = stat.next()
            k.op("act", "activation", junk.t[:, :], hb.t[:, :], AF.Square, accum_out=st.t[:, 0:1], r=[hb], w=[junk, st])
            k.op("dve", "tensor_scalar", st.t[:, 1:2], st.t[:, 0:1], 1.0 / D, EPS, ALU.mult, ALU.add, r=[st], w=[st])
            k.op("act", "activation", st.t[:, 2:3], st.t[:, 1:2], AF.Sqrt, r=[st], w=[st])
            k.op("dve", "reciprocal", st.t[:, 3:4], st.t[:, 2:3], r=[st], w=[st])
            k.op("dve", "scalar_tensor_tensor", hb.t[:, :], hb.t[:, :], st.t[:, 3:4], gb.t[:, :], ALU.mult, ALU.mult, r=[hb, st, gb], w=[hb])
            k.dma("sp", out_ap[t0 - NCTX:t0 - NCTX + 128, :], hb.t[:, :], r=[hb])


ALL_WEIGHTS = ["w_ada", "b_ada", "norm_mix", "norm_ffn", "w_in", "lru_conv_w", "lru_conv_b", "lru_w_r", "lru_b_r", "lru_w_i", "lru_b_i",
               "lru_lambda", "ssd_conv_w", "ssd_conv_b", "ssd_a_log", "ssd_dt_bias", "ssd_d", "ssd_norm", "rpbx",
               "w_branch_lru", "w_branch_ssd", "w_branch_na", "w_out", "w_router", "w1", "w3", "w2", "norm_final"]


def build_layer(P, l):
    S = P.scr
    hsrc = P.inp["hin"] if l == 0 else S["H"]
    phase_ada(P, l)
    phase_norm(P, l, 0, hsrc, P.inp["norm_mix"][l:l + 1, :], S["UT"])
    phase_win(P, l)
    phase_lru(P, l)
    phase_ssd_prep(P, l)
    phase_ssd(P, l)
    phase_na(P, l)
    phase_merge(P, l)
    phase_wout(P, l, hsrc)
    if "HMID" in P.dbg and l == 0:
        hm = P.nc.dram_tensor("HMID", [T, D], F32, kind="ExternalOutput").ap()
        P.k.dma("sp", hm[:, :], S["H"][:, :])
        P.k.barrier()
    phase_norm(P, l, 1, S["H"], P.inp["norm_ffn"][l:l + 1, :], S["UT"], dst_tok=S["VTOK"])
    phase_router(P, l)
    if P.old_moe:
        phase_moe1(P, l)
        phase_moe2(P, l)
    else:
        phase_moe(P, l)


def host_inputs(inputs, b, nl=None):
    nl = nl or NL
    rc, rs_ = rope_tables()
    rpbx, negmask = na_tables(np.asarray(inputs["na_rpb"][:nl]))
    im = {"hin": np.ascontiguousarray(np.concatenate([inputs["ctx"][b], inputs["x"][b]], 0)),
          "cvec": np.ascontiguousarray(np.stack([inputs["c"][b], inputs["c_ctx"]], 0)),
          "ropec": rc, "ropes": rs_, "negmask": negmask, "rpbx": rpbx}
    for n in ALL_WEIGHTS:
        if n == "rpbx":
            continue
        if n == "norm_final":
            im[n] = np.ascontiguousarray(np.asarray(inputs[n]).reshape(1, D))
        else:
            im[n] = np.ascontiguousarray(np.asarray(inputs[n][:nl]))
    return im


def build_program():
    P = Prog()
    declare(P, ALL_WEIGHTS)
    out = P.dout("out", [NLAT, D])
    for l in range(DEPTH):
        build_layer(P, l)
    phase_final(P, out)
    P.k.barrier()
    return P


def kernel(**inputs):
    inputs = {n: np.asarray(v) for n, v in inputs.items()}
    B = inputs["x"].shape[0]
    P = build_program()
    ims = [host_inputs(inputs, b, DEPTH) for b in range(B)]
    res = run_bass_kernel_spmd(P.nc, ims, core_ids=list(range(B)))
    return np.stack([np.asarray(res.results[b]["out"]) for b in range(B)], 0).astype(np.float32)


I32 = mybir.dt.int32


def idma(k, out, in_, r=(), w=(), out_idx=None, in_idx=None, **kw):
    q = "pool"
    lanes = k.lanes[q]
    i = k.lane_rr[q]
    k.lane_rr[q] = (i + 1) % len(lanes)
    lane = lanes[i]
    key = "d_%s%d" % (q, i)
    k._deps(q, r, w)
    k._wait(q, (key, lane[0], lane[1] * 16))
    ins = k.eng[q].indirect_dma_start(
        out=out, out_offset=(bass.IndirectOffsetOnAxis(ap=out_idx, axis=0) if out_idx is not None else None),
        in_=in_, in_offset=(bass.IndirectOffsetOnAxis(ap=in_idx, axis=0) if in_idx is not None else None), **kw)
    lane[1] += 1
    ins.then_inc(lane[0], 16)
    tok = (key, lane[0], lane[1] * 16)
    k._mark(tok, r, w)
    k.n_ins += 1
    return tok


def phase_moe(P, l):
    k = P.k
    S = P.scr
    I = P.inp
    NT = T // 128
    with k.phase() as ph:
        C = make_consts(k, ph)
        pa = ph.sb([128, NT, 32], F32, "pa")
        k.dma("sp", pa.t[:, :, :], S["PA"].rearrange("(tt p) c -> p tt c", p=128), w=[pa])
        iota = ph.sb([128, 512], F32, "iota")
        k.op("pool", "iota", iota.t[:, :], pattern=[[1, 512]], base=0, channel_multiplier=0, allow_small_or_imprecise_dtypes=True, w=[iota])
        tg = ph.sb([128, NT, 16, 2], F32, "tg")
        tid = ph.sb([128, NT, 16], F32, "tid")
        k.op("pool", "iota", tid.t[:, :, :], pattern=[[128, NT], [0, 16]], base=0, channel_multiplier=1, allow_small_or_imprecise_dtypes=True, w=[tid])
        k.op("dve", "tensor_copy", tg.t[:, :, :, 0], tid.t[:, :, :], r=[tid], w=[tg])
        k.op("dve", "tensor_copy", tg.t[:, :, :, 1], pa.t[:, :, 16:32], r=[pa], w=[tg])
        g2 = [ph.sb([128, D], F32, "g2") for _ in range(2)]
        load_mod_bcast(k, P, g2, 5)
        selr = Ring(ph, 2, [128, 512], F32, "sel")
        idxr = Ring(ph, 2, [128, 8], I32, "idx")
        gater = Ring(ph, 2, [128, 8], F32, "gate")
        xgr = Ring(ph, 3, [128, D], F32, "xgtok")
        yew = ph.sb([128, 5, D], F32, "yew")
        xg = ph.sb([128, 16, NSLOT], F32R, "xg")
        h1s = ph.sb([128, 8, NSLOT], F32, "h1s")
        hid = ph.sb([128, 8, NSLOT], F32R, "hid")
        wring = Ring(ph, 2, [128, 16, 256], F32R, "wexp")
        Hbuf = Buf()
        subs = {"fm": [(0, 512), (512, CAP_C)], "tm": [(0, 128), (128, 128), (256, 128), (384, 128), (512, CAP_C)]}
        NR = [128, 128, 128, 128, CAP_C]
        state = {}

        def idx_gather(e):
            idx = idxr.next(); gate = gater.next()
            k.ps_rr = 0
            banks = k.psum[0:5]
            for tt in range(NT):
                sel = selr.next()
                if tt < 2:
                    k.op("dve", "tensor_scalar", sel.t[:, 0:CAP_C], iota.t[:, 0:CAP_C], pa.t[:, tt, e:e + 1], None, ALU.is_equal, r=[iota, pa], w=[sel])
                    k.op("pe", "matmul", banks[4].t[0:CAP_C, 0:2], sel.t[:, 0:CAP_C], tg.t[:, tt, e, :], start=(tt == 0), stop=(tt == 1),
                         r=[sel, tg], w=[banks[4]])
                else:
                    k.op("dve", "tensor_scalar", sel.t[:, :], iota.t[:, :], pa.t[:, tt, e:e + 1], None, ALU.is_equal, r=[iota, pa], w=[sel])
                    for sc in range(4):
                        k.op("pe", "matmul", banks[sc].t[:, 0:2], sel.t[:, sc * 128:(sc + 1) * 128], tg.t[:, tt, e, :], start=(tt == 2), stop=(tt == NT - 1),
                             r=[sel, tg], w=[banks[sc]])
            for sc in range(5):
                n = NR[sc]
                k.op("dve", "tensor_copy", idx.t[0:n, sc:sc + 1], banks[sc].t[0:n, 0:1], r=[banks[sc]], w=[idx])
                k.op("dve", "tensor_copy", gate.t[0:n, sc:sc + 1], banks[sc].t[0:n, 1:2], r=[banks[sc]], w=[gate])
            k.ps_rr = 5
            chunks = []
            for sc in range(3):
                chunks.append(gather_chunk(idx, sc))
            state[e] = (idx, gate, chunks)

        def gather_chunk(idx, sc):
            n = NR[sc]
            xt = xgr.next()
            idma(k, xt.t[0:n, :], S["VTOK"][:, :], r=[idx], w=[xt], in_idx=idx.t[0:n, sc:sc + 1])
            return xt

        def transposes(e, idx, chunks):
            flip = 0
            for sc in range(5):
                xt = chunks[sc]
                if sc < 4:
                    for dc0 in range(0, 16, 4):
                        ps = k.next_psum()
                        for q in range(4):
                            dc = dc0 + q
                            k.op("pe", "transpose", ps.t[:, q * 128:(q + 1) * 128], xt.t[:, dc * 128:(dc + 1) * 128], C["ident"].t[:, :],
                                 r=[xt, C["ident"]], w=[ps])
                        outv = xg.t[:, dc0:dc0 + 4, sc * 128:(sc + 1) * 128]
                        inv = ps.t[:, :].rearrange("p (q t) -> p q t", q=4)
                        flip ^= 1
                        if flip:
                            k.op("act", "activation", outv, inv, AF.Copy, r=[ps], w=[xg])
                        else:
                            k.op("dve", "tensor_copy", outv, inv, r=[ps], w=[xg])
                else:
                    ps = k.next_psum()
                    for dc in range(16):
                        k.op("pe", "transpose", ps.t[:, dc * CAP_C:(dc + 1) * CAP_C], xt.t[0:CAP_C, dc * 128:(dc + 1) * 128], C["ident"].t[0:CAP_C, 0:CAP_C],
                             r=[xt, C["ident"]], w=[ps])
                    k.op("dve", "tensor_copy", xg.t[:, :, CAP_L:NSLOT], ps.t[:, :].rearrange("p (a b) -> p a b", b=CAP_C), r=[ps], w=[xg])
                if sc + 3 < 5:
                    chunks.append(gather_chunk(idx, sc + 3))

        idx_gather(0)
        for e in range(NEXP):
            idx, gate, chunks = state.pop(e)
            transposes(e, idx, chunks)
            if e + 1 < NEXP:
                idx_gather(e + 1)

            def ev1(ps, m, n, c, t0):
                k.op("act", "activation", h1s.t[0:m, c // 128, t0:t0 + n], ps.t[0:m, 0:n], AF.Silu, r=[ps], w=[h1s])

            def ev3(ps, m, n, c, t0):
                k.op("dve", "tensor_tensor", hid.t[0:m, c // 128, t0:t0 + n], ps.t[0:m, 0:n], h1s.t[0:m, c // 128, t0:t0 + n], ALU.mult, r=[ps, h1s], w=[hid])

            def ev2(ps, n, ncw, c, t0, gate=gate):
                sc = t0 // 128
                s = 1 if sc == 4 else 0
                k.op("dve", "scalar_tensor_tensor", yew.t[0:n, sc, c:c + ncw], ps.t[0:n, 0:ncw], gate.t[0:n, sc:sc + 1], g2[s].t[0:n, c:c + ncw],
                     ALU.mult, ALU.mult, r=[ps, gate, g2[s]], w=[yew])

            gemm(k, xg, 16, subs, I["w1"][l, e], [(c0, 256, "fm", ev1) for c0 in range(0, EFF, 256)], wring)
            gemm(k, xg, 16, subs, I["w3"][l, e], [(c0, 256, "fm", ev3) for c0 in range(0, EFF, 256)], wring)
            gemm(k, hid, 8, subs, I["w2"][l, e], [(c0, 256, "tm", ev2) for c0 in range(0, D, 256)], wring)
            for sc in range(5):
                n = NR[sc]
                idma(k, S["H"][:, :], yew.t[0:n, sc, :], r=[yew, idx], w=[Hbuf], out_idx=idx.t[0:n, sc:sc + 1], compute_op=ALU.add)
```

```python
import contextlib
import numpy as np
import concourse.bass as bass
import concourse.mybir as mybir
from concourse.bass_utils import run_bass_kernel_spmd

F32 = mybir.dt.float32
F32R = mybir.dt.float32r
AF = mybir.ActivationFunctionType
ALU = mybir.AluOpType
AXL = mybir.AxisListType

D = 2048
NCTX = 256
NLAT = 4096
T = NCTX + NLAT
DEPTH = 4
EPS = 1e-6
PROJ = 14368
O_AX, O_AG, O_Z, O_XBC, O_DT, O_Q, O_K, O_V, O_G = 0, 1024, 2048, 3072, 5120, 5152, 6176, 7200, 8224
NEXP = 16
EFF = 1024


class Buf:
    __slots__ = ("t", "w", "r")

    def __init__(self, t=None):
        self.t = t
        self.w = {}
        self.r = {}


class K:
    def __init__(self, nc):
        self.nc = nc
        self.es = contextlib.ExitStack()
        self.eng = dict(pe=nc.tensor, act=nc.scalar, dve=nc.vector, pool=nc.gpsimd, sp=nc.sync)
        self.sem = {}
        self.cnt = {}
        for e in ("pe", "act", "dve", "pool"):
            self.sem[e] = self.es.enter_context(nc.semaphore("s_" + e))
            self.cnt[e] = 0
        self.waited = {e: {} for e in self.eng}
        self.lanes = {}
        for q, n in (("sp", 8), ("pool", 8), ("act", 4)):
            self.lanes[q] = [[self.es.enter_context(nc.semaphore("d_%s%d" % (q, i))), 0] for i in range(n)]
        self.lane_rr = {q: 0 for q in self.lanes}
        self.pst = self.es.enter_context(nc.psum_tensor("pst", [128, 4096], F32))
        self.psum = [Buf(self.pst[:, i * 512:(i + 1) * 512]) for i in range(8)]
        self.ps_rr = 0
        self.n_ins = 0
        self.fill_regs = {}

    def _wait(self, eng, tok):
        key, h, v = tok
        if v <= 0 or self.waited[eng].get(key, 0) >= v:
            return
        self.eng[eng].wait_ge(h, v)
        self.waited[eng][key] = v

    def _deps(self, eng, r, w):
        for b in r:
            for tok in b.w.values():
                if eng == "pe" and tok[0] == "s_pe":
                    continue
                self._wait(eng, tok)
        for b in w:
            for tok in b.w.values():
                if eng == "pe" and tok[0] == "s_pe":
                    continue
                self._wait(eng, tok)
            for tok in b.r.values():
                if eng == "pe" and tok[0] == "s_pe":
                    continue
                self._wait(eng, tok)

    def _mark(self, tok, r, w):
        for b in r:
            b.r[tok[0]] = tok
        for b in w:
            b.w[tok[0]] = tok

    def op(self, eng, name, *args, r=(), w=(), **kw):
        if name == "affine_select" and isinstance(kw.get("fill"), float):
            key = (eng, kw["fill"])
            if key not in self.fill_regs:
                self.fill_regs[key] = self.eng[eng].to_reg(kw["fill"])
            kw["fill"] = self.fill_regs[key]
        self._deps(eng, r, w)
        ins = getattr(self.eng[eng], name)(*args, **kw)
        self.cnt[eng] += 1
        ins.then_inc(self.sem[eng], 1)
        tok = ("s_" + eng, self.sem[eng], self.cnt[eng])
        self._mark(tok, r, w)
        self.n_ins += 1
        return tok

    def dma(self, q, out, in_, r=(), w=(), **kw):
        lanes = self.lanes[q]
        i = self.lane_rr[q]
        self.lane_rr[q] = (i + 1) % len(lanes)
        lane = lanes[i]
        key = "d_%s%d" % (q, i)
        self._deps(q, r, w)
        self._wait(q, (key, lane[0], lane[1] * 16))
        ins = self.eng[q].dma_start(out=out, in_=in_, **kw)
        lane[1] += 1
        ins.then_inc(lane[0], 16)
        tok = (key, lane[0], lane[1] * 16)
        self._mark(tok, r, w)
        self.n_ins += 1
        return tok

    def barrier(self, engines=None):
        toks = [("s_" + e, self.sem[e], self.cnt[e]) for e in self.sem]
        for q, lanes in self.lanes.items():
            for i, lane in enumerate(lanes):
                toks.append(("d_%s%d" % (q, i), lane[0], lane[1] * 16))
        for e in (engines or self.eng):
            for tok in toks:
                if tok[0] == "s_" + e:
                    continue
                self._wait(e, tok)

    def next_psum(self):
        b = self.psum[self.ps_rr]
        self.ps_rr = (self.ps_rr + 1) % 8
        return b

    def psum_group(self, nb):
        if self.ps_rr + nb > 8:
            self.ps_rr = 0
        i = self.ps_rr
        self.ps_rr = (i + nb) % 8
        return self.psum[i:i + nb], self.pst[:, i * 512:(i + nb) * 512]

    @contextlib.contextmanager
    def phase(self):
        ph = Phase(self)
        try:
            yield ph
        finally:
            self.barrier()
            ph.es.close()


_UID = [0]


class Phase:
    def __init__(self, k):
        self.k = k
        self.es = contextlib.ExitStack()
        self.n = 0

    def sb(self, shape, dtype=F32, name=None):
        _UID[0] += 1
        t = self.es.enter_context(self.k.nc.sbuf_tensor("%s_%d" % (name or "t", _UID[0]), list(shape), dtype))
        return Buf(t)


class Ring:
    def __init__(self, ph, n, shape, dtype=F32, name="ring"):
        self.bufs = [ph.sb(shape, dtype, name) for _ in range(n)]
        self.i = 0

    def next(self):
        b = self.bufs[self.i]
        self.i = (self.i + 1) % len(self.bufs)
        return b


def R_(ap):
    return ap.bitcast(F32R)


def F_(ap):
    return ap.bitcast(F32)


def gemm(k, xT, KC, subs, W_ap, blocks, wring, round_eng="pool"):
    Wv = W_ap.rearrange("(kc p) c -> p kc c", p=128)
    loaded = {}

    def load(i):
        c0, ncw, mode, ev = blocks[i]
        wb = wring.next()
        k.dma("pool", wb.t[:, 0:KC, 0:ncw], Wv[:, :, c0:c0 + ncw], w=[wb])
        loaded[i] = wb

    load(0)
    for i, (c0, ncw, mode, ev) in enumerate(blocks):
        if i + 1 < len(blocks):
            load(i + 1)
        wb = loaded.pop(i)
        if mode == "fm":
            for cc in range(0, ncw, 128):
                m = min(128, ncw - cc)
                for (t0, n) in subs["fm"]:
                    ps = k.next_psum()
                    for kc in range(KC):
                        k.op("pe", "matmul", ps.t[0:m, 0:n], wb.t[:, kc, cc:cc + m], xT.t[:, kc, t0:t0 + n],
                             start=(kc == 0), stop=(kc == KC - 1), r=[wb, xT], w=[ps])
                    ev(ps, m, n, c0 + cc, t0)
        else:
            for (t0, n) in subs["tm"]:
                ps = k.next_psum()
                for kc in range(KC):
                    k.op("pe", "matmul", ps.t[0:n, 0:ncw], xT.t[:, kc, t0:t0 + n], wb.t[:, kc, 0:ncw],
                         start=(kc == 0), stop=(kc == KC - 1), r=[wb, xT], w=[ps])
                ev(ps, n, ncw, c0, t0)


def tok_subs(nt):
    return {"fm": [(t, min(512, nt - t)) for t in range(0, nt, 512)],
            "tm": [(t, min(128, nt - t)) for t in range(0, nt, 128)]}


def load_xT(k, xb, src_ap, KC, t0, nt, eng="dve"):
    v = src_ap.rearrange("(kc p) t -> p kc t", p=128)
    k.dma("pool", xb.t[:, 0:KC, 0:nt], v[:, :, t0:t0 + nt], w=[xb])


class Evac:
    def __init__(self, k, ph, n=4):
        self.k = k
        self.ring = Ring(ph, n, [128, 512], F32, "stg")
        self.flip = 0

    def copy(self, st, ps, m, n, func=None):
        k = self.k
        if func is not None:
            k.op("act", "activation", st.t[0:m, 0:n], ps.t[0:m, 0:n], func, r=[ps], w=[st])
        else:
            self.flip ^= 1
            if self.flip:
                k.op("dve", "tensor_copy", st.t[0:m, 0:n], ps.t[0:m, 0:n], r=[ps], w=[st])
            else:
                k.op("act", "activation", st.t[0:m, 0:n], ps.t[0:m, 0:n], AF.Copy, r=[ps], w=[st])

    def fm(self, dst, row0=0, func=None, tbase=0):
        def ev(ps, m, n, c, t0):
            st = self.ring.next()
            self.copy(st, ps, m, n, func)
            self.k.dma("sp", dst[row0 + c:row0 + c + m, tbase + t0:tbase + t0 + n], st.t[0:m, 0:n], r=[st])
        return ev

    def tm(self, dst, col0=0, func=None, tbase=0):
        def ev(ps, n, ncw, c, t0):
            st = self.ring.next()
            self.copy(st, ps, n, ncw, func)
            self.k.dma("sp", dst[tbase + t0:tbase + t0 + n, col0 + c:col0 + c + ncw], st.t[0:n, 0:ncw], r=[st])
        return ev


class Prog:
    def __init__(self, dbg=()):
        self.dbg = set(dbg)
        nc = bass.Bass("TRN2", target_bir_lowering=False)
        self.nc = nc
        self.k = K(nc)
        self.inp = {}
        self.scr = {}
        self.old_moe = False

    def din(self, name, shape):
        ap = self.nc.dram_tensor(name, list(shape), F32, kind="ExternalInput").ap()
        self.inp[name] = ap
        return ap

    def dscr(self, name, shape):
        kind = "ExternalOutput" if name in self.dbg else "Internal"
        ap = self.nc.dram_tensor(name, list(shape), F32, kind=kind).ap()
        self.scr[name] = ap
        return ap

    def dump(self, name, buf, ap, shape):
        if ("dump_" + name) not in self.dbg:
            return
        o = self.nc.dram_tensor("dump_" + name, list(shape), F32, kind="ExternalOutput").ap()
        self.k.dma("sp", o, ap, r=[buf])

    def dout(self, name, shape):
        return self.nc.dram_tensor(name, list(shape), F32, kind="ExternalOutput").ap()


def phase_ada(P, l):
    k = P.k
    with k.phase() as ph:
        cs = ph.sb([128, 16, 2], F32R, "cs")
        ctmp = ph.sb([128, 2, 16], F32, "ctmp")
        bias = ph.sb([2, 6 * D], F32, "adab")
        modrow = ph.sb([2, 6 * D], F32, "modrow")
        wring = Ring(ph, 2, [128, 16, 512], F32R, "wada")
        k.dma("sp", ctmp.t[:, :, :], P.inp["cvec"].rearrange("s (kc p) -> p s kc", p=128), w=[ctmp], allow_slow_non_contiguous=True)
        for s in range(2):
            k.op("act", "activation", cs.t[:, :, s], ctmp.t[:, s, :], AF.Silu, r=[ctmp], w=[cs])
        for s in range(2):
            k.dma("sp", bias.t[s:s + 1, :], P.inp["b_ada"][l:l + 1, :], w=[bias])

        def ev(ps, n, ncw, c, t0):
            k.op("dve", "tensor_tensor", modrow.t[0:2, c:c + ncw], ps.t[0:2, 0:ncw], bias.t[0:2, c:c + ncw], ALU.add,
                 r=[ps, bias], w=[modrow])

        blocks = [(c, 512, "tm", ev) for c in range(0, 6 * D, 512)]
        gemm(k, cs, 16, {"tm": [(0, 2)], "fm": []}, P.inp["w_ada"][l], blocks, wring)
        k.dma("sp", P.scr["MOD"][:, :], modrow.t[:, :], r=[modrow])


def bcast_rows(k, dst, src_row_ap, n=128):
    k.dma("sp", dst.t[0:n, :], src_row_ap.partition_broadcast(n) if hasattr(src_row_ap, "partition_broadcast") else src_row_ap, w=[dst])


def phase_norm(P, l, which, src, gain_row_ap, dstT, dst_tok=None, t_lo=0, t_hi=T):
    k = P.k
    MOD = P.scr["MOD"]
    with k.phase() as ph:
        ident = ph.sb([128, 128], F32, "ident")
        make_ident(k, ident)
        gsc = [ph.sb([128, D], F32, "gsc") for _ in range(2)]
        shb = [ph.sb([128, D], F32, "shb") for _ in range(2)]
        gb = ph.sb([128, D], F32, "gb")
        o_sh = (0 if which == 0 else 3) * D
        o_sc = o_sh + D
        k.dma("sp", gb.t[:, :], gain_row_ap.partition_broadcast(128), w=[gb])
        for s in range(2):
            k.dma("sp", shb[s].t[:, :], MOD[s, o_sh:o_sh + D].partition_broadcast(128), w=[shb[s]])
            k.dma("sp", gsc[s].t[:, :], MOD[s, o_sc:o_sc + D].partition_broadcast(128), w=[gsc[s]])
            k.op("dve", "scalar_tensor_tensor", gsc[s].t[:, :], gsc[s].t[:, :], 1.0, gb.t[:, :], ALU.add, ALU.mult,
                 r=[gsc[s], gb], w=[gsc[s]])
        hring = Ring(ph, 3, [128, D], F32, "h")
        vring = Ring(ph, 2, [128, D], F32, "v")
        junk = ph.sb([128, D], F32, "junk")
        stat = Ring(ph, 4, [128, 4], F32, "stat")
        tring = Ring(ph, 2, [128, 16, 512], F32, "uT")
        tb = None
        tiles = list(range(t_lo, t_hi, 128))
        hq = []

        def ldh(ti):
            hb_ = hring.next()
            k.dma("sp", hb_.t[:, :], src[tiles[ti]:tiles[ti] + 128, :], w=[hb_])
            hq.append(hb_)

        ldh(0)
        for ti, t0 in enumerate(tiles):
            s = 1 if t0 < NCTX else 0
            if ti + 1 < len(tiles):
                ldh(ti + 1)
            hb = hq.pop(0)
            st = stat.next()
            k.op("dve", "scalar_tensor_tensor", junk.t[:, :], hb.t[:, :], 1.0, hb.t[:, :], ALU.mult, ALU.mult, accum_out=st.t[:, 0:1],
                 r=[hb], w=[junk, st])
            k.op("act", "activation", st.t[:, 1:2], st.t[:, 0:1], AF.Ln, scale=1.0 / D, bias=EPS, r=[st], w=[st])
            k.op("act", "activation", st.t[:, 3:4], st.t[:, 1:2], AF.Exp, scale=-0.5, r=[st], w=[st])
            vb = vring.next()
            k.op("dve", "scalar_tensor_tensor", vb.t[:, :], hb.t[:, :], st.t[:, 3:4], gsc[s].t[:, :], ALU.mult, ALU.mult,
                 r=[hb, st, gsc[s]], w=[vb])
            k.op("pool", "tensor_tensor", vb.t[:, :], vb.t[:, :], shb[s].t[:, :], ALU.add, r=[vb, shb[s]], w=[vb])
            if dst_tok is not None:
                k.dma("sp", dst_tok[t0:t0 + 128, :], vb.t[:, :], r=[vb])
            j = ti % 4
            if j == 0:
                tb = tring.next()
            for g4 in range(4):
                ps = k.next_psum()
                for q in range(4):
                    kc = g4 * 4 + q
                    k.op("pe", "transpose", ps.t[:, q * 128:(q + 1) * 128], vb.t[:, kc * 128:(kc + 1) * 128], ident.t[:, :],
                         r=[vb, ident], w=[ps])
                eng = "act" if g4 % 2 == 0 else "dve"
                outv = tb.t[:, g4 * 4:(g4 + 1) * 4, j * 128:(j + 1) * 128]
                inv = ps.t[:, :].rearrange("p (q t) -> p q t", q=4)
                if eng == "act":
                    k.op("act", "activation", outv, inv, AF.Copy, r=[ps], w=[tb])
                else:
                    k.op("dve", "tensor_copy", outv, inv, r=[ps], w=[tb])
            if j == 3 or ti == len(tiles) - 1:
                tb0 = tiles[ti - j]
                nt = (j + 1) * 128
                k.dma("sp", dstT.rearrange("(kc p) t -> p kc t", p=128)[:, :, tb0:tb0 + nt], tb.t[:, :, 0:nt], r=[tb])


def make_ident(k, ident):
    k.op("pool", "memset", ident.t[:, :], 0.0, w=[ident])
    k.op("pool", "affine_select", ident.t[:, :], ident.t[:, :], pattern=[[-1, 128]], compare_op=ALU.not_equal, fill=1.0,
         base=0, channel_multiplier=1, r=[ident], w=[ident])


def phase_win(P, l):
    k = P.k
    S = P.scr
    with k.phase() as ph:
        evs = Evac(k, ph, 6)
        wring = Ring(ph, 2, [128, 16, 512], F32R, "win")
        xb = ph.sb([128, 16, 1024], F32R, "xT")
        W = P.inp["w_in"][l]
        for t0 in range(0, T, 1024):
            nt = min(1024, T - t0)
            load_xT(k, xb, S["UT"], 16, t0, nt)
            blocks = []
            for c in range(O_AX, O_AG, 512):
                blocks.append((c, 512, "fm", evs.fm(S["AXT"], -O_AX, None, t0)))
            for c in range(O_AG, O_Z, 512):
                blocks.append((c, 512, "fm", evs.fm(S["AGT"], -O_AG, AF.Gelu, t0)))
            for c in range(O_Z, O_XBC, 512):
                blocks.append((c, 512, "tm", evs.tm(S["Z"], -O_Z, AF.Silu, t0)))
            for c in range(O_XBC, O_DT, 512):
                blocks.append((c, 512, "fm", evs.fm(S["XBCT"], -O_XBC, None, t0)))
            blocks.append((O_DT, 32, "tm", evs.tm(S["DTR"], -O_DT, None, t0)))
            for c in range(O_Q, O_K, 512):
                blocks.append((c, 512, "fm", evs.fm(S["QT"], -O_Q, None, t0)))
            for c in range(O_K, O_V, 512):
                blocks.append((c, 512, "fm", evs.fm(S["KT"], -O_K, None, t0)))
            for c in range(O_V, O_G, 512):
                blocks.append((c, 512, "tm", evs.tm(S["V"], -O_V, None, t0)))
            for c in range(O_G, PROJ, 512):
                blocks.append((c, 512, "fm", evs.fm(S["GT"], -O_G, AF.Sigmoid, t0)))
            gemm(k, xb, 16, tok_subs(nt), W, blocks, wring)


SCRATCH = {
    "MOD": [2, 6 * D], "H": [T, D], "UT": [D, T], "VTOK": [T, D],
    "AXT": [1024, T], "AGT": [1024, T], "XBCT": [2048, T], "QT": [1024, T], "KT": [1024, T], "GT": [6144, T],
    "Z": [T, 1024], "DTR": [T, 32], "V": [T, 1024],
    "YAT": [1024, T], "YBT": [1024, T], "YCT": [1024, T], "YMT": [D, T],
    "XS": [T, 1024], "BTK": [T, 512], "BFT": [512, T], "CFT": [512, T], "YBR": [T, 1024],
    "POSM": [16, T], "PA": [T, 32], "YE": [16, 544, D],
}

NL = DEPTH


def weight_shapes():
    return {
        "w_ada": [NL, D, 6 * D], "b_ada": [NL, 6 * D], "norm_mix": [NL, D], "norm_ffn": [NL, D],
        "w_in": [NL, D, PROJ],
        "lru_conv_w": [NL, 4, 1024], "lru_conv_b": [NL, 1024], "lru_w_r": [NL, 2, 16, 64, 64], "lru_b_r": [NL, 2, 1024],
        "lru_w_i": [NL, 2, 16, 64, 64], "lru_b_i": [NL, 2, 1024], "lru_lambda": [NL, 2, 1024],
        "ssd_conv_w": [NL, 4, 2048], "ssd_conv_b": [NL, 2048], "ssd_a_log": [NL, 2, 16], "ssd_dt_bias": [NL, 2, 16],
        "ssd_d": [NL, 16], "ssd_norm": [NL, 1024],
        "w_branch_lru": [NL, 1024, D], "w_branch_ssd": [NL, 1024, D], "w_branch_na": [NL, 1024, D], "w_out": [NL, D, D],
        "w_router": [NL, D, 16], "w1": [NL, 16, D, EFF], "w3": [NL, 16, D, EFF], "w2": [NL, 16, EFF, D],
        "norm_final": [1, D],
    }


def declare(P, weights):
    P.din("hin", [T, D])
    P.din("cvec", [2, D])
    P.din("ropec", [128, T])
    P.din("ropes", [128, T])
    P.din("negmask", [64, 64])
    if "rpbx" in weights:
        P.din("rpbx", [NL, 16, 64, 15, 64])
    ws = weight_shapes()
    for n in weights:
        if n != "rpbx":
            P.din(n, ws[n])
    for n, s in SCRATCH.items():
        P.dscr(n, s)


def small_T(k, dst, src_ap, pattern, **kw):
    k.dma("sp", dst, src_ap.rearrange(pattern, **kw), allow_slow_non_contiguous=True)


def phase_lru(P, l):
    k = P.k
    S = P.scr
    I = P.inp
    SEGS = [(0, NCTX), (NCTX, T)]
    with k.phase() as ph:
        wg = ph.sb([128, 32, 128], F32R, "wg")
        prm = ph.sb([128, 8, 16], F32, "prm")
        lamt = ph.sb([128, 2, 8], F32, "lam")
        brt = ph.sb([128, 2, 8], F32, "br")
        bit = ph.sb([128, 2, 8], F32, "bi")
        c8 = ph.sb([128, 2, 8], F32, "c8")
        zt = ph.sb([128, 32, 128], F32, "zt")
        k.op("pool", "memset", zt.t[:, :, :], 0.0, w=[zt])
        k.op("dve", "tensor_copy", wg.t[:, :, :], zt.t[:, :, :], r=[zt], w=[wg])
        for c in range(8):
            for d in range(2):
                for g, nm in enumerate(("lru_w_r", "lru_w_i")):
                    idx = (c * 2 + d) * 2 + g
                    for hb in range(2):
                        k.dma("pool", wg.t[hb * 64:(hb + 1) * 64, idx, hb * 64:(hb + 1) * 64], I[nm][l, d, 2 * c + hb, :, :], w=[wg])
        for tap in range(4):
            k.dma("sp", prm.t[:, :, tap], I["lru_conv_w"][l, tap, :].rearrange("(c p) -> p c", p=128), w=[prm], allow_slow_non_contiguous=True)
        k.dma("sp", prm.t[:, :, 4], I["lru_conv_b"][l, :].rearrange("(c p) -> p c", p=128), w=[prm], allow_slow_non_contiguous=True)
        for tl, nm in ((lamt, "lru_lambda"), (brt, "lru_b_r"), (bit, "lru_b_i")):
            for d in range(2):
                k.dma("sp", tl.t[:, d, :], I[nm][l, d, :].rearrange("(c p) -> p c", p=128), w=[tl], allow_slow_non_contiguous=True)
        k.op("act", "activation", c8.t[:, :, :], lamt.t[:, :, :], AF.Exp, scale=-1.0, r=[lamt], w=[c8])
        k.op("act", "activation", c8.t[:, :, :], c8.t[:, :, :], AF.Ln, bias=1.0, r=[c8], w=[c8])
        k.op("dve", "tensor_scalar", c8.t[:, :, :], c8.t[:, :, :], -8.0, None, ALU.mult, r=[c8], w=[c8])

        ax = ph.sb([128, T], F32, "ax")
        xc = ph.sb([128, T], F32R, "xc")
        ag = ph.sb([128, T], F32, "ag")
        at = ph.sb([128, T], F32, "a")
        ut = ph.sb([128, T], F32, "u")
        sq = ph.sb([128, T], F32, "sq")
        hd = [ph.sb([128, T], F32, "hd%d" % d) for d in range(2)]
        for c in range(8):
            rows = slice(c * 128, (c + 1) * 128)
            k.dma("sp", ax.t[:, :], S["AXT"][rows, :], w=[ax])
            k.dma("sp", ag.t[:, :], S["AGT"][rows, :], w=[ag])
            k.op("dve", "tensor_scalar", xc.t[:, :], ax.t[:, :], prm.t[:, c, 2:3], prm.t[:, c, 4:5], ALU.mult, ALU.add,
                 r=[ax, prm], w=[xc])
            for (s0, s1) in SEGS:
                for tap, off in ((0, -2), (1, -1), (3, 1)):
                    if off < 0:
                        o = xc.t[:, s0 - off:s1]
                        i0 = ax.t[:, s0:s1 + off]
                    else:
                        o = xc.t[:, s0:s1 - off]
                        i0 = ax.t[:, s0 + off:s1]
                    k.op("dve", "scalar_tensor_tensor", o, i0, prm.t[:, c, tap:tap + 1], F_(o), ALU.mult, ALU.add,
                         r=[ax, prm, xc], w=[xc])
            for d in range(2):
                for g, (dst, bt) in enumerate(((at, brt), (ut, bit))):
                    idx = (c * 2 + d) * 2 + g
                    for t0 in range(0, T, 512):
                        n = min(512, T - t0)
                        ps = k.next_psum()
                        k.op("pe", "matmul", ps.t[:, 0:n], wg.t[:, idx, :], xc.t[:, t0:t0 + n], start=True, stop=True,
                             r=[wg, xc], w=[ps])
                        k.op("act", "activation", dst.t[:, t0:t0 + n], ps.t[:, 0:n], AF.Sigmoid, bias=bt.t[:, d, c:c + 1],
                             r=[ps, bt], w=[dst])
                k.op("act", "activation", at.t[:, :], at.t[:, :], AF.Exp, scale=c8.t[:, d, c:c + 1], r=[at, c8], w=[at])
                k.op("pool", "tensor_tensor", ut.t[:, :], ut.t[:, :], F_(xc.t[:, :]), ALU.mult, r=[ut, xc], w=[ut])
                k.op("dve", "tensor_tensor", sq.t[:, :], at.t[:, :], at.t[:, :], ALU.mult, r=[at], w=[sq])
                k.op("act", "activation", sq.t[:, :], sq.t[:, :], AF.Sqrt, scale=-1.0, bias=1.0, r=[sq], w=[sq])
                k.op("dve", "tensor_tensor", ut.t[:, :], ut.t[:, :], sq.t[:, :], ALU.mult, r=[ut, sq], w=[ut])
                h = hd[d]
                if d == 0:
                    k.op("dve", "tensor_tensor_scan", h.t[:, :], at.t[:, :], ut.t[:, :], 0.0, ALU.mult, ALU.add,
                         r=[at, ut], w=[h])
                else:
                    k.op("dve", "tensor_tensor_scan", h.t[:, 0:NCTX][:, ::-1], at.t[:, 0:NCTX][:, ::-1], ut.t[:, 0:NCTX][:, ::-1],
                         0.0, ALU.mult, ALU.add, r=[at, ut], w=[h])
                    k.op("dve", "tensor_tensor_scan", h.t[:, NCTX:T][:, ::-1], at.t[:, NCTX:T][:, ::-1], ut.t[:, NCTX:T][:, ::-1],
                         h.t[:, 0:1], ALU.mult, ALU.add, r=[at, ut, h], w=[h])
            k.op("pool", "tensor_tensor", hd[0].t[:, :], hd[0].t[:, :], hd[1].t[:, :], ALU.add, r=[hd[0], hd[1]], w=[hd[0]])
            k.op("dve", "tensor_tensor", hd[0].t[:, :], hd[0].t[:, :], ag.t[:, :], ALU.mult, r=[hd[0], ag], w=[hd[0]])
            k.dma("sp", S["YAT"][rows, :], hd[0].t[:, :], r=[hd[0]])


def make_consts(k, ph):
    c = {}
    c["ident"] = ph.sb([128, 128], F32, "ident")
    make_ident(k, c["ident"])
    for nm, pat, cm in (("trif", [[1, 128]], -1), ("trib", [[-1, 128]], 1)):
        t = ph.sb([128, 128], F32, nm)
        k.op("pool", "memset", t.t[:, :], 1.0, w=[t])
        k.op("pool", "affine_select", t.t[:, :], t.t[:, :], pattern=pat, compare_op=ALU.is_ge, fill=0.0, base=0,
             channel_multiplier=cm, r=[t], w=[t])
        c[nm] = t
    ones = ph.sb([128, 128], F32, "ones")
    k.op("pool", "memset", ones.t[:, :], 1.0, w=[ones])
    c["ones"] = ones
    sw = ph.sb([128, 128], F32, "swapf")
    k.op("pool", "memset", sw.t[:, :], 0.0, w=[sw])
    for base in (-64, 64):
        k.op("pool", "affine_select", sw.t[:, :], sw.t[:, :], pattern=[[-1, 128]], compare_op=ALU.not_equal, fill=1.0,
             base=base, channel_multiplier=1, r=[sw], w=[sw])
    swr = ph.sb([128, 128], F32R, "swap")
    k.op("dve", "tensor_copy", swr.t[:, :], sw.t[:, :], r=[sw], w=[swr])
    c["swap"] = swr
    return c


def dwconv(k, xc, ax, prm, c, segs):
    k.op("dve", "tensor_scalar", xc.t[:, :], ax.t[:, :], prm.t[:, c, 2:3], prm.t[:, c, 4:5], ALU.mult, ALU.add,
         r=[ax, prm], w=[xc])
    for (s0, s1) in segs:
        for tap, off in ((0, -2), (1, -1), (3, 1)):
            if off < 0:
                o = xc.t[:, s0 - off:s1]
                i0 = ax.t[:, s0:s1 + off]
            else:
                o = xc.t[:, s0:s1 - off]
                i0 = ax.t[:, s0 + off:s1]
            k.op("dve", "scalar_tensor_tensor", o, i0, prm.t[:, c, tap:tap + 1], F_(o), ALU.mult, ALU.add,
                 r=[ax, prm, xc], w=[xc])


def to_token_major(k, ident, src_buf, src_view, dst, col0, stg_ring, nt=T):
    dv = dst.rearrange("(tt p) c -> p tt c", p=128)
    ntile = nt // 128
    for tt0 in range(0, ntile, 4):
        nq = min(4, ntile - tt0)
        ps = k.next_psum()
        for q in range(nq):
            t0 = (tt0 + q) * 128
            k.op("pe", "transpose", ps.t[:, q * 128:(q + 1) * 128], src_view[:, t0:t0 + 128], ident.t[:, :], r=[ident, src_buf], w=[ps])
        st = stg_ring.next()
        k.op("act", "activation", st.t[:, 0:nq * 128], ps.t[:, 0:nq * 128], AF.Copy, r=[ps], w=[st])
        k.dma("sp", dv[:, tt0:tt0 + nq, col0:col0 + 128], st.t[:, 0:nq * 128].rearrange("p (q c) -> p q c", q=nq), r=[st])


def phase_ssd_prep(P, l):
    k = P.k
    S = P.scr
    I = P.inp
    SEGS = [(0, NCTX), (NCTX, T)]
    with k.phase() as ph:
        C = make_consts(k, ph)
        prm = ph.sb([128, 16, 8], F32, "prm")
        for tap in range(4):
            k.dma("sp", prm.t[:, :, tap], I["ssd_conv_w"][l, tap, :].rearrange("(c p) -> p c", p=128), w=[prm], allow_slow_non_contiguous=True)
        k.dma("sp", prm.t[:, :, 4], I["ssd_conv_b"][l, :].rearrange("(c p) -> p c", p=128), w=[prm], allow_slow_non_contiguous=True)
        cosf = ph.sb([128, T], F32, "cosf")
        sinf = ph.sb([128, T], F32, "sinf")
        k.dma("sp", cosf.t[:, :], I["ropec"][:, :], w=[cosf])
        k.dma("sp", sinf.t[:, :], I["ropes"][:, :], w=[sinf])
        axr = Ring(ph, 2, [128, T], F32, "ax")
        xcr = Ring(ph, 2, [128, T], F32R, "xc")
        rot = ph.sb([128, T], F32, "rot")
        tmp = ph.sb([128, T], F32, "tmp")
        stg = Ring(ph, 3, [128, 512], F32, "stg")
        for c in range(16):
            ax = axr.next()
            xc = xcr.next()
            k.dma("sp", ax.t[:, :], S["XBCT"][c * 128:(c + 1) * 128, :], w=[ax])
            dwconv(k, xc, ax, prm, c, SEGS)
            k.op("act", "activation", xc.t[:, :], F_(xc.t[:, :]), AF.Silu, r=[xc], w=[xc])
            if c < 8:
                to_token_major(k, C["ident"], xc, F_(xc.t[:, :]), S["XS"], c * 128, stg)
                k_ = None
            else:
                k.op("pool", "tensor_tensor", rot.t[:, :], F_(xc.t[:, :]), cosf.t[:, :], ALU.mult, r=[xc, cosf], w=[rot])
                for t0 in range(0, T, 512):
                    n = min(512, T - t0)
                    ps = k.next_psum()
                    k.op("pe", "matmul", ps.t[:, 0:n], C["swap"].t[:, :], xc.t[:, t0:t0 + n], start=True, stop=True,
                         r=[C["swap"], xc], w=[ps])
                    k.op("dve", "tensor_tensor", tmp.t[:, t0:t0 + n], ps.t[:, 0:n], sinf.t[:, t0:t0 + n], ALU.mult,
                         r=[ps, sinf], w=[tmp])
                k.op("dve", "tensor_tensor", rot.t[:, :], rot.t[:, :], tmp.t[:, :], ALU.add, r=[rot, tmp], w=[rot])
                if c < 12:
                    g = c - 8
                    k.dma("sp", S["BFT"][g * 128:(g + 1) * 128, :], rot.t[:, :], r=[rot])
                    to_token_major(k, C["ident"], rot, rot.t[:, :], S["BTK"], g * 128, stg)
                else:
                    g = c - 12
                    k.dma("sp", S["CFT"][g * 128:(g + 1) * 128, :], rot.t[:, :], r=[rot])


def phase_ssd(P, l):
    k = P.k
    S = P.scr
    I = P.inp
    NCH = T // 128
    with k.phase() as ph:
        C = make_consts(k, ph)
        dtb = ph.sb([128, 32], F32, "dtb")
        alog = ph.sb([128, 32], F32, "alog")
        dsk = ph.sb([128, 16], F32, "dsk")
        gn = ph.sb([128, 1024], F32, "gn")
        k.dma("sp", dtb.t[:, :], I["ssd_dt_bias"][l].rearrange("d h -> (d h)").partition_broadcast(128), w=[dtb])
        k.dma("sp", alog.t[:, :], I["ssd_a_log"][l].rearrange("d h -> (d h)").partition_broadcast(128), w=[alog])
        k.dma("sp", dsk.t[:, :], I["ssd_d"][l, :].partition_broadcast(128), w=[dsk])
        k.dma("sp", gn.t[:, :], I["ssd_norm"][l, :].partition_broadcast(128), w=[gn])
        k.op("act", "activation", alog.t[:, :], alog.t[:, :], AF.Exp, r=[alog], w=[alog])
        k.op("dve", "tensor_scalar", alog.t[:, :], alog.t[:, :], -1.0, None, ALU.mult, r=[alog], w=[alog])
        Hf = ph.sb([128, 1024], F32, "Hf")
        Hr = ph.sb([128, 1024], F32R, "Hr")
        xsr = Ring(ph, 2, [128, 1024], F32, "xs")
        xdtr = Ring(ph, 2, [128, 1024], F32R, "xdt")
        xddr = Ring(ph, 2, [128, 1024], F32R, "xdd")
        btkr = Ring(ph, 2, [128, 512], F32R, "btk")
        bftr = Ring(ph, 2, [128, 4, 128], F32R, "bft")
        cftr = Ring(ph, 2, [128, 4, 128], F32R, "cft")
        Ltr = Ring(ph, 2, [128, 16, 128], F32, "Lt")
        MTr = Ring(ph, 2, [128, 16, 128], F32R, "MT")
        ychr = Ring(ph, 2, [128, 1024], F32, "ych")
        tmpr = Ring(ph, 2, [128, 1024], F32, "ytmp")
        zr = Ring(ph, 2, [128, 1024], F32, "z")
        ypr = Ring(ph, 2, [128, 1024], F32, "yprev")
        junk = ph.sb([128, 1024], F32, "junk")
        stat = Ring(ph, 4, [128, 4], F32, "stat")
        stg = Ring(ph, 3, [128, 512], F32, "stg")
        bft_v = S["BFT"].rearrange("(g n) t -> n g t", n=128)
        cft_v = S["CFT"].rearrange("(g n) t -> n g t", n=128)
        ybt_v = S["YBT"].rearrange("(c p) t -> p c t", p=128)

        def b16(ap, n):
            return ap.unsqueeze(2).broadcast_to([128, 16, n])

        def v3(ap):
            return ap.rearrange("p (h q) -> p h q", h=16)

        SM = ph.sb([128, NCH, 9, 16], F32, "SM")
        cbmr = Ring(ph, 2, [128, 4, 128], F32, "cbm")
        dtr_v = S["DTR"].rearrange("(c p) h -> p c h", p=128)

        def cb16(ap):
            return ap.unsqueeze(1).broadcast_to([128, NCH, 16])

        for d in range(2):
            tri = C["trif"] if d == 0 else C["trib"]
            order = list(range(NCH)) if d == 0 else [1, 0] + list(range(NCH - 1, 1, -1))
            k.dma("sp", SM.t[:, :, 0, :], dtr_v[:, :, d * 16:(d + 1) * 16], w=[SM])
            k.op("dve", "tensor_tensor", SM.t[:, :, 0, :], SM.t[:, :, 0, :], cb16(dtb.t[:, d * 16:(d + 1) * 16]), ALU.add, r=[SM, dtb], w=[SM])
            k.op("act", "activation", SM.t[:, :, 0, :], SM.t[:, :, 0, :], AF.Exp, r=[SM], w=[SM])
            k.op("act", "activation", SM.t[:, :, 0, :], SM.t[:, :, 0, :], AF.Ln, bias=1.0, r=[SM], w=[SM])
            k.op("dve", "tensor_tensor", SM.t[:, :, 1, :], SM.t[:, :, 0, :], cb16(alog.t[:, d * 16:(d + 1) * 16]), ALU.mult, r=[SM, alog], w=[SM])
            HC = NCH // 2
            for (mat, slot) in ((tri, 2), (C["ones"], 3)):
                for half in range(2):
                    ps = k.next_psum()
                    k.op("pe", "matmul", ps.t[:, 0:HC * 16].rearrange("p (c h) -> p c h", h=16), mat.t[:, :], SM.t[:, half * HC:(half + 1) * HC, 1, :],
                         start=True, stop=True, r=[mat, SM], w=[ps])
                    k.op("dve", "tensor_copy", SM.t[:, half * HC:(half + 1) * HC, slot, :], ps.t[:, 0:HC * 16].rearrange("p (c h) -> p c h", h=16),
                         r=[ps], w=[SM])
            k.op("dve", "tensor_tensor", SM.t[:, :, 7, :], SM.t[:, :, 3, :], SM.t[:, :, 2, :], ALU.subtract, r=[SM], w=[SM])
            k.op("act", "activation", SM.t[:, :, 4, :], SM.t[:, :, 2, :], AF.Exp, r=[SM], w=[SM])
            k.op("act", "activation", SM.t[:, :, 5, :], SM.t[:, :, 7, :], AF.Exp, r=[SM], w=[SM])
            k.op("act", "activation", SM.t[:, :, 6, :], SM.t[:, :, 3, :], AF.Exp, r=[SM], w=[SM])
            k.op("dve", "tensor_tensor", SM.t[:, :, 8, :], SM.t[:, :, 0, :], SM.t[:, :, 5, :], ALU.mult, r=[SM], w=[SM])
            k.op("pool", "memset", Hf.t[:, :], 0.0, w=[Hf])
            k.op("dve", "tensor_copy", Hr.t[:, :], Hf.t[:, :], r=[Hf], w=[Hr])
            for ci in order:
                t0 = ci * 128
                xs = xsr.next(); xdt = xdtr.next(); xdd = xddr.next(); btk = btkr.next(); bft = bftr.next(); cft = cftr.next()
                Lt = Ltr.next(); MT = MTr.next()

                def sm(i, ci=ci):
                    return SM.t[:, ci, i, :]

                k.dma("sp", xs.t[:, :], S["XS"][t0:t0 + 128, :], w=[xs])
                k.dma("pool", btk.t[:, :], S["BTK"][t0:t0 + 128, :], w=[btk])
                k.dma("pool", bft.t[:, :, :], bft_v[:, :, t0:t0 + 128], w=[bft])
                k.dma("pool", cft.t[:, :, :], cft_v[:, :, t0:t0 + 128], w=[cft])
                k.op("dve", "tensor_tensor", v3(xdt.t[:, :]), v3(xs.t[:, :]), b16(sm(0), 64), ALU.mult, r=[xs, SM], w=[xdt])
                k.op("dve", "tensor_tensor", v3(xdd.t[:, :]), v3(xs.t[:, :]), b16(sm(8), 64), ALU.mult, r=[xs, SM], w=[xdd])
                ab_, aap = k.psum_group(4)
                for h in range(16):
                    k.op("pe", "matmul", aap[:, h * 128:(h + 1) * 128], SM.t[:, ci, 1, h:h + 1].broadcast_to([128, 128]), tri.t[:, :],
                         start=True, stop=True, r=[SM, tri], w=[ab_[h // 4]])
                cb = k.next_psum()
                for g in range(4):
                    k.op("pe", "matmul", cb.t[:, g * 128:(g + 1) * 128], bft.t[:, g, :], cft.t[:, g, :], start=True, stop=True,
                         r=[bft, cft], w=[cb])
                cbm = cbmr.next()
                k.op("dve", "tensor_tensor", cbm.t[:, :, :], cb.t[:, :].rearrange("p (g i) -> p g i", g=4),
                     tri.t[:, :].unsqueeze(1).broadcast_to([128, 4, 128]), ALU.mult, r=[cb, tri], w=[cbm])
                for h in range(16):
                    k.op("dve", "tensor_scalar", Lt.t[:, h, :], aap[:, h * 128:(h + 1) * 128], SM.t[:, ci, 2, h:h + 1], 0.0,
                         ALU.subtract, ALU.min, r=[ab_[h // 4], SM], w=[Lt])
                k.op("act", "activation", Lt.t[:, :, :], Lt.t[:, :, :], AF.Exp, r=[Lt], w=[Lt])
                k.op("dve", "tensor_tensor", MT.t[:, :, :].rearrange("p (g r) i -> p g r i", g=4), Lt.t[:, :, :].rearrange("p (g r) i -> p g r i", g=4),
                     cbm.t[:, :, :].unsqueeze(2).broadcast_to([128, 4, 4, 128]), ALU.mult, r=[Lt, cbm], w=[MT])
                sb_, sap = k.psum_group(2)
                for g in range(4):
                    k.op("pe", "matmul", sap[:, g * 256:(g + 1) * 256], btk.t[:, g * 128:(g + 1) * 128], xdd.t[:, g * 256:(g + 1) * 256],
                         start=True, stop=True, r=[btk, xdd], w=[sb_[g // 2]])
                yb_, yap = k.psum_group(2)
                for h in range(16):
                    k.op("pe", "matmul", yap[:, h * 64:(h + 1) * 64], MT.t[:, h, :], xdt.t[:, h * 64:(h + 1) * 64], start=True, stop=True,
                         r=[MT, xdt], w=[yb_[h // 8]])
                ob_, oap = k.psum_group(2)
                for g in range(4):
                    k.op("pe", "matmul", oap[:, g * 256:(g + 1) * 256], cft.t[:, g, :], Hr.t[:, g * 256:(g + 1) * 256], start=True, stop=True,
                         r=[cft, Hr], w=[ob_[g // 2]])
                k.op("dve", "tensor_tensor", v3(Hf.t[:, :]), v3(Hf.t[:, :]), b16(sm(6), 64), ALU.mult, r=[Hf, SM], w=[Hf])
                k.op("dve", "tensor_tensor", Hf.t[:, :], Hf.t[:, :], sap, ALU.add, r=[Hf, sb_[0], sb_[1]], w=[Hf])
                k.op("act", "activation", Hr.t[:, :], Hf.t[:, :], AF.Copy, r=[Hf], w=[Hr])
                ych = ychr.next(); ytmp = tmpr.next()
                k.op("dve", "tensor_tensor", v3(ytmp.t[:, :]), v3(oap), b16(sm(4), 64), ALU.mult, r=[ob_[0], ob_[1], SM], w=[ytmp])
                k.op("dve", "tensor_tensor", ych.t[:, :], yap, ytmp.t[:, :], ALU.add, r=[yb_[0], yb_[1], ytmp], w=[ych])
                if d == 0:
                    k.dma("sp", S["YBR"][t0:t0 + 128, :], ych.t[:, :], r=[ych])
                else:
                    yp = ypr.next(); z = zr.next()
                    k.dma("sp", yp.t[:, :], S["YBR"][t0:t0 + 128, :], w=[yp])
                    k.dma("sp", z.t[:, :], S["Z"][t0:t0 + 128, :], w=[z])
                    k.op("pool", "tensor_tensor", ych.t[:, :], ych.t[:, :], yp.t[:, :], ALU.add, r=[ych, yp], w=[ych])
                    k.op("pool", "tensor_tensor", v3(ytmp.t[:, :]), v3(xs.t[:, :]), b16(dsk.t[:, :], 64), ALU.mult, r=[xs, dsk, ytmp], w=[ytmp])
                    k.op("pool", "tensor_tensor", ych.t[:, :], ych.t[:, :], ytmp.t[:, :], ALU.add, r=[ych, ytmp], w=[ych])
                    k.op("dve", "tensor_tensor", ych.t[:, :], ych.t[:, :], z.t[:, :], ALU.mult, r=[ych, z], w=[ych])
                    st = stat.next()
                    k.op("dve", "scalar_tensor_tensor", junk.t[:, :], ych.t[:, :], 1.0, ych.t[:, :], ALU.mult, ALU.mult, accum_out=st.t[:, 0:1],
                         r=[ych], w=[junk, st])
                    k.op("act", "activation", st.t[:, 1:2], st.t[:, 0:1], AF.Ln, scale=1.0 / 1024, bias=EPS, r=[st], w=[st])
                    k.op("act", "activation", st.t[:, 3:4], st.t[:, 1:2], AF.Exp, scale=-0.5, r=[st], w=[st])
                    k.op("dve", "scalar_tensor_tensor", ych.t[:, :], ych.t[:, :], st.t[:, 3:4], gn.t[:, :], ALU.mult, ALU.mult,
                         r=[ych, st, gn], w=[ych])
                    for c0 in range(0, 8, 4):
                        ps = k.next_psum()
                        for q in range(4):
                            k.op("pe", "transpose", ps.t[:, q * 128:(q + 1) * 128], ych.t[:, (c0 + q) * 128:(c0 + q + 1) * 128], C["ident"].t[:, :],
                                 r=[ych, C["ident"]], w=[ps])
                        sg = stg.next()
                        k.op("act", "activation", sg.t[:, :], ps.t[:, :], AF.Copy, r=[ps], w=[sg])
                        k.dma("sp", ybt_v[:, c0:c0 + 4, t0:t0 + 128], sg.t[:, :].rearrange("p (q t) -> p q t", q=4), r=[sg])
            k.barrier()


def rope_tables():
    pos = np.arange(NLAT)
    row = (pos // 64).astype(np.float32)
    col = (pos % 64).astype(np.float32)
    freqs = (np.float32(10000.0) ** (-np.arange(32, dtype=np.float32) / np.float32(32))).astype(np.float32)
    ang = np.concatenate([row[:, None] * freqs, col[:, None] * freqs], axis=-1).astype(np.float32)
    cos = np.cos(ang).astype(np.float32).T
    sin = np.sin(ang).astype(np.float32).T
    c = np.ones((128, T), np.float32)
    s = np.zeros((128, T), np.float32)
    c[0:64, NCTX:] = cos
    c[64:128, NCTX:] = cos
    s[0:64, NCTX:] = -sin
    s[64:128, NCTX:] = sin
    return c, s


def na_tables(rpb):
    Ld = rpb.shape[0]
    kc = np.arange(64)[:, None]
    j = np.arange(64)[None, :]
    ci = np.clip(kc - j + 15, 0, 30)
    s = np.arange(15)
    x = rpb[:, :, 14 - s, :]
    x = x[:, :, :, ci]
    rpbx = np.ascontiguousarray(np.transpose(x, (0, 1, 3, 2, 4))).astype(np.float32)
    cs = np.clip(np.arange(64) - 8, 0, 48)[None, :]
    inside = (kc >= cs) & (kc < cs + 16)
    negmask = np.where(inside, 0.0, -30000.0).astype(np.float32)
    return rpbx, negmask


def phase_na(P, l):
    k = P.k
    S = P.scr
    I = P.inp

    def rs(r):
        return min(max(r - 4, 0), 56)

    with k.phase() as ph:
        C = make_consts(k, ph)
        negm = ph.sb([64, 64], F32, "negm")
        k.dma("sp", negm.t[:, :], I["negmask"][:, :], w=[negm])
        qTr = Ring(ph, 1, [64, T], F32R, "qT")
        kTr = Ring(ph, 1, [64, T], F32R, "kT")
        var = Ring(ph, 2, [64, 64, 66], F32R, "vaug")
        vcr = Ring(ph, 2, [128, 2, 66], F32R, "vc")
        tblr = Ring(ph, 2, [64, 15, 64], F32, "tbl")
        yctr = Ring(ph, 2, [64, T], F32, "yct")
        PTr = Ring(ph, 2, [64, 15, 512], F32R, "PT")
        PTcr = Ring(ph, 2, [128, 2, 512], F32R, "PTc")
        tmpr = Ring(ph, 3, [128, 512], F32, "natmp")
        yblk = Ring(ph, 2, [128, 8, 64], F32, "yblk")
        recr = Ring(ph, 2, [128, 8], F32, "rec")
        zer = ph.sb([64, 64], F32, "zer")
        k.op("pool", "memset", zer.t[:, :], 0.0, w=[zer])
        heads = {}

        def load_head(h):
            qT = qTr.next(); kT = kTr.next(); va = var.next(); vc = vcr.next(); tbl = tblr.next(); yct = yctr.next()
            k.dma("pool", qT.t[:, :], S["QT"][h * 64:(h + 1) * 64, :], w=[qT])
            k.dma("pool", kT.t[:, :], S["KT"][h * 64:(h + 1) * 64, :], w=[kT])
            k.dma("pool", va.t[:, :, 0:64], S["V"][NCTX:T, h * 64:(h + 1) * 64].rearrange("(r kc) d -> kc r d", kc=64), w=[va])
            k.dma("pool", vc.t[:, :, 0:64], S["V"][0:NCTX, h * 64:(h + 1) * 64].rearrange("(c p) d -> p c d", p=128), w=[vc])
            k.op("dve", "tensor_copy", va.t[:, :, 64:66], C["ones"].t[0:64, 0:128].rearrange("p (a b) -> p a b", b=2), r=[C["ones"]], w=[va])
            k.op("dve", "tensor_copy", vc.t[:, :, 64:66], C["ones"].t[:, 0:4].rearrange("p (a b) -> p a b", b=2), r=[C["ones"]], w=[vc])
            k.dma("sp", tbl.t[:, :, :], I["rpbx"][l, h], w=[tbl])
            k.op("pool", "tensor_tensor", tbl.t[:, :, :], tbl.t[:, :, :], negm.t[:, :].unsqueeze(1).broadcast_to([64, 15, 64]), ALU.add,
                 r=[tbl, negm], w=[tbl])
            heads[h] = dict(qT=qT, kT=kT, va=va, vc=vc, tbl=tbl, yct=yct)

        def stageS(item):
            h, b = item
            if b == "c":
                load_head(h)
            H = heads[h]
            qT, kT, tbl = H["qT"], H["kT"], H["tbl"]
            c = dict(item=item, H=H)
            PTc = c["PTc"] = PTcr.next()
            if b == "c":
                for cc in range(2):
                    ps = k.next_psum()
                    k.op("pe", "matmul", ps.t[:, 0:NCTX], kT.t[:, cc * 128:(cc + 1) * 128], qT.t[:, 0:NCTX], start=True, stop=True, r=[kT, qT], w=[ps])
                    k.op("act", "activation", PTc.t[:, cc, 0:NCTX], ps.t[:, 0:NCTX], AF.Exp, scale=0.125, r=[ps], w=[PTc])
                return c
            r0 = b * 8
            rows = list(range(r0, r0 + 8))
            Rlo = rs(r0); Rhi = rs(r0 + 7) + 7
            PT = c["PT"] = PTr.next()
            q0 = NCTX + r0 * 64
            for cc in range(2):
                ps = k.next_psum()
                k.op("pe", "matmul", ps.t[:, :], kT.t[:, cc * 128:(cc + 1) * 128], qT.t[:, q0:q0 + 512], start=True, stop=True, r=[kT, qT], w=[ps])
                k.op("act", "activation", PTc.t[:, cc, :], ps.t[:, :], AF.Exp, scale=0.125, r=[ps], w=[PTc])
            for R in range(Rlo, Rhi + 1):
                vr = [r for r in rows if rs(r) <= R <= rs(r) + 7]
                ra, rb = vr[0], vr[-1]
                nr = rb - ra + 1
                n = nr * 64
                s0 = 7 - R + ra
                ps = k.next_psum()
                k.op("pe", "matmul", ps.t[0:64, 0:n], kT.t[:, NCTX + R * 64:NCTX + (R + 1) * 64], qT.t[:, NCTX + ra * 64:NCTX + (rb + 1) * 64],
                     start=True, stop=True, r=[kT, qT], w=[ps])
                tm = tmpr.next()
                k.op("dve", "scalar_tensor_tensor", tm.t[0:64, 0:n], ps.t[0:64, 0:n], 0.125, tbl.t[:, s0:s0 + nr, :].rearrange("p a b -> p (a b)"),
                     ALU.mult, ALU.add, r=[ps, tbl], w=[tm])
                o0 = (ra - r0) * 64
                k.op("act", "activation", PT.t[:, R - Rlo, o0:o0 + n], tm.t[0:64, 0:n], AF.Exp, r=[tm], w=[PT])
            for pi in range(4):
                pa_, pb2 = r0 + 2 * pi, r0 + 2 * pi + 1
                for R in range(rs(pa_), rs(pb2) + 8):
                    for row in (pa_, pb2):
                        if not (rs(row) <= R <= rs(row) + 7):
                            ri = row - r0
                            k.op("pool", "tensor_copy", PT.t[:, R - Rlo, ri * 64:(ri + 1) * 64], zer.t[0:64, 0:64], r=[zer], w=[PT])
            return c

        def stageV(c):
            h, b = c["item"]
            H = c["H"]
            va, vc, yct = H["va"], H["vc"], H["yct"]
            PTc = c["PTc"]
            if b == "c":
                ps2 = k.next_psum()
                for qc in range(2):
                    for cc in range(2):
                        k.op("pe", "matmul", ps2.t[:, qc * 66:(qc + 1) * 66], PTc.t[:, cc, qc * 128:(qc + 1) * 128], vc.t[:, cc, :],
                             start=(cc == 0), stop=(cc == 1), r=[PTc, vc], w=[ps2])
                rec = recr.next(); yb = yblk.next()
                p2v = ps2.t[:, 0:132].rearrange("p (a b) -> p a b", b=66)
                k.op("dve", "reciprocal", rec.t[:, 0:2], p2v[:, :, 64], r=[ps2], w=[rec])
                k.op("dve", "tensor_tensor", yb.t[:, 0:2, :], p2v[:, :, 0:64], rec.t[:, 0:2].unsqueeze(2).broadcast_to([128, 2, 64]), ALU.mult,
                     r=[ps2, rec], w=[yb])
                pst = k.next_psum()
                for qc in range(2):
                    k.op("pe", "transpose", pst.t[0:64, qc * 128:(qc + 1) * 128], yb.t[:, qc, :], C["ident"].t[:, :], r=[yb, C["ident"]], w=[pst])
                k.op("act", "activation", yct.t[:, 0:NCTX], pst.t[0:64, 0:NCTX], AF.Copy, r=[pst], w=[yct])
                return
            PT = c["PT"]
            r0 = b * 8
            Rlo = rs(r0)
            q0 = NCTX + r0 * 64
            pp = k.next_psum()
            for pi in range(4):
                ra = r0 + 2 * pi
                rb = ra + 1
                off = pi * 66
                U = list(range(rs(ra), rs(rb) + 8))
                for bi, R in enumerate(U):
                    k.op("pe", "matmul", pp.t[:, off:off + 66], PT.t[:, R - Rlo, pi * 128:(pi + 1) * 128], va.t[:, R, :],
                         start=(bi == 0), stop=False, r=[PT, va], w=[pp])
                for cc in range(2):
                    k.op("pe", "matmul", pp.t[:, off:off + 66], PTc.t[:, cc, pi * 128:(pi + 1) * 128], vc.t[:, cc, :],
                         start=False, stop=(cc == 1), r=[PTc, vc], w=[pp])
            rec = recr.next(); yb = yblk.next()
            pv = pp.t[:, 0:264].rearrange("p (a b) -> p a b", b=66)
            k.op("dve", "reciprocal", rec.t[:, 0:4], pv[:, :, 64], r=[pp], w=[rec])
            k.op("dve", "tensor_tensor", yb.t[:, 0:4, :], pv[:, :, 0:64], rec.t[:, 0:4].unsqueeze(2).broadcast_to([128, 4, 64]), ALU.mult,
                 r=[pp, rec], w=[yb])
            pst = k.next_psum()
            for pi in range(4):
                k.op("pe", "transpose", pst.t[0:64, pi * 128:(pi + 1) * 128], yb.t[:, pi, :], C["ident"].t[:, :], r=[yb, C["ident"]], w=[pst])
            k.op("act", "activation", yct.t[:, q0:q0 + 512], pst.t[0:64, :], AF.Copy, r=[pst], w=[yct])
            if b == 7:
                k.dma("sp", S["YCT"][h * 64:(h + 1) * 64, :], yct.t[:, :], r=[yct])

        items = [(h, b) for h in range(16) for b in (["c"] + list(range(8)))]
        pend = stageS(items[0])
        for i in range(len(items)):
            nxt = stageS(items[i + 1]) if i + 1 < len(items) else None
            stageV(pend)
            pend = nxt


def phase_merge(P, l):
    k = P.k
    S = P.scr
    I = P.inp
    srcs = [(S["YAT"], I["w_branch_lru"][l]), (S["YBT"], I["w_branch_ssd"][l]), (S["YCT"], I["w_branch_na"][l])]
    with k.phase() as ph:
        wring = Ring(ph, 2, [128, 8, 512], F32R, "wbr")
        xb = ph.sb([128, 8, 1024], F32R, "xT")
        acc = ph.sb([128, 16, 1024], F32, "acc")
        gring = Ring(ph, 8, [128, 512], F32, "g")
        tring = Ring(ph, 3, [128, 512], F32, "tmp")
        for tb0 in range(0, T, 1024):
            nt = min(1024, T - tb0)
            subs = tok_subs(nt)
            for br in range(3):
                load_xT(k, xb, srcs[br][0], 8, tb0, nt)
                seq = [(c0 + cc, t0, n) for c0 in range(0, D, 512) for cc in range(0, 512, 128) for (t0, n) in subs["fm"]]
                gq = []

                def ldg(i, br=br, seq=seq, gq=gq, tb0=tb0):
                    if i < len(seq):
                        c, t0, n = seq[i]
                        g = gring.next()
                        k.dma("sp", g.t[:, 0:n], S["GT"][br * D + c:br * D + c + 128, tb0 + t0:tb0 + t0 + n], w=[g])
                        gq.append(g)

                for i0 in range(5):
                    ldg(i0)
                cnt = [0]

                def ev(ps, m, n, c, t0, br=br, gq=gq, cnt=cnt, ldg=ldg, tb0=tb0):
                    ldg(cnt[0] + 5)
                    cnt[0] += 1
                    g = gq.pop(0)
                    av = acc.t[:, c // 128, t0:t0 + n]
                    if br == 0:
                        k.op("dve", "tensor_tensor", av, ps.t[:, 0:n], g.t[:, 0:n], ALU.mult, r=[ps, g], w=[acc])
                    else:
                        tm = tring.next()
                        k.op("dve", "tensor_tensor", tm.t[:, 0:n], ps.t[:, 0:n], g.t[:, 0:n], ALU.mult, r=[ps, g], w=[tm])
                        k.op("pool", "tensor_tensor", av, av, tm.t[:, 0:n], ALU.add, r=[acc, tm], w=[acc])
                        if br == 2:
                            k.dma("sp", S["YMT"][c:c + 128, tb0 + t0:tb0 + t0 + n], av, r=[acc])

                blocks = [(c0, 512, "fm", ev) for c0 in range(0, D, 512)]
                gemm(k, xb, 8, subs, srcs[br][1], blocks, wring)


def load_mod_bcast(k, P, tiles, chunk):
    for s in range(2):
        k.dma("sp", tiles[s].t[:, :], P.scr["MOD"][s, chunk * D:(chunk + 1) * D].partition_broadcast(128), w=[tiles[s]])


def phase_wout(P, l, hsrc):
    k = P.k
    S = P.scr
    I = P.inp
    with k.phase() as ph:
        wring = Ring(ph, 2, [128, 16, 512], F32R, "wout")
        xb = ph.sb([128, 16, 1024], F32R, "xT")
        g1 = [ph.sb([128, D], F32, "g1") for _ in range(2)]
        load_mod_bcast(k, P, g1, 2)
        hring = Ring(ph, 4, [128, 512], F32, "h")
        tring = Ring(ph, 3, [128, 512], F32, "tmp")
        for tb0 in range(0, T, 1024):
            nt = min(1024, T - tb0)
            subs = tok_subs(nt)
            load_xT(k, xb, S["YMT"], 16, tb0, nt)
            seq = [(c0, t0, n) for c0 in range(0, D, 512) for (t0, n) in subs["tm"]]
            hq = []

            def ldh(i, seq=seq, hq=hq, tb0=tb0):
                if i < len(seq):
                    c, t0, n = seq[i]
                    h = hring.next()
                    k.dma("sp", h.t[0:n, :], hsrc[tb0 + t0:tb0 + t0 + n, c:c + 512], w=[h])
                    hq.append(h)

            ldh(0); ldh(1)
            cnt = [0]

            def ev(ps, n, ncw, c, t0, hq=hq, cnt=cnt, ldh=ldh, tb0=tb0):
                ldh(cnt[0] + 2)
                cnt[0] += 1
                h = hq.pop(0)
                s = 1 if (tb0 + t0) < NCTX else 0
                tm = tring.next()
                k.op("dve", "tensor_tensor", tm.t[0:n, :], ps.t[0:n, 0:ncw], g1[s].t[0:n, c:c + ncw], ALU.mult, r=[ps, g1[s]], w=[tm])
                k.op("pool", "tensor_tensor", tm.t[0:n, :], tm.t[0:n, :], h.t[0:n, :], ALU.add, r=[tm, h], w=[tm])
                k.dma("sp", S["H"][tb0 + t0:tb0 + t0 + n, c:c + ncw], tm.t[0:n, :], r=[tm])

            blocks = [(c0, 512, "tm", ev) for c0 in range(0, D, 512)]
            gemm(k, xb, 16, subs, I["w_out"][l], blocks, wring)


CAP_L = 512
CAP_C = 32
NSLOT = CAP_L + CAP_C


def phase_router(P, l):
    k = P.k
    S = P.scr
    I = P.inp
    with k.phase() as ph:
        C = make_consts(k, ph)
        wr = ph.sb([128, 16, 16], F32, "wr")
        k.dma("sp", wr.t[:, :, :], I["w_router"][l].rearrange("(kc p) e -> p kc e", p=128), w=[wr])
        affT = ph.sb([16, T], F32, "affT")
        work = ph.sb([16, T], F32, "work")
        mask = ph.sb([16, T], F32, "mask")
        ones16 = ph.sb([16, T], F32, "ones16")
        scan = ph.sb([16, T], F32, "scan")
        k.op("pool", "memset", ones16.t[:, :], 1.0, w=[ones16])
        xr = Ring(ph, 2, [128, 16, 512], F32, "vT")
        sm = Ring(ph, 4, [128, 64], F32, "rsm")
        uv = S["UT"].rearrange("(kc p) t -> p kc t", p=128)
        for tb0 in range(0, T, 512):
            nt = min(512, T - tb0)
            xb = xr.next()
            k.dma("sp", xb.t[:, :, 0:nt], uv[:, :, tb0:tb0 + nt], w=[xb])
            for t0 in range(0, nt, 128):
                ps = k.next_psum()
                for kc in range(16):
                    k.op("pe", "matmul", ps.t[:, 0:16], xb.t[:, kc, t0:t0 + 128], wr.t[:, kc, :], start=(kc == 0), stop=(kc == 15),
                         r=[xb, wr], w=[ps])
                s = sm.next()
                k.op("dve", "tensor_reduce", s.t[:, 0:1], ps.t[:, 0:16], axis=AXL.X, op=ALU.max, r=[ps], w=[s])
                k.op("dve", "tensor_scalar", s.t[:, 1:2], s.t[:, 0:1], -1.0, None, ALU.mult, r=[s], w=[s])
                k.op("act", "activation", s.t[:, 16:32], ps.t[:, 0:16], AF.Exp, bias=s.t[:, 1:2], accum_out=s.t[:, 2:3], r=[ps, s], w=[s])
                k.op("dve", "reciprocal", s.t[:, 3:4], s.t[:, 2:3], r=[s], w=[s])
                k.op("dve", "tensor_scalar", s.t[:, 32:48], s.t[:, 16:32], s.t[:, 3:4], None, ALU.mult, r=[s], w=[s])
                pt = k.next_psum()
                k.op("pe", "transpose", pt.t[0:16, 0:128], s.t[:, 32:48], C["ident"].t[:, :], r=[s, C["ident"]], w=[pt])
                k.op("act", "activation", affT.t[:, tb0 + t0:tb0 + t0 + 128], pt.t[0:16, 0:128], AF.Copy, r=[pt], w=[affT])
        thr = ph.sb([16, 8], F32, "thr")
        mx = Ring(ph, 2, [16, 8], F32, "mx")
        k.op("dve", "tensor_copy", work.t[:, :], affT.t[:, :], r=[affT], w=[work])
        for (s0, s1, cap, ti) in ((0, NCTX, CAP_C, 0), (NCTX, T, CAP_L, 1)):
            nit = cap // 8
            for it in range(nit):
                m = mx.next()
                k.op("dve", "max", m.t[:, :], work.t[:, s0:s1], r=[work], w=[m])
                if it < nit - 1:
                    k.op("dve", "match_replace", work.t[:, s0:s1], m.t[:, :], work.t[:, s0:s1], -1.0, r=[work, m], w=[work])
                else:
                    k.op("dve", "tensor_copy", thr.t[:, ti:ti + 1], m.t[:, 7:8], r=[m], w=[thr])
            k.op("dve", "tensor_scalar", mask.t[:, s0:s1], affT.t[:, s0:s1], thr.t[:, ti:ti + 1], None, ALU.is_ge, r=[affT, thr], w=[mask])
            k.op("dve", "tensor_tensor_scan", scan.t[:, s0:s1], ones16.t[:, s0:s1], mask.t[:, s0:s1], 0.0, ALU.mult, ALU.add,
                 r=[ones16, mask], w=[scan])
        k.op("dve", "tensor_tensor", scan.t[:, :], scan.t[:, :], mask.t[:, :], ALU.mult, r=[scan, mask], w=[scan])
        k.op("dve", "tensor_scalar", scan.t[:, :], scan.t[:, :], -1.0, None, ALU.add, r=[scan], w=[scan])
        k.op("pool", "tensor_tensor", mask.t[:, :], mask.t[:, :], affT.t[:, :], ALU.mult, r=[mask, affT], w=[mask])
        k.dma("sp", S["POSM"][:, :], scan.t[:, :], r=[scan])
        stg = Ring(ph, 3, [128, 32], F32, "pa")
        for t0 in range(0, T, 128):
            pt = k.next_psum()
            k.op("pe", "transpose", pt.t[:, 0:16], scan.t[:, t0:t0 + 128], C["ident"].t[0:16, 0:16], r=[scan, C["ident"]], w=[pt])
            k.op("pe", "transpose", pt.t[:, 16:32], mask.t[:, t0:t0 + 128], C["ident"].t[0:16, 0:16], r=[mask, C["ident"]], w=[pt])
            st = stg.next()
            k.op("act", "activation", st.t[:, :], pt.t[:, 0:32], AF.Copy, r=[pt], w=[st])
            k.dma("sp", S["PA"][t0:t0 + 128, :], st.t[:, :], r=[st])


def phase_moe1(P, l):
    k = P.k
    S = P.scr
    I = P.inp
    NT = T // 128
    with k.phase() as ph:
        pa = ph.sb([128, NT, 32], F32, "pa")
        k.dma("sp", pa.t[:, :, :], S["PA"].rearrange("(tt p) c -> p tt c", p=128), w=[pa])
        iota = ph.sb([128, 512], F32, "iota")
        k.op("pool", "iota", iota.t[:, :], pattern=[[1, 512]], base=0, channel_multiplier=0, allow_small_or_imprecise_dtypes=True, w=[iota])
        vctx = ph.sb([128, 2, D], F32R, "vctx")
        k.dma("pool", vctx.t[:, :, :], S["VTOK"][0:NCTX, :].rearrange("(tt p) c -> p tt c", p=128), w=[vctx])
        vring = Ring(ph, 3, [128, 1024], F32R, "vtok")
        selr = Ring(ph, 3, [128, 512], F32R, "sel")
        xg = ph.sb([128, 16, NSLOT], F32R, "xg")
        h1s = ph.sb([128, 8, NSLOT], F32, "h1s")
        hid = ph.sb([128, 8, NSLOT], F32R, "hid")
        wring = Ring(ph, 2, [128, 16, 512], F32R, "wexp")
        evs = Evac(k, ph, 4)
        subs = {"fm": [(0, 512), (512, CAP_C)], "tm": [(0, 128), (128, 128), (256, 128), (384, 128), (512, CAP_C)]}
        for e in range(NEXP):
            for half in range(2):
                k.ps_rr = 0
                for tt in range(2, NT):
                    vt = vring.next()
                    k.dma("pool", vt.t[:, :], S["VTOK"][tt * 128:(tt + 1) * 128, half * 1024:(half + 1) * 1024], w=[vt])
                    sel = selr.next()
                    k.op("dve", "tensor_scalar", sel.t[:, :], iota.t[:, :], pa.t[:, tt, e:e + 1], None, ALU.is_equal, r=[iota, pa], w=[sel])
                    for dc in range(8):
                        k.op("pe", "matmul", k.psum[dc].t[:, :], vt.t[:, dc * 128:(dc + 1) * 128], sel.t[:, :], start=(tt == 2), stop=(tt == NT - 1),
                             r=[vt, sel], w=[k.psum[dc]])
                for dc in range(8):
                    if dc % 2 == 0:
                        k.op("act", "activation", xg.t[:, half * 8 + dc, 0:512], k.psum[dc].t[:, :], AF.Copy, r=[k.psum[dc]], w=[xg])
                    else:
                        k.op("dve", "tensor_copy", xg.t[:, half * 8 + dc, 0:512], k.psum[dc].t[:, :], r=[k.psum[dc]], w=[xg])
            selc = [selr.next() for _ in range(2)]
            for tt in range(2):
                k.op("dve", "tensor_scalar", selc[tt].t[:, 0:CAP_C], iota.t[:, 0:CAP_C], pa.t[:, tt, e:e + 1], None, ALU.is_equal,
                     r=[iota, pa], w=[selc[tt]])
            ps = k.next_psum()
            for dc in range(16):
                for tt in range(2):
                    k.op("pe", "matmul", ps.t[:, dc * CAP_C:(dc + 1) * CAP_C], vctx.t[:, tt, dc * 128:(dc + 1) * 128], selc[tt].t[:, 0:CAP_C],
                         start=(tt == 0), stop=(tt == 1), r=[vctx, selc[tt]], w=[ps])
            k.op("dve", "tensor_copy", xg.t[:, :, 512:NSLOT], ps.t[:, :].rearrange("p (a b) -> p a b", b=CAP_C), r=[ps], w=[xg])

            def ev1(ps, m, n, c, t0):
                k.op("act", "activation", h1s.t[:, c // 128, t0:t0 + n], ps.t[:, 0:n], AF.Silu, r=[ps], w=[h1s])

            def ev3(ps, m, n, c, t0):
                k.op("dve", "tensor_tensor", hid.t[:, c // 128, t0:t0 + n], ps.t[:, 0:n], h1s.t[:, c // 128, t0:t0 + n], ALU.mult, r=[ps, h1s], w=[hid])

            gemm(k, xg, 16, subs, I["w1"][l, e], [(c0, 512, "fm", ev1) for c0 in range(0, EFF, 512)], wring)
            gemm(k, xg, 16, subs, I["w3"][l, e], [(c0, 512, "fm", ev3) for c0 in range(0, EFF, 512)], wring)
            gemm(k, hid, 8, subs, I["w2"][l, e], [(c0, 512, "tm", evs.tm(S["YE"][e], 0, None, 0)) for c0 in range(0, D, 512)], wring)


def phase_moe2(P, l):
    k = P.k
    S = P.scr
    NT = T // 128
    with k.phase() as ph:
        pa = ph.sb([128, NT, 32], F32, "pa")
        k.dma("sp", pa.t[:, :, :], S["PA"].rearrange("(tt p) c -> p tt c", p=128), w=[pa])
        posm = ph.sb([16, T], F32, "posm")
        k.dma("sp", posm.t[:, :], S["POSM"][:, :], w=[posm])
        iop = ph.sb([128, 4], F32, "iop")
        k.op("pool", "iota", iop.t[:, :], pattern=[[128, 4]], base=0, channel_multiplier=1, allow_small_or_imprecise_dtypes=True, w=[iop])
        sele = ph.sb([16, 16, 128], F32, "sele")
        k.op("pool", "memset", sele.t[:, :, :], 1.0, w=[sele])
        k.op("pool", "affine_select", sele.t[:, :, :], sele.t[:, :, :], pattern=[[-1, 16], [0, 128]], compare_op=ALU.is_equal, fill=0.0,
             base=0, channel_multiplier=1, r=[sele], w=[sele])
        g2 = [ph.sb([128, D], F32, "g2") for _ in range(2)]
        load_mod_bcast(k, P, g2, 5)
        acc = ph.sb([128, 4, D], F32, "acc")
        yer = Ring(ph, 2, [128, 5, D], F32R, "ye")
        selT = Ring(ph, 2, [128, 4, 512], F32R, "selT")
        hring = Ring(ph, 2, [128, D], F32, "h")
        blocks = [(0, 2, True)] + [(tt, 4, False) for tt in range(2, NT, 4)]
        for (tt0, ntile, is_ctx) in blocks:
            nt = ntile * 128
            tb0 = tt0 * 128
            for e in range(NEXP):
                ye = yer.next()
                if is_ctx:
                    k.dma("pool", ye.t[0:CAP_C, 4, :], S["YE"][e, CAP_L:NSLOT, :], w=[ye])
                else:
                    k.dma("pool", ye.t[:, 0:4, :], S["YE"][e, 0:CAP_L, :].rearrange("(sc p) c -> p sc c", p=128), w=[ye])
                pb = k.next_psum()
                k.op("pe", "matmul", pb.t[:, 0:nt], sele.t[:, e, :], posm.t[:, tb0:tb0 + nt], start=True, stop=True, r=[sele, posm], w=[pb])
                st = selT.next()
                if is_ctx:
                    k.op("dve", "tensor_scalar", st.t[0:CAP_C, 0, 0:nt], pb.t[0:CAP_C, 0:nt], iop.t[0:CAP_C, 0:1], None, ALU.is_equal, r=[pb, iop], w=[st])
                else:
                    for sc in range(4):
                        k.op("dve", "tensor_scalar", st.t[:, sc, 0:nt], pb.t[:, 0:nt], iop.t[:, sc:sc + 1], None, ALU.is_equal, r=[pb, iop], w=[st])
                for ti in range(ntile):
                    ob_, oap = k.psum_group(4)
                    for cb in range(4):
                        if is_ctx:
                            k.op("pe", "matmul", oap[:, cb * 512:(cb + 1) * 512], st.t[0:CAP_C, 0, ti * 128:(ti + 1) * 128], ye.t[0:CAP_C, 4, cb * 512:(cb + 1) * 512],
                                 start=True, stop=True, r=[st, ye], w=[ob_[cb]])
                        else:
                            for sc in range(4):
                                k.op("pe", "matmul", oap[:, cb * 512:(cb + 1) * 512], st.t[:, sc, ti * 128:(ti + 1) * 128], ye.t[:, sc, cb * 512:(cb + 1) * 512],
                                     start=(sc == 0), stop=(sc == 3), r=[st, ye], w=[ob_[cb]])
                    gate = pa.t[:, tt0 + ti, 16 + e:17 + e]
                    if e == 0:
                        k.op("dve", "tensor_scalar", acc.t[:, ti, :], oap, gate, None, ALU.mult, r=ob_ + [pa], w=[acc])
                    else:
                        k.op("dve", "scalar_tensor_tensor", acc.t[:, ti, :], oap, gate, acc.t[:, ti, :], ALU.mult, ALU.add, r=ob_ + [pa, acc], w=[acc])
            for ti in range(ntile):
                t0 = tb0 + ti * 128
                s = 1 if is_ctx else 0
                h = hring.next()
                k.dma("sp", h.t[:, :], S["H"][t0:t0 + 128, :], w=[h])
                k.op("pool", "tensor_tensor", acc.t[:, ti, :], acc.t[:, ti, :], g2[s].t[:, :], ALU.mult, r=[acc, g2[s]], w=[acc])
                k.op("dve", "tensor_tensor", h.t[:, :], h.t[:, :], acc.t[:, ti, :], ALU.add, r=[h, acc], w=[h])
                k.dma("sp", S["H"][t0:t0 + 128, :], h.t[:, :], r=[h])


def phase_final(P, out_ap):
    k = P.k
    S = P.scr
    with k.phase() as ph:
        gb = ph.sb([128, D], F32, "gb")
        k.dma("sp", gb.t[:, :], P.inp["norm_final"][0, :].partition_broadcast(128), w=[gb])
        hring = Ring(ph, 3, [128, D], F32, "h")
        junk = ph.sb([128, D], F32, "junk")
        stat = Ring(ph, 4, [128, 4], F32, "stat")
        for t0 in range(NCTX, T, 128):
            hb = hring.next()
            k.dma("sp", hb.t[:, :], S["H"][t0:t0 + 128, :], w=[hb])
            st = stat.next()
            k.op("dve", "scalar_tensor_tensor", junk.t[:, :], hb.t[:, :], 1.0, hb.t[:, :], ALU.mult, ALU.mult, accum_out=st.t[:, 0:1],
                 r=[hb], w=[junk, st])
            k.op("act", "activation", st.t[:, 1:2], st.t[:, 0:1], AF.Ln, scale=1.0 / D, bias=EPS, r=[st], w=[st])
            k.op("act", "activation", st.t[:, 3:4], st.t[:, 1:2], AF.Exp, scale=-0.5, r=[st], w=[st])
            k.op("dve", "scalar_tensor_tensor", hb.t[:, :], hb.t[:, :], st.t[:, 3:4], gb.t[:, :], ALU.mult, ALU.mult, r=[hb, st, gb], w=[hb])
            k.dma("sp", out_ap[t0 - NCTX:t0 - NCTX + 128, :], hb.t[:, :], r=[hb])


ALL_WEIGHTS = ["w_ada", "b_ada", "norm_mix", "norm_ffn", "w_in", "lru_conv_w", "lru_conv_b", "lru_w_r", "lru_b_r", "lru_w_i", "lru_b_i",
               "lru_lambda", "ssd_conv_w", "ssd_conv_b", "ssd_a_log", "ssd_dt_bias", "ssd_d", "ssd_norm", "rpbx",
               "w_branch_lru", "w_branch_ssd", "w_branch_na", "w_out", "w_router", "w1", "w3", "w2", "norm_final"]


def build_layer(P, l):
    S = P.scr
    hsrc = P.inp["hin"] if l == 0 else S["H"]
    phase_ada(P, l)
    phase_norm(P, l, 0, hsrc, P.inp["norm_mix"][l:l + 1, :], S["UT"])
    phase_win(P, l)
    phase_lru(P, l)
    phase_ssd_prep(P, l)
    phase_ssd(P, l)
    phase_na(P, l)
    phase_merge(P, l)
    phase_wout(P, l, hsrc)
    if "HMID" in P.dbg and l == 0:
        hm = P.nc.dram_tensor("HMID", [T, D], F32, kind="ExternalOutput").ap()
        P.k.dma("sp", hm[:, :], S["H"][:, :])
        P.k.barrier()
    phase_norm(P, l, 1, S["H"], P.inp["norm_ffn"][l:l + 1, :], S["UT"], dst_tok=S["VTOK"])
    phase_router(P, l)
    if P.old_moe:
        phase_moe1(P, l)
        phase_moe2(P, l)
    else:
        phase_moe(P, l)


def host_inputs(inputs, b, nl=None):
    nl = nl or NL
    rc, rs_ = rope_tables()
    rpbx, negmask = na_tables(np.asarray(inputs["na_rpb"][:nl]))
    im = {"hin": np.ascontiguousarray(np.concatenate([inputs["ctx"][b], inputs["x"][b]], 0)),
          "cvec": np.ascontiguousarray(np.stack([inputs["c"][b], inputs["c_ctx"]], 0)),
          "ropec": rc, "ropes": rs_, "negmask": negmask, "rpbx": rpbx}
    for n in ALL_WEIGHTS:
        if n == "rpbx":
            continue
        if n == "norm_final":
            im[n] = np.ascontiguousarray(np.asarray(inputs[n]).reshape(1, D))
        else:
            im[n] = np.ascontiguousarray(np.asarray(inputs[n][:nl]))
    return im


def build_program():
    P = Prog()
    declare(P, ALL_WEIGHTS)
    out = P.dout("out", [NLAT, D])
    for l in range(DEPTH):
        build_layer(P, l)
    phase_final(P, out)
    P.k.barrier()
    return P


def kernel(**inputs):
    inputs = {n: np.asarray(v) for n, v in inputs.items()}
    B = inputs["x"].shape[0]
    P = build_program()
    ims = [host_inputs(inputs, b, DEPTH) for b in range(B)]
    res = run_bass_kernel_spmd(P.nc, ims, core_ids=list(range(B)))
    return np.stack([np.asarray(res.results[b]["out"]) for b in range(B)], 0).astype(np.float32)


I32 = mybir.dt.int32


def idma(k, out, in_, r=(), w=(), out_idx=None, in_idx=None, **kw):
    q = "pool"
    lanes = k.lanes[q]
    i = k.lane_rr[q]
    k.lane_rr[q] = (i + 1) % len(lanes)
    lane = lanes[i]
    key = "d_%s%d" % (q, i)
    k._deps(q, r, w)
    k._wait(q, (key, lane[0], lane[1] * 16))
    ins = k.eng[q].indirect_dma_start(
        out=out, out_offset=(bass.IndirectOffsetOnAxis(ap=out_idx, axis=0) if out_idx is not None else None),
        in_=in_, in_offset=(bass.IndirectOffsetOnAxis(ap=in_idx, axis=0) if in_idx is not None else None), **kw)
    lane[1] += 1
    ins.then_inc(lane[0], 16)
    tok = (key, lane[0], lane[1] * 16)
    k._mark(tok, r, w)
    k.n_ins += 1
    return tok


def phase_moe(P, l):
    k = P.k
    S = P.scr
    I = P.inp
    NT = T // 128
    with k.phase() as ph:
        C = make_consts(k, ph)
        pa = ph.sb([128, NT, 32], F32, "pa")
        k.dma("sp", pa.t[:, :, :], S["PA"].rearrange("(tt p) c -> p tt c", p=128), w=[pa])
        iota = ph.sb([128, 512], F32, "iota")
        k.op("pool", "iota", iota.t[:, :], pattern=[[1, 512]], base=0, channel_multiplier=0, allow_small_or_imprecise_dtypes=True, w=[iota])
        tg = ph.sb([128, NT, 16, 4], F32R, "tg")
        tid = ph.sb([128, NT, 16], F32, "tid")
        k.op("pool", "iota", tid.t[:, :, :], pattern=[[1, NT], [0, 16]], base=0, channel_multiplier=0, allow_small_or_imprecise_dtypes=True, w=[tid])
        k.op("dve", "tensor_copy", tg.t[:, :, :, 0], tid.t[:, :, :], r=[tid], w=[tg])
        tid2 = ph.sb([128, NT, 16], F32, "tid2")
        k.op("pool", "iota", tid2.t[:, :, :], pattern=[[0, NT], [0, 16]], base=0, channel_multiplier=1, allow_small_or_imprecise_dtypes=True, w=[tid2])
        k.op("dve", "tensor_copy", tg.t[:, :, :, 1], tid2.t[:, :, :], r=[tid2], w=[tg])
        k.op("dve", "tensor_copy", tg.t[:, :, :, 2], pa.t[:, :, 16:32], r=[pa], w=[tg])
        k.op("pool", "memset", tid2.t[:, :, :], 0.0, r=[tid2], w=[tid2])
        k.op("dve", "tensor_copy", tg.t[:, :, :, 3], tid2.t[:, :, :], r=[tid2], w=[tg])
        g2 = [ph.sb([128, D], F32, "g2") for _ in range(2)]
        load_mod_bcast(k, P, g2, 5)
        selr = Ring(ph, 2, [128, 512], F32R, "sel")
        idxf = ph.sb([128, 8], F32, "idxf")
        raw4 = ph.sb([128, 5, 4], F32, "raw4")
        idxr = Ring(ph, 2, [128, 8], I32, "idx")
        gater = Ring(ph, 2, [128, 8], F32, "gate")
        xgr = Ring(ph, 3, [128, D], F32, "xgtok")
        yew = ph.sb([128, 5, D], F32, "yew")
        xg = ph.sb([128, 16, NSLOT], F32R, "xg")
        h1s = ph.sb([128, 8, NSLOT], F32, "h1s")
        hid = ph.sb([128, 8, NSLOT], F32R, "hid")
        wring = Ring(ph, 2, [128, 16, 256], F32R, "wexp")
        Hbuf = Buf()
        subs = {"fm": [(0, NSLOT // 2), (NSLOT // 2, NSLOT // 2)], "tm": [(0, 128), (128, 128), (256, 128), (384, 128), (512, CAP_C)]}
        NR = [128, 128, 128, 128, CAP_C]
        state = {}

        def idx_gather(e):
            idx = idxr.next(); gate = gater.next()
            k.ps_rr = 0
            banks = k.psum[0:5]
            for tt in range(NT):
                sel = selr.next()
                if tt < 2:
                    k.op("dve", "tensor_scalar", sel.t[:, 0:CAP_C], iota.t[:, 0:CAP_C], pa.t[:, tt, e:e + 1], None, ALU.is_equal, r=[iota, pa], w=[sel])
                    k.op("pe", "matmul", banks[4].t[0:CAP_C, 0:4], sel.t[:, 0:CAP_C], tg.t[:, tt, e, :], start=(tt == 0), stop=(tt == 1),
                         r=[sel, tg], w=[banks[4]])
                else:
                    k.op("dve", "tensor_scalar", sel.t[:, :], iota.t[:, :], pa.t[:, tt, e:e + 1], None, ALU.is_equal, r=[iota, pa], w=[sel])
                    for sc in range(4):
                        k.op("pe", "matmul", banks[sc].t[:, 0:4], sel.t[:, sc * 128:(sc + 1) * 128], tg.t[:, tt, e, :], start=(tt == 2), stop=(tt == NT - 1),
                             r=[sel, tg], w=[banks[sc]])
            for sc in range(5):
                n = NR[sc]
                k.op("dve", "tensor_copy", raw4.t[0:n, sc, :], banks[sc].t[0:n, 0:4], r=[banks[sc]], w=[raw4])
                k.op("dve", "scalar_tensor_tensor", idxf.t[0:n, sc:sc + 1], raw4.t[0:n, sc, 0:1], 128.0, raw4.t[0:n, sc, 1:2], ALU.mult, ALU.add,
                     r=[raw4], w=[idxf])
                k.op("dve", "tensor_copy", idx.t[0:n, sc:sc + 1], idxf.t[0:n, sc:sc + 1], r=[idxf], w=[idx])
                k.op("dve", "tensor_copy", gate.t[0:n, sc:sc + 1], raw4.t[0:n, sc, 2:3], r=[raw4], w=[gate])
            k.ps_rr = 5
            chunks = []
            for sc in range(3):
                chunks.append(gather_chunk(idx, sc))
            state[e] = (idx, gate, chunks)

        def gather_chunk(idx, sc):
            n = NR[sc]
            xt = xgr.next()
            idma(k, xt.t[0:n, :], S["VTOK"][:, :], r=[idx], w=[xt], in_idx=idx.t[0:n, sc:sc + 1])
            return xt

        def transposes(e, idx, chunks):
            flip = 0
            for sc in range(5):
                xt = chunks[sc]
                if sc < 4:
                    for dc0 in range(0, 16, 4):
                        ps = k.next_psum()
                        for q in range(4):
                            dc = dc0 + q
                            k.op("pe", "transpose", ps.t[:, q * 128:(q + 1) * 128], xt.t[:, dc * 128:(dc + 1) * 128], C["ident"].t[:, :],
                                 r=[xt, C["ident"]], w=[ps])
                        outv = xg.t[:, dc0:dc0 + 4, sc * 128:(sc + 1) * 128]
                        inv = ps.t[:, :].rearrange("p (q t) -> p q t", q=4)
                        flip ^= 1
                        if flip:
                            k.op("act", "activation", outv, inv, AF.Copy, r=[ps], w=[xg])
                        else:
                            k.op("dve", "tensor_copy", outv, inv, r=[ps], w=[xg])
                else:
                    ps = k.next_psum()
                    for dc in range(16):
                        k.op("pe", "transpose", ps.t[:, dc * CAP_C:(dc + 1) * CAP_C], xt.t[0:CAP_C, dc * 128:(dc + 1) * 128], C["ident"].t[0:CAP_C, 0:CAP_C],
                             r=[xt, C["ident"]], w=[ps])
                    k.op("dve", "tensor_copy", xg.t[:, :, CAP_L:NSLOT], ps.t[:, :].rearrange("p (a b) -> p a b", b=CAP_C), r=[ps], w=[xg])
                if sc + 3 < 5:
                    chunks.append(gather_chunk(idx, sc + 3))

        idx_gather(0)
        for e in range(NEXP):
            idx, gate, chunks = state.pop(e)
            transposes(e, idx, chunks)
            if e + 1 < NEXP:
                idx_gather(e + 1)

            def ev1(ps, m, n, c, t0):
                k.op("act", "activation", h1s.t[0:m, c // 128, t0:t0 + n], ps.t[0:m, 0:n], AF.Silu, r=[ps], w=[h1s])

            def ev3(ps, m, n, c, t0):
                k.op("dve", "tensor_tensor", hid.t[0:m, c // 128, t0:t0 + n], ps.t[0:m, 0:n], h1s.t[0:m, c // 128, t0:t0 + n], ALU.mult, r=[ps, h1s], w=[hid])

            def ev2(ps, n, ncw, c, t0, gate=gate):
                sc = t0 // 128
                s = 1 if sc == 4 else 0
                k.op("dve", "scalar_tensor_tensor", yew.t[0:n, sc, c:c + ncw], ps.t[0:n, 0:ncw], gate.t[0:n, sc:sc + 1], g2[s].t[0:n, c:c + ncw],
                     ALU.mult, ALU.mult, r=[ps, gate, g2[s]], w=[yew])

            gemm(k, xg, 16, subs, I["w1"][l, e], [(c0, 256, "fm", ev1) for c0 in range(0, EFF, 256)], wring)
            gemm(k, xg, 16, subs, I["w3"][l, e], [(c0, 256, "fm", ev3) for c0 in range(0, EFF, 256)], wring)
            gemm(k, hid, 8, subs, I["w2"][l, e], [(c0, 256, "tm", ev2) for c0 in range(0, D, 256)], wring)
            for sc in range(5):
                n = NR[sc]
                idma(k, S["H"][:, :], yew.t[0:n, sc, :], r=[yew, idx], w=[Hbuf], out_idx=idx.t[0:n, sc:sc + 1], compute_op=ALU.add)
```

```python
import contextlib
import numpy as np
import concourse.bass as bass
import concourse.mybir as mybir
from concourse.bass_utils import run_bass_kernel_spmd

F32 = mybir.dt.float32
F32R = mybir.dt.float32r
AF = mybir.ActivationFunctionType
ALU = mybir.AluOpType
AXL = mybir.AxisListType

D = 2048
NCTX = 256
NLAT = 4096
T = NCTX + NLAT
DEPTH = 4
EPS = 1e-6
PROJ = 14368
O_AX, O_AG, O_Z, O_XBC, O_DT, O_Q, O_K, O_V, O_G = 0, 1024, 2048, 3072, 5120, 5152, 6176, 7200, 8224
NEXP = 16
EFF = 1024


class Buf:
    __slots__ = ("t", "w", "r")

    def __init__(self, t=None):
        self.t = t
        self.w = {}
        self.r = {}


class K:
    def __init__(self, nc):
        self.nc = nc
        self.es = contextlib.ExitStack()
        self.eng = dict(pe=nc.tensor, act=nc.scalar, dve=nc.vector, pool=nc.gpsimd, sp=nc.sync)
        self.sem = {}
        self.cnt = {}
        for e in ("pe", "act", "dve", "pool"):
            self.sem[e] = self.es.enter_context(nc.semaphore("s_" + e))
            self.cnt[e] = 0
        self.waited = {e: {} for e in self.eng}
        self.lanes = {}
        for q, n in (("sp", 8), ("pool", 8), ("act", 4)):
            self.lanes[q] = [[self.es.enter_context(nc.semaphore("d_%s%d" % (q, i))), 0] for i in range(n)]
        self.lane_rr = {q: 0 for q in self.lanes}
        self.pst = self.es.enter_context(nc.psum_tensor("pst", [128, 4096], F32))
        self.psum = [Buf(self.pst[:, i * 512:(i + 1) * 512]) for i in range(8)]
        self.ps_rr = 0
        self.n_ins = 0
        self.fill_regs = {}

    def _wait(self, eng, tok):
        key, h, v = tok
        if v <= 0 or self.waited[eng].get(key, 0) >= v:
            return
        self.eng[eng].wait_ge(h, v)
        self.waited[eng][key] = v

    def _deps(self, eng, r, w):
        for b in r:
            for tok in b.w.values():
                if eng == "pe" and tok[0] == "s_pe":
                    continue
                self._wait(eng, tok)
        for b in w:
            for tok in b.w.values():
                if eng == "pe" and tok[0] == "s_pe":
                    continue
                self._wait(eng, tok)
            for tok in b.r.values():
                if eng == "pe" and tok[0] == "s_pe":
                    continue
                self._wait(eng, tok)

    def _mark(self, tok, r, w):
        for b in r:
            b.r[tok[0]] = tok
        for b in w:
            b.w[tok[0]] = tok

    def op(self, eng, name, *args, r=(), w=(), **kw):
        if name == "affine_select" and isinstance(kw.get("fill"), float):
            key = (eng, kw["fill"])
            if key not in self.fill_regs:
                self.fill_regs[key] = self.eng[eng].to_reg(kw["fill"])
            kw["fill"] = self.fill_regs[key]
        self._deps(eng, r, w)
        ins = getattr(self.eng[eng], name)(*args, **kw)
        self.cnt[eng] += 1
        ins.then_inc(self.sem[eng], 1)
        tok = ("s_" + eng, self.sem[eng], self.cnt[eng])
        self._mark(tok, r, w)
        self.n_ins += 1
        return tok

    def dma(self, q, out, in_, r=(), w=(), **kw):
        lanes = self.lanes[q]
        i = self.lane_rr[q]
        self.lane_rr[q] = (i + 1) % len(lanes)
        lane = lanes[i]
        key = "d_%s%d" % (q, i)
        self._deps(q, r, w)
        self._wait(q, (key, lane[0], lane[1] * 16))
        ins = self.eng[q].dma_start(out=out, in_=in_, **kw)
        lane[1] += 1
        ins.then_inc(lane[0], 16)
        tok = (key, lane[0], lane[1] * 16)
        self._mark(tok, r, w)
        self.n_ins += 1
        return tok

    def barrier(self, engines=None):
        toks = [("s_" + e, self.sem[e], self.cnt[e]) for e in self.sem]
        for q, lanes in self.lanes.items():
            for i, lane in enumerate(lanes):
                toks.append(("d_%s%d" % (q, i), lane[0], lane[1] * 16))
        for e in (engines or self.eng):
            for tok in toks:
                if tok[0] == "s_" + e:
                    continue
                self._wait(e, tok)

    def next_psum(self):
        b = self.psum[self.ps_rr]
        self.ps_rr = (self.ps_rr + 1) % 8
        return b

    def psum_group(self, nb):
        if self.ps_rr + nb > 8:
            self.ps_rr = 0
        i = self.ps_rr
        self.ps_rr = (i + nb) % 8
        return self.psum[i:i + nb], self.pst[:, i * 512:(i + nb) * 512]

    @contextlib.contextmanager
    def phase(self):
        ph = Phase(self)
        try:
            yield ph
        finally:
            self.barrier()
            ph.es.close()


_UID = [0]


class Phase:
    def __init__(self, k):
        self.k = k
        self.es = contextlib.ExitStack()
        self.n = 0

    def sb(self, shape, dtype=F32, name=None):
        _UID[0] += 1
        t = self.es.enter_context(self.k.nc.sbuf_tensor("%s_%d" % (name or "t", _UID[0]), list(shape), dtype))
        return Buf(t)


class Ring:
    def __init__(self, ph, n, shape, dtype=F32, name="ring"):
        self.bufs = [ph.sb(shape, dtype, name) for _ in range(n)]
        self.i = 0

    def next(self):
        b = self.bufs[self.i]
        self.i = (self.i + 1) % len(self.bufs)
        return b


def R_(ap):
    return ap.bitcast(F32R)


def F_(ap):
    return ap.bitcast(F32)


def gemm(k, xT, KC, subs, W_ap, blocks, wring, round_eng="pool"):
    Wv = W_ap.rearrange("(kc p) c -> p kc c", p=128)
    loaded = {}

    def load(i):
        c0, ncw, mode, ev = blocks[i]
        wb = wring.next()
        k.dma("pool", wb.t[:, 0:KC, 0:ncw], Wv[:, :, c0:c0 + ncw], w=[wb])
        loaded[i] = wb

    load(0)
    for i, (c0, ncw, mode, ev) in enumerate(blocks):
        if i + 1 < len(blocks):
            load(i + 1)
        wb = loaded.pop(i)
        if mode == "fm":
            for cc in range(0, ncw, 128):
                m = min(128, ncw - cc)
                for (t0, n) in subs["fm"]:
                    ps = k.next_psum()
                    for kc in range(KC):
                        k.op("pe", "matmul", ps.t[0:m, 0:n], wb.t[:, kc, cc:cc + m], xT.t[:, kc, t0:t0 + n],
                             start=(kc == 0), stop=(kc == KC - 1), r=[wb, xT], w=[ps])
                    ev(ps, m, n, c0 + cc, t0)
        else:
            for (t0, n) in subs["tm"]:
                ps = k.next_psum()
                for kc in range(KC):
                    k.op("pe", "matmul", ps.t[0:n, 0:ncw], xT.t[:, kc, t0:t0 + n], wb.t[:, kc, 0:ncw],
                         start=(kc == 0), stop=(kc == KC - 1), r=[wb, xT], w=[ps])
                ev(ps, n, ncw, c0, t0)


def tok_subs(nt):
    return {"fm": [(t, min(512, nt - t)) for t in range(0, nt, 512)],
            "tm": [(t, min(128, nt - t)) for t in range(0, nt, 128)]}


def load_xT(k, xb, src_ap, KC, t0, nt, eng="dve"):
    v = src_ap.rearrange("(kc p) t -> p kc t", p=128)
    k.dma("pool", xb.t[:, 0:KC, 0:nt], v[:, :, t0:t0 + nt], w=[xb])


class Evac:
    def __init__(self, k, ph, n=4):
        self.k = k
        self.ring = Ring(ph, n, [128, 512], F32, "stg")
        self.flip = 0

    def copy(self, st, ps, m, n, func=None):
        k = self.k
        if func is not None:
            k.op("act", "activation", st.t[0:m, 0:n], ps.t[0:m, 0:n], func, r=[ps], w=[st])
        else:
            self.flip ^= 1
            if self.flip:
                k.op("dve", "tensor_copy", st.t[0:m, 0:n], ps.t[0:m, 0:n], r=[ps], w=[st])
            else:
                k.op("act", "activation", st.t[0:m, 0:n], ps.t[0:m, 0:n], AF.Copy, r=[ps], w=[st])

    def fm(self, dst, row0=0, func=None, tbase=0):
        def ev(ps, m, n, c, t0):
            st = self.ring.next()
            self.copy(st, ps, m, n, func)
            self.k.dma("sp", dst[row0 + c:row0 + c + m, tbase + t0:tbase + t0 + n], st.t[0:m, 0:n], r=[st])
        return ev

    def tm(self, dst, col0=0, func=None, tbase=0):
        def ev(ps, n, ncw, c, t0):
            st = self.ring.next()
            self.copy(st, ps, n, ncw, func)
            self.k.dma("sp", dst[tbase + t0:tbase + t0 + n, col0 + c:col0 + c + ncw], st.t[0:n, 0:ncw], r=[st])
        return ev


class Prog:
    def __init__(self, dbg=()):
        self.dbg = set(dbg)
        nc = bass.Bass("TRN2", target_bir_lowering=False)
        self.nc = nc
        self.k = K(nc)
        self.inp = {}
        self.scr = {}
        self.old_moe = False

    def din(self, name, shape):
        ap = self.nc.dram_tensor(name, list(shape), F32, kind="ExternalInput").ap()
        self.inp[name] = ap
        return ap

    def dscr(self, name, shape):
        kind = "ExternalOutput" if name in self.dbg else "Internal"
        ap = self.nc.dram_tensor(name, list(shape), F32, kind=kind).ap()
        self.scr[name] = ap
        return ap

    def dump(self, name, buf, ap, shape):
        if ("dump_" + name) not in self.dbg:
            return
        o = self.nc.dram_tensor("dump_" + name, list(shape), F32, kind="ExternalOutput").ap()
        self.k.dma("sp", o, ap, r=[buf])

    def dout(self, name, shape):
        return self.nc.dram_tensor(name, list(shape), F32, kind="ExternalOutput").ap()


def phase_ada(P, l):
    k = P.k
    with k.phase() as ph:
        cs = ph.sb([128, 16, 2], F32R, "cs")
        ctmp = ph.sb([128, 2, 16], F32, "ctmp")
        bias = ph.sb([2, 6 * D], F32, "adab")
        modrow = ph.sb([2, 6 * D], F32, "modrow")
        wring = Ring(ph, 2, [128, 16, 512], F32R, "wada")
        k.dma("sp", ctmp.t[:, :, :], P.inp["cvec"].rearrange("s (kc p) -> p s kc", p=128), w=[ctmp], allow_slow_non_contiguous=True)
        for s in range(2):
            k.op("act", "activation", cs.t[:, :, s], ctmp.t[:, s, :], AF.Silu, r=[ctmp], w=[cs])
        for s in range(2):
            k.dma("sp", bias.t[s:s + 1, :], P.inp["b_ada"][l:l + 1, :], w=[bias])

        def ev(ps, n, ncw, c, t0):
            k.op("dve", "tensor_tensor", modrow.t[0:2, c:c + ncw], ps.t[0:2, 0:ncw], bias.t[0:2, c:c + ncw], ALU.add,
                 r=[ps, bias], w=[modrow])

        blocks = [(c, 512, "tm", ev) for c in range(0, 6 * D, 512)]
        gemm(k, cs, 16, {"tm": [(0, 2)], "fm": []}, P.inp["w_ada"][l], blocks, wring)
        k.dma("sp", P.scr["MOD"][:, :], modrow.t[:, :], r=[modrow])


def bcast_rows(k, dst, src_row_ap, n=128):
    k.dma("sp", dst.t[0:n, :], src_row_ap.partition_broadcast(n) if hasattr(src_row_ap, "partition_broadcast") else src_row_ap, w=[dst])


def phase_norm(P, l, which, src, gain_row_ap, dstT, dst_tok=None, t_lo=0, t_hi=T):
    k = P.k
    MOD = P.scr["MOD"]
    with k.phase() as ph:
        ident = ph.sb([128, 128], F32, "ident")
        make_ident(k, ident)
        gsc = [ph.sb([128, D], F32, "gsc") for _ in range(2)]
        shb = [ph.sb([128, D], F32, "shb") for _ in range(2)]
        gb = ph.sb([128, D], F32, "gb")
        o_sh = (0 if which == 0 else 3) * D
        o_sc = o_sh + D
        k.dma("sp", gb.t[:, :], gain_row_ap.partition_broadcast(128), w=[gb])
        for s in range(2):
            k.dma("sp", shb[s].t[:, :], MOD[s, o_sh:o_sh + D].partition_broadcast(128), w=[shb[s]])
            k.dma("sp", gsc[s].t[:, :], MOD[s, o_sc:o_sc + D].partition_broadcast(128), w=[gsc[s]])
            k.op("dve", "scalar_tensor_tensor", gsc[s].t[:, :], gsc[s].t[:, :], 1.0, gb.t[:, :], ALU.add, ALU.mult,
                 r=[gsc[s], gb], w=[gsc[s]])
        hring = Ring(ph, 3, [128, D], F32, "h")
        vring = Ring(ph, 2, [128, D], F32, "v")
        junk = ph.sb([128, D], F32, "junk")
        stat = Ring(ph, 4, [128, 4], F32, "stat")
        tring = Ring(ph, 2, [128, 16, 512], F32, "uT")
        tb = None
        tiles = list(range(t_lo, t_hi, 128))
        hq = []

        def ldh(ti):
            hb_ = hring.next()
            k.dma("sp", hb_.t[:, :], src[tiles[ti]:tiles[ti] + 128, :], w=[hb_])
            hq.append(hb_)

        ldh(0)
        for ti, t0 in enumerate(tiles):
            s = 1 if t0 < NCTX else 0
            if ti + 1 < len(tiles):
                ldh(ti + 1)
            hb = hq.pop(0)
            st = stat.next()
            k.op("dve", "scalar_tensor_tensor", junk.t[:, :], hb.t[:, :], 1.0, hb.t[:, :], ALU.mult, ALU.mult, accum_out=st.t[:, 0:1],
                 r=[hb], w=[junk, st])
            k.op("act", "activation", st.t[:, 1:2], st.t[:, 0:1], AF.Ln, scale=1.0 / D, bias=EPS, r=[st], w=[st])
            k.op("act", "activation", st.t[:, 3:4], st.t[:, 1:2], AF.Exp, scale=-0.5, r=[st], w=[st])
            vb = vring.next()
            k.op("dve", "scalar_tensor_tensor", vb.t[:, :], hb.t[:, :], st.t[:, 3:4], gsc[s].t[:, :], ALU.mult, ALU.mult,
                 r=[hb, st, gsc[s]], w=[vb])
            k.op("pool", "tensor_tensor", vb.t[:, 0:768], vb.t[:, 0:768], shb[s].t[:, 0:768], ALU.add, r=[vb, shb[s]], w=[vb])
            k.op("dve", "tensor_tensor", vb.t[:, 768:D], vb.t[:, 768:D], shb[s].t[:, 768:D], ALU.add, r=[vb, shb[s]], w=[vb])
            if dst_tok is not None:
                k.dma("sp", dst_tok[t0:t0 + 128, :], vb.t[:, :], r=[vb])
            j = ti % 4
            if j == 0:
                tb = tring.next()
            for g4 in range(4):
                ps = k.next_psum()
                for q in range(4):
                    kc = g4 * 4 + q
                    k.op("pe", "transpose", ps.t[:, q * 128:(q + 1) * 128], vb.t[:, kc * 128:(kc + 1) * 128], ident.t[:, :],
                         r=[vb, ident], w=[ps])
                eng = "act" if g4 % 2 == 0 else "dve"
                outv = tb.t[:, g4 * 4:(g4 + 1) * 4, j * 128:(j + 1) * 128]
                inv = ps.t[:, :].rearrange("p (q t) -> p q t", q=4)
                if eng == "act":
                    k.op("act", "activation", outv, inv, AF.Copy, r=[ps], w=[tb])
                else:
                    k.op("dve", "tensor_copy", outv, inv, r=[ps], w=[tb])
            if j == 3 or ti == len(tiles) - 1:
                tb0 = tiles[ti - j]
                nt = (j + 1) * 128
                k.dma("sp", dstT.rearrange("(kc p) t -> p kc t", p=128)[:, :, tb0:tb0 + nt], tb.t[:, :, 0:nt], r=[tb])


def make_ident(k, ident):
    k.op("pool", "memset", ident.t[:, :], 0.0, w=[ident])
    k.op("pool", "affine_select", ident.t[:, :], ident.t[:, :], pattern=[[-1, 128]], compare_op=ALU.not_equal, fill=1.0,
         base=0, channel_multiplier=1, r=[ident], w=[ident])


def phase_win(P, l):
    k = P.k
    S = P.scr
    with k.phase() as ph:
        evs = Evac(k, ph, 6)
        wring = Ring(ph, 2, [128, 16, 512], F32R, "win")
        xbr = Ring(ph, 2, [128, 16, 1024], F32R, "xT")
        W = P.inp["w_in"][l]
        tbs = list(range(0, T, 1024))
        xq = []

        def ldx(j):
            if j < len(tbs):
                xb_ = xbr.next()
                load_xT(k, xb_, S["UT"], 16, tbs[j], min(1024, T - tbs[j]))
                xq.append(xb_)

        ldx(0)
        for ti_, t0 in enumerate(tbs):
            nt = min(1024, T - t0)
            ldx(ti_ + 1)
            xb = xq.pop(0)
            blocks = []
            for c in range(O_AX, O_AG, 512):
                blocks.append((c, 512, "fm", evs.fm(S["AXT"], -O_AX, None, t0)))
            for c in range(O_AG, O_Z, 512):
                blocks.append((c, 512, "fm", evs.fm(S["AGT"], -O_AG, AF.Gelu, t0)))
            for c in range(O_Z, O_XBC, 512):
                blocks.append((c, 512, "tm", evs.tm(S["Z"], -O_Z, AF.Silu, t0)))
            for c in range(O_XBC, O_DT, 512):
                blocks.append((c, 512, "fm", evs.fm(S["XBCT"], -O_XBC, None, t0)))
            blocks.append((O_DT, 32, "tm", evs.tm(S["DTR"], -O_DT, None, t0)))
            for c in range(O_Q, O_K, 512):
                blocks.append((c, 512, "fm", evs.fm(S["QT"], -O_Q, None, t0)))
            for c in range(O_K, O_V, 512):
                blocks.append((c, 512, "fm", evs.fm(S["KT"], -O_K, None, t0)))
            for c in range(O_V, O_G, 512):
                blocks.append((c, 512, "tm", evs.tm(S["V"], -O_V, None, t0)))
            for c in range(O_G, PROJ, 512):
                blocks.append((c, 512, "fm", evs.fm(S["GT"], -O_G, AF.Sigmoid, t0)))
            gemm(k, xb, 16, tok_subs(nt), W, blocks, wring)


SCRATCH = {
    "MOD": [2, 6 * D], "H": [T, D], "UT": [D, T], "VTOK": [T, D],
    "AXT": [1024, T], "AGT": [1024, T], "XBCT": [2048, T], "QT": [1024, T], "KT": [1024, T], "GT": [6144, T],
    "Z": [T, 1024], "DTR": [T, 32], "V": [T, 1024],
    "YAT": [1024, T], "YBT": [1024, T], "YCT": [1024, T], "YMT": [D, T],
    "XS": [T, 1024], "BTK": [T, 512], "BFT": [512, T], "CFT": [512, T], "YBR": [T, 1024],
    "POSM": [16, T], "PA": [T, 32], "YE": [16, 544, D],
}

NL = DEPTH


def weight_shapes():
    return {
        "w_ada": [NL, D, 6 * D], "b_ada": [NL, 6 * D], "norm_mix": [NL, D], "norm_ffn": [NL, D],
        "w_in": [NL, D, PROJ],
        "lru_conv_w": [NL, 4, 1024], "lru_conv_b": [NL, 1024], "lru_w_r": [NL, 2, 16, 64, 64], "lru_b_r": [NL, 2, 1024],
        "lru_w_i": [NL, 2, 16, 64, 64], "lru_b_i": [NL, 2, 1024], "lru_lambda": [NL, 2, 1024],
        "ssd_conv_w": [NL, 4, 2048], "ssd_conv_b": [NL, 2048], "ssd_a_log": [NL, 2, 16], "ssd_dt_bias": [NL, 2, 16],
        "ssd_d": [NL, 16], "ssd_norm": [NL, 1024],
        "w_branch_lru": [NL, 1024, D], "w_branch_ssd": [NL, 1024, D], "w_branch_na": [NL, 1024, D], "w_out": [NL, D, D],
        "w_router": [NL, D, 16], "w1": [NL, 16, D, EFF], "w3": [NL, 16, D, EFF], "w2": [NL, 16, EFF, D],
        "norm_final": [1, D],
    }


def declare(P, weights):
    P.din("hin", [T, D])
    P.din("cvec", [2, D])
    P.din("ropec", [128, T])
    P.din("ropes", [128, T])
    P.din("negmask", [64, 64])
    if "rpbx" in weights:
        P.din("rpbx", [NL, 16, 64, 15, 64])
    ws = weight_shapes()
    for n in weights:
        if n != "rpbx":
            P.din(n, ws[n])
    for n, s in SCRATCH.items():
        P.dscr(n, s)


def small_T(k, dst, src_ap, pattern, **kw):
    k.dma("sp", dst, src_ap.rearrange(pattern, **kw), allow_slow_non_contiguous=True)


def phase_lru(P, l):
    k = P.k
    S = P.scr
    I = P.inp
    SEGS = [(0, NCTX), (NCTX, T)]
    with k.phase() as ph:
        wg = ph.sb([128, 32, 128], F32R, "wg")
        prm = ph.sb([128, 8, 16], F32, "prm")
        lamt = ph.sb([128, 2, 8], F32, "lam")
        brt = ph.sb([128, 2, 8], F32, "br")
        bit = ph.sb([128, 2, 8], F32, "bi")
        c8 = ph.sb([128, 2, 8], F32, "c8")
        zt = ph.sb([128, 32, 128], F32, "zt")
        k.op("pool", "memset", zt.t[:, :, :], 0.0, w=[zt])
        k.op("dve", "tensor_copy", wg.t[:, :, :], zt.t[:, :, :], r=[zt], w=[wg])
        for c in range(8):
            for d in range(2):
                for g, nm in enumerate(("lru_w_r", "lru_w_i")):
                    idx = (c * 2 + d) * 2 + g
                    for hb in range(2):
                        k.dma("pool", wg.t[hb * 64:(hb + 1) * 64, idx, hb * 64:(hb + 1) * 64], I[nm][l, d, 2 * c + hb, :, :], w=[wg])
        for tap in range(4):
            k.dma("sp", prm.t[:, :, tap], I["lru_conv_w"][l, tap, :].rearrange("(c p) -> p c", p=128), w=[prm], allow_slow_non_contiguous=True)
        k.dma("sp", prm.t[:, :, 4], I["lru_conv_b"][l, :].rearrange("(c p) -> p c", p=128), w=[prm], allow_slow_non_contiguous=True)
        for tl, nm in ((lamt, "lru_lambda"), (brt, "lru_b_r"), (bit, "lru_b_i")):
            for d in range(2):
                k.dma("sp", tl.t[:, d, :], I[nm][l, d, :].rearrange("(c p) -> p c", p=128), w=[tl], allow_slow_non_contiguous=True)
        k.op("act", "activation", c8.t[:, :, :], lamt.t[:, :, :], AF.Exp, scale=-1.0, r=[lamt], w=[c8])
        k.op("act", "activation", c8.t[:, :, :], c8.t[:, :, :], AF.Ln, bias=1.0, r=[c8], w=[c8])
        k.op("dve", "tensor_scalar", c8.t[:, :, :], c8.t[:, :, :], -8.0, None, ALU.mult, r=[c8], w=[c8])

        ax = ph.sb([128, T], F32, "ax")
        xc = ph.sb([128, T], F32R, "xc")
        ag = ph.sb([128, T], F32, "ag")
        at = ph.sb([128, T], F32, "a")
        ut = ph.sb([128, T], F32, "u")
        sq = ph.sb([128, T], F32, "sq")
        hd = [ph.sb([128, T], F32, "hd%d" % d) for d in range(2)]
        for c in range(8):
            rows = slice(c * 128, (c + 1) * 128)
            k.dma("sp", ax.t[:, :], S["AXT"][rows, :], w=[ax])
            k.dma("sp", ag.t[:, :], S["AGT"][rows, :], w=[ag])
            k.op("dve", "tensor_scalar", xc.t[:, :], ax.t[:, :], prm.t[:, c, 2:3], prm.t[:, c, 4:5], ALU.mult, ALU.add,
                 r=[ax, prm], w=[xc])
            for (s0, s1) in SEGS:
                for tap, off in ((0, -2), (1, -1), (3, 1)):
                    if off < 0:
                        o = xc.t[:, s0 - off:s1]
                        i0 = ax.t[:, s0:s1 + off]
                    else:
                        o = xc.t[:, s0:s1 - off]
                        i0 = ax.t[:, s0 + off:s1]
                    k.op("dve", "scalar_tensor_tensor", o, i0, prm.t[:, c, tap:tap + 1], F_(o), ALU.mult, ALU.add,
                         r=[ax, prm, xc], w=[xc])
            for d in range(2):
                for g, (dst, bt) in enumerate(((at, brt), (ut, bit))):
                    idx = (c * 2 + d) * 2 + g
                    for t0 in range(0, T, 512):
                        n = min(512, T - t0)
                        ps = k.next_psum()
                        k.op("pe", "matmul", ps.t[:, 0:n], wg.t[:, idx, :], xc.t[:, t0:t0 + n], start=True, stop=True,
                             r=[wg, xc], w=[ps])
                        k.op("act", "activation", dst.t[:, t0:t0 + n], ps.t[:, 0:n], AF.Sigmoid, bias=bt.t[:, d, c:c + 1],
                             r=[ps, bt], w=[dst])
                k.op("act", "activation", at.t[:, :], at.t[:, :], AF.Exp, scale=c8.t[:, d, c:c + 1], r=[at, c8], w=[at])
                k.op("pool", "tensor_tensor", ut.t[:, :], ut.t[:, :], F_(xc.t[:, :]), ALU.mult, r=[ut, xc], w=[ut])
                k.op("dve", "tensor_tensor", sq.t[:, :], at.t[:, :], at.t[:, :], ALU.mult, r=[at], w=[sq])
                k.op("act", "activation", sq.t[:, :], sq.t[:, :], AF.Sqrt, scale=-1.0, bias=1.0, r=[sq], w=[sq])
                k.op("dve", "tensor_tensor", ut.t[:, :], ut.t[:, :], sq.t[:, :], ALU.mult, r=[ut, sq], w=[ut])
                h = hd[d]
                if d == 0:
                    k.op("dve", "tensor_tensor_scan", h.t[:, :], at.t[:, :], ut.t[:, :], 0.0, ALU.mult, ALU.add,
                         r=[at, ut], w=[h])
                else:
                    k.op("dve", "tensor_tensor_scan", h.t[:, 0:NCTX][:, ::-1], at.t[:, 0:NCTX][:, ::-1], ut.t[:, 0:NCTX][:, ::-1],
                         0.0, ALU.mult, ALU.add, r=[at, ut], w=[h])
                    k.op("dve", "tensor_tensor_scan", h.t[:, NCTX:T][:, ::-1], at.t[:, NCTX:T][:, ::-1], ut.t[:, NCTX:T][:, ::-1],
                         h.t[:, 0:1], ALU.mult, ALU.add, r=[at, ut, h], w=[h])
            k.op("pool", "tensor_tensor", hd[0].t[:, :], hd[0].t[:, :], hd[1].t[:, :], ALU.add, r=[hd[0], hd[1]], w=[hd[0]])
            k.op("dve", "tensor_tensor", hd[0].t[:, :], hd[0].t[:, :], ag.t[:, :], ALU.mult, r=[hd[0], ag], w=[hd[0]])
            k.dma("sp", S["YAT"][rows, :], hd[0].t[:, :], r=[hd[0]])


def make_consts(k, ph):
    c = {}
    c["ident"] = ph.sb([128, 128], F32, "ident")
    make_ident(k, c["ident"])
    for nm, pat, cm in (("trif", [[1, 128]], -1), ("trib", [[-1, 128]], 1)):
        t = ph.sb([128, 128], F32, nm)
        k.op("pool", "memset", t.t[:, :], 1.0, w=[t])
        k.op("pool", "affine_select", t.t[:, :], t.t[:, :], pattern=pat, compare_op=ALU.is_ge, fill=0.0, base=0,
             channel_multiplier=cm, r=[t], w=[t])
        c[nm] = t
    ones = ph.sb([128, 128], F32, "ones")
    k.op("pool", "memset", ones.t[:, :], 1.0, w=[ones])
    c["ones"] = ones
    sw = ph.sb([128, 128], F32, "swapf")
    k.op("pool", "memset", sw.t[:, :], 0.0, w=[sw])
    for base in (-64, 64):
        k.op("pool", "affine_select", sw.t[:, :], sw.t[:, :], pattern=[[-1, 128]], compare_op=ALU.not_equal, fill=1.0,
             base=base, channel_multiplier=1, r=[sw], w=[sw])
    swr = ph.sb([128, 128], F32R, "swap")
    k.op("dve", "tensor_copy", swr.t[:, :], sw.t[:, :], r=[sw], w=[swr])
    c["swap"] = swr
    return c


def dwconv(k, xc, ax, prm, c, segs):
    k.op("dve", "tensor_scalar", xc.t[:, :], ax.t[:, :], prm.t[:, c, 2:3], prm.t[:, c, 4:5], ALU.mult, ALU.add,
         r=[ax, prm], w=[xc])
    for (s0, s1) in segs:
        for tap, off in ((0, -2), (1, -1), (3, 1)):
            if off < 0:
                o = xc.t[:, s0 - off:s1]
                i0 = ax.t[:, s0:s1 + off]
            else:
                o = xc.t[:, s0:s1 - off]
                i0 = ax.t[:, s0 + off:s1]
            k.op("dve", "scalar_tensor_tensor", o, i0, prm.t[:, c, tap:tap + 1], F_(o), ALU.mult, ALU.add,
                 r=[ax, prm, xc], w=[xc])


def to_token_major(k, ident, src_buf, src_view, dst, col0, stg_ring, nt=T):
    dv = dst.rearrange("(tt p) c -> p tt c", p=128)
    ntile = nt // 128
    for tt0 in range(0, ntile, 4):
        nq = min(4, ntile - tt0)
        ps = k.next_psum()
        for q in range(nq):
            t0 = (tt0 + q) * 128
            k.op("pe", "transpose", ps.t[:, q * 128:(q + 1) * 128], src_view[:, t0:t0 + 128], ident.t[:, :], r=[ident, src_buf], w=[ps])
        st = stg_ring.next()
        k.op("act", "activation", st.t[:, 0:nq * 128], ps.t[:, 0:nq * 128], AF.Copy, r=[ps], w=[st])
        k.dma("sp", dv[:, tt0:tt0 + nq, col0:col0 + 128], st.t[:, 0:nq * 128].rearrange("p (q c) -> p q c", q=nq), r=[st])


def phase_ssd_prep(P, l):
    k = P.k
    S = P.scr
    I = P.inp
    SEGS = [(0, NCTX), (NCTX, T)]
    with k.phase() as ph:
        C = make_consts(k, ph)
        prm = ph.sb([128, 16, 8], F32, "prm")
        for tap in range(4):
            k.dma("sp", prm.t[:, :, tap], I["ssd_conv_w"][l, tap, :].rearrange("(c p) -> p c", p=128), w=[prm], allow_slow_non_contiguous=True)
        k.dma("sp", prm.t[:, :, 4], I["ssd_conv_b"][l, :].rearrange("(c p) -> p c", p=128), w=[prm], allow_slow_non_contiguous=True)
        cosf = ph.sb([128, T], F32, "cosf")
        sinf = ph.sb([128, T], F32, "sinf")
        k.dma("sp", cosf.t[:, :], I["ropec"][:, :], w=[cosf])
        k.dma("sp", sinf.t[:, :], I["ropes"][:, :], w=[sinf])
        axr = Ring(ph, 2, [128, T], F32, "ax")
        xcr = Ring(ph, 2, [128, T], F32R, "xc")
        rot = ph.sb([128, T], F32, "rot")
        tmp = ph.sb([128, T], F32, "tmp")
        stg = Ring(ph, 3, [128, 512], F32, "stg")
        for c in range(16):
            ax = axr.next()
            xc = xcr.next()
            k.dma("sp", ax.t[:, :], S["XBCT"][c * 128:(c + 1) * 128, :], w=[ax])
            dwconv(k, xc, ax, prm, c, SEGS)
            k.op("act", "activation", xc.t[:, :], F_(xc.t[:, :]), AF.Silu, r=[xc], w=[xc])
            if c < 8:
                to_token_major(k, C["ident"], xc, F_(xc.t[:, :]), S["XS"], c * 128, stg)
                k_ = None
            else:
                k.op("pool", "tensor_tensor", rot.t[:, :], F_(xc.t[:, :]), cosf.t[:, :], ALU.mult, r=[xc, cosf], w=[rot])
                for t0 in range(0, T, 512):
                    n = min(512, T - t0)
                    ps = k.next_psum()
                    k.op("pe", "matmul", ps.t[:, 0:n], C["swap"].t[:, :], xc.t[:, t0:t0 + n], start=True, stop=True,
                         r=[C["swap"], xc], w=[ps])
                    k.op("dve", "tensor_tensor", tmp.t[:, t0:t0 + n], ps.t[:, 0:n], sinf.t[:, t0:t0 + n], ALU.mult,
                         r=[ps, sinf], w=[tmp])
                k.op("dve", "tensor_tensor", rot.t[:, :], rot.t[:, :], tmp.t[:, :], ALU.add, r=[rot, tmp], w=[rot])
                if c < 12:
                    g = c - 8
                    k.dma("sp", S["BFT"][g * 128:(g + 1) * 128, :], rot.t[:, :], r=[rot])
                    to_token_major(k, C["ident"], rot, rot.t[:, :], S["BTK"], g * 128, stg)
                else:
                    g = c - 12
                    k.dma("sp", S["CFT"][g * 128:(g + 1) * 128, :], rot.t[:, :], r=[rot])


def phase_ssd(P, l):
    k = P.k
    S = P.scr
    I = P.inp
    NCH = T // 128
    with k.phase() as ph:
        C = make_consts(k, ph)
        dtb = ph.sb([128, 32], F32, "dtb")
        alog = ph.sb([128, 32], F32, "alog")
        dsk = ph.sb([128, 16], F32, "dsk")
        gn = ph.sb([128, 1024], F32, "gn")
        k.dma("sp", dtb.t[:, :], I["ssd_dt_bias"][l].rearrange("d h -> (d h)").partition_broadcast(128), w=[dtb])
        k.dma("sp", alog.t[:, :], I["ssd_a_log"][l].rearrange("d h -> (d h)").partition_broadcast(128), w=[alog])
        k.dma("sp", dsk.t[:, :], I["ssd_d"][l, :].partition_broadcast(128), w=[dsk])
        k.dma("sp", gn.t[:, :], I["ssd_norm"][l, :].partition_broadcast(128), w=[gn])
        k.op("act", "activation", alog.t[:, :], alog.t[:, :], AF.Exp, r=[alog], w=[alog])
        k.op("dve", "tensor_scalar", alog.t[:, :], alog.t[:, :], -1.0, None, ALU.mult, r=[alog], w=[alog])
        Hf = ph.sb([128, 1024], F32, "Hf")
        Hr = ph.sb([128, 1024], F32R, "Hr")
        xsr = Ring(ph, 2, [128, 1024], F32, "xs")
        xdtr = Ring(ph, 2, [128, 1024], F32R, "xdt")
        xddr = Ring(ph, 2, [128, 1024], F32R, "xdd")
        btkr = Ring(ph, 2, [128, 512], F32R, "btk")
        bftr = Ring(ph, 2, [128, 4, 128], F32R, "bft")
        cftr = Ring(ph, 2, [128, 4, 128], F32R, "cft")
        Ltr = Ring(ph, 2, [128, 16, 128], F32, "Lt")
        MTr = Ring(ph, 2, [128, 16, 128], F32R, "MT")
        ychr = Ring(ph, 2, [128, 1024], F32, "ych")
        tmpr = Ring(ph, 2, [128, 1024], F32, "ytmp")
        zr = Ring(ph, 2, [128, 1024], F32, "z")
        ypr = Ring(ph, 2, [128, 1024], F32, "yprev")
        junk = ph.sb([128, 1024], F32, "junk")
        stat = Ring(ph, 4, [128, 4], F32, "stat")
        stg = Ring(ph, 3, [128, 512], F32, "stg")
        bft_v = S["BFT"].rearrange("(g n) t -> n g t", n=128)
        cft_v = S["CFT"].rearrange("(g n) t -> n g t", n=128)
        ybt_v = S["YBT"].rearrange("(c p) t -> p c t", p=128)

        def b16(ap, n):
            return ap.unsqueeze(2).broadcast_to([128, 16, n])

        def v3(ap):
            return ap.rearrange("p (h q) -> p h q", h=16)

        SM = ph.sb([128, NCH, 9, 16], F32, "SM")
        cbmr = Ring(ph, 2, [128, 4, 128], F32, "cbm")
        dtr_v = S["DTR"].rearrange("(c p) h -> p c h", p=128)

        def cb16(ap):
            return ap.unsqueeze(1).broadcast_to([128, NCH, 16])

        for d in range(2):
            tri = C["trif"] if d == 0 else C["trib"]
            order = list(range(NCH)) if d == 0 else [1, 0] + list(range(NCH - 1, 1, -1))
            k.dma("sp", SM.t[:, :, 0, :], dtr_v[:, :, d * 16:(d + 1) * 16], w=[SM])
            k.op("dve", "tensor_tensor", SM.t[:, :, 0, :], SM.t[:, :, 0, :], cb16(dtb.t[:, d * 16:(d + 1) * 16]), ALU.add, r=[SM, dtb], w=[SM])
            k.op("act", "activation", SM.t[:, :, 0, :], SM.t[:, :, 0, :], AF.Exp, r=[SM], w=[SM])
            k.op("act", "activation", SM.t[:, :, 0, :], SM.t[:, :, 0, :], AF.Ln, bias=1.0, r=[SM], w=[SM])
            k.op("dve", "tensor_tensor", SM.t[:, :, 1, :], SM.t[:, :, 0, :], cb16(alog.t[:, d * 16:(d + 1) * 16]), ALU.mult, r=[SM, alog], w=[SM])
            HC = NCH // 2
            for (mat, slot) in ((tri, 2), (C["ones"], 3)):
                for half in range(2):
                    ps = k.next_psum()
                    k.op("pe", "matmul", ps.t[:, 0:HC * 16].rearrange("p (c h) -> p c h", h=16), mat.t[:, :], SM.t[:, half * HC:(half + 1) * HC, 1, :],
                         start=True, stop=True, r=[mat, SM], w=[ps])
                    k.op("dve", "tensor_copy", SM.t[:, half * HC:(half + 1) * HC, slot, :], ps.t[:, 0:HC * 16].rearrange("p (c h) -> p c h", h=16),
                         r=[ps], w=[SM])
            k.op("dve", "tensor_tensor", SM.t[:, :, 7, :], SM.t[:, :, 3, :], SM.t[:, :, 2, :], ALU.subtract, r=[SM], w=[SM])
            k.op("act", "activation", SM.t[:, :, 4, :], SM.t[:, :, 2, :], AF.Exp, r=[SM], w=[SM])
            k.op("act", "activation", SM.t[:, :, 5, :], SM.t[:, :, 7, :], AF.Exp, r=[SM], w=[SM])
            k.op("act", "activation", SM.t[:, :, 6, :], SM.t[:, :, 3, :], AF.Exp, r=[SM], w=[SM])
            k.op("dve", "tensor_tensor", SM.t[:, :, 8, :], SM.t[:, :, 0, :], SM.t[:, :, 5, :], ALU.mult, r=[SM], w=[SM])
            k.op("pool", "memset", Hf.t[:, :], 0.0, w=[Hf])
            k.op("dve", "tensor_copy", Hr.t[:, :], Hf.t[:, :], r=[Hf], w=[Hr])
            for ci in order:
                t0 = ci * 128
                xs = xsr.next(); xdt = xdtr.next(); xdd = xddr.next(); btk = btkr.next(); bft = bftr.next(); cft = cftr.next()
                Lt = Ltr.next(); MT = MTr.next()

                def sm(i, ci=ci):
                    return SM.t[:, ci, i, :]

                k.dma("sp", xs.t[:, :], S["XS"][t0:t0 + 128, :], w=[xs])
                k.dma("pool", btk.t[:, :], S["BTK"][t0:t0 + 128, :], w=[btk])
                k.dma("pool", bft.t[:, :, :], bft_v[:, :, t0:t0 + 128], w=[bft])
                k.dma("pool", cft.t[:, :, :], cft_v[:, :, t0:t0 + 128], w=[cft])
                k.op("dve", "tensor_tensor", v3(xdt.t[:, :]), v3(xs.t[:, :]), b16(sm(0), 64), ALU.mult, r=[xs, SM], w=[xdt])
                k.op("dve", "tensor_tensor", v3(xdd.t[:, :]), v3(xs.t[:, :]), b16(sm(8), 64), ALU.mult, r=[xs, SM], w=[xdd])
                ab_, aap = k.psum_group(4)
                for h in range(16):
                    k.op("pe", "matmul", aap[:, h * 128:(h + 1) * 128], SM.t[:, ci, 1, h:h + 1].broadcast_to([128, 128]), tri.t[:, :],
                         start=True, stop=True, r=[SM, tri], w=[ab_[h // 4]])
                cb = k.next_psum()
                for g in range(4):
                    k.op("pe", "matmul", cb.t[:, g * 128:(g + 1) * 128], bft.t[:, g, :], cft.t[:, g, :], start=True, stop=True,
                         r=[bft, cft], w=[cb])
                cbm = cbmr.next()
                k.op("dve", "tensor_tensor", cbm.t[:, :, :], cb.t[:, :].rearrange("p (g i) -> p g i", g=4),
                     tri.t[:, :].unsqueeze(1).broadcast_to([128, 4, 128]), ALU.mult, r=[cb, tri], w=[cbm])
                for h in range(16):
                    k.op("dve", "tensor_scalar", Lt.t[:, h, :], aap[:, h * 128:(h + 1) * 128], SM.t[:, ci, 2, h:h + 1], 0.0,
                         ALU.subtract, ALU.min, r=[ab_[h // 4], SM], w=[Lt])
                k.op("act", "activation", Lt.t[:, :, :], Lt.t[:, :, :], AF.Exp, r=[Lt], w=[Lt])
                k.op("dve", "tensor_tensor", MT.t[:, :, :].rearrange("p (g r) i -> p g r i", g=4), Lt.t[:, :, :].rearrange("p (g r) i -> p g r i", g=4),
                     cbm.t[:, :, :].unsqueeze(2).broadcast_to([128, 4, 4, 128]), ALU.mult, r=[Lt, cbm], w=[MT])
                sb_, sap = k.psum_group(2)
                for g in range(4):
                    k.op("pe", "matmul", sap[:, g * 256:(g + 1) * 256], btk.t[:, g * 128:(g + 1) * 128], xdd.t[:, g * 256:(g + 1) * 256],
                         start=True, stop=True, r=[btk, xdd], w=[sb_[g // 2]])
                yb_, yap = k.psum_group(2)
                for h in range(16):
                    k.op("pe", "matmul", yap[:, h * 64:(h + 1) * 64], MT.t[:, h, :], xdt.t[:, h * 64:(h + 1) * 64], start=True, stop=True,
                         r=[MT, xdt], w=[yb_[h // 8]])
                ob_, oap = k.psum_group(2)
                for g in range(4):
                    k.op("pe", "matmul", oap[:, g * 256:(g + 1) * 256], cft.t[:, g, :], Hr.t[:, g * 256:(g + 1) * 256], start=True, stop=True,
                         r=[cft, Hr], w=[ob_[g // 2]])
                k.op("dve", "tensor_tensor", v3(Hf.t[:, :]), v3(Hf.t[:, :]), b16(sm(6), 64), ALU.mult, r=[Hf, SM], w=[Hf])
                k.op("dve", "tensor_tensor", Hf.t[:, :], Hf.t[:, :], sap, ALU.add, r=[Hf, sb_[0], sb_[1]], w=[Hf])
                k.op("act", "activation", Hr.t[:, :], Hf.t[:, :], AF.Copy, r=[Hf], w=[Hr])
                ych = ychr.next(); ytmp = tmpr.next()
                k.op("dve", "tensor_tensor", v3(ytmp.t[:, :]), v3(oap), b16(sm(4), 64), ALU.mult, r=[ob_[0], ob_[1], SM], w=[ytmp])
                k.op("dve", "tensor_tensor", ych.t[:, :], yap, ytmp.t[:, :], ALU.add, r=[yb_[0], yb_[1], ytmp], w=[ych])
                if d == 0:
                    k.dma("sp", S["YBR"][t0:t0 + 128, :], ych.t[:, :], r=[ych])
                else:
                    yp = ypr.next(); z = zr.next()
                    k.dma("sp", yp.t[:, :], S["YBR"][t0:t0 + 128, :], w=[yp])
                    k.dma("sp", z.t[:, :], S["Z"][t0:t0 + 128, :], w=[z])
                    k.op("pool", "tensor_tensor", ych.t[:, :], ych.t[:, :], yp.t[:, :], ALU.add, r=[ych, yp], w=[ych])
                    k.op("pool", "tensor_tensor", v3(ytmp.t[:, :]), v3(xs.t[:, :]), b16(dsk.t[:, :], 64), ALU.mult, r=[xs, dsk, ytmp], w=[ytmp])
                    k.op("pool", "tensor_tensor", ych.t[:, :], ych.t[:, :], ytmp.t[:, :], ALU.add, r=[ych, ytmp], w=[ych])
                    k.op("dve", "tensor_tensor", ych.t[:, :], ych.t[:, :], z.t[:, :], ALU.mult, r=[ych, z], w=[ych])
                    st = stat.next()
                    k.op("dve", "scalar_tensor_tensor", junk.t[:, :], ych.t[:, :], 1.0, ych.t[:, :], ALU.mult, ALU.mult, accum_out=st.t[:, 0:1],
                         r=[ych], w=[junk, st])
                    k.op("act", "activation", st.t[:, 1:2], st.t[:, 0:1], AF.Ln, scale=1.0 / 1024, bias=EPS, r=[st], w=[st])
                    k.op("act", "activation", st.t[:, 3:4], st.t[:, 1:2], AF.Exp, scale=-0.5, r=[st], w=[st])
                    k.op("dve", "scalar_tensor_tensor", ych.t[:, :], ych.t[:, :], st.t[:, 3:4], gn.t[:, :], ALU.mult, ALU.mult,
                         r=[ych, st, gn], w=[ych])
                    for c0 in range(0, 8, 4):
                        ps = k.next_psum()
                        for q in range(4):
                            k.op("pe", "transpose", ps.t[:, q * 128:(q + 1) * 128], ych.t[:, (c0 + q) * 128:(c0 + q + 1) * 128], C["ident"].t[:, :],
                                 r=[ych, C["ident"]], w=[ps])
                        sg = stg.next()
                        k.op("act", "activation", sg.t[:, :], ps.t[:, :], AF.Copy, r=[ps], w=[sg])
                        k.dma("sp", ybt_v[:, c0:c0 + 4, t0:t0 + 128], sg.t[:, :].rearrange("p (q t) -> p q t", q=4), r=[sg])
            k.barrier()


def rope_tables():
    pos = np.arange(NLAT)
    row = (pos // 64).astype(np.float32)
    col = (pos % 64).astype(np.float32)
    freqs = (np.float32(10000.0) ** (-np.arange(32, dtype=np.float32) / np.float32(32))).astype(np.float32)
    ang = np.concatenate([row[:, None] * freqs, col[:, None] * freqs], axis=-1).astype(np.float32)
    cos = np.cos(ang).astype(np.float32).T
    sin = np.sin(ang).astype(np.float32).T
    c = np.ones((128, T), np.float32)
    s = np.zeros((128, T), np.float32)
    c[0:64, NCTX:] = cos
    c[64:128, NCTX:] = cos
    s[0:64, NCTX:] = -sin
    s[64:128, NCTX:] = sin
    return c, s


def na_tables(rpb):
    Ld = rpb.shape[0]
    kc = np.arange(64)[:, None]
    j = np.arange(64)[None, :]
    ci = np.clip(kc - j + 15, 0, 30)
    s = np.arange(15)
    x = rpb[:, :, 14 - s, :]
    x = x[:, :, :, ci]
    rpbx = np.ascontiguousarray(np.transpose(x, (0, 1, 3, 2, 4))).astype(np.float32)
    cs = np.clip(np.arange(64) - 8, 0, 48)[None, :]
    inside = (kc >= cs) & (kc < cs + 16)
    negmask = np.where(inside, 0.0, -30000.0).astype(np.float32)
    return rpbx, negmask


def phase_na(P, l):
    k = P.k
    S = P.scr
    I = P.inp

    def rs(r):
        return min(max(r - 4, 0), 56)

    with k.phase() as ph:
        C = make_consts(k, ph)
        negm = ph.sb([64, 64], F32, "negm")
        k.dma("sp", negm.t[:, :], I["negmask"][:, :], w=[negm])
        qTr = Ring(ph, 1, [64, T], F32R, "qT")
        kTr = Ring(ph, 1, [64, T], F32R, "kT")
        var = Ring(ph, 2, [64, 64, 66], F32R, "vaug")
        vcr = Ring(ph, 2, [128, 2, 66], F32R, "vc")
        tblr = Ring(ph, 2, [64, 15, 64], F32, "tbl")
        yctr = Ring(ph, 2, [64, T], F32, "yct")
        PTr = Ring(ph, 2, [64, 15, 512], F32R, "PT")
        PTcr = Ring(ph, 2, [128, 2, 512], F32R, "PTc")
        tmpr = Ring(ph, 3, [128, 512], F32, "natmp")
        yblk = Ring(ph, 2, [128, 8, 64], F32, "yblk")
        recr = Ring(ph, 2, [128, 8], F32, "rec")
        zer = ph.sb([64, 64], F32, "zer")
        k.op("pool", "memset", zer.t[:, :], 0.0, w=[zer])
        heads = {}

        def load_head(h):
            qT = qTr.next(); kT = kTr.next(); va = var.next(); vc = vcr.next(); tbl = tblr.next(); yct = yctr.next()
            k.dma("pool", qT.t[:, :], S["QT"][h * 64:(h + 1) * 64, :], w=[qT])
            k.dma("pool", kT.t[:, :], S["KT"][h * 64:(h + 1) * 64, :], w=[kT])
            k.dma("pool", va.t[:, :, 0:64], S["V"][NCTX:T, h * 64:(h + 1) * 64].rearrange("(r kc) d -> kc r d", kc=64), w=[va])
            k.dma("pool", vc.t[:, :, 0:64], S["V"][0:NCTX, h * 64:(h + 1) * 64].rearrange("(c p) d -> p c d", p=128), w=[vc])
            k.op("dve", "tensor_copy", va.t[:, :, 64:66], C["ones"].t[0:64, 0:128].rearrange("p (a b) -> p a b", b=2), r=[C["ones"]], w=[va])
            k.op("dve", "tensor_copy", vc.t[:, :, 64:66], C["ones"].t[:, 0:4].rearrange("p (a b) -> p a b", b=2), r=[C["ones"]], w=[vc])
            k.dma("sp", tbl.t[:, :, :], I["rpbx"][l, h], w=[tbl])
            k.op("pool", "tensor_tensor", tbl.t[:, :, :], tbl.t[:, :, :], negm.t[:, :].unsqueeze(1).broadcast_to([64, 15, 64]), ALU.add,
                 r=[tbl, negm], w=[tbl])
            heads[h] = dict(qT=qT, kT=kT, va=va, vc=vc, tbl=tbl, yct=yct)

        def stageS(item):
            h, b = item
            if b == "c":
                load_head(h)
            H = heads[h]
            qT, kT, tbl = H["qT"], H["kT"], H["tbl"]
            c = dict(item=item, H=H)
            PTc = c["PTc"] = PTcr.next()
            if b == "c":
                for cc in range(2):
                    ps = k.next_psum()
                    k.op("pe", "matmul", ps.t[:, 0:NCTX], kT.t[:, cc * 128:(cc + 1) * 128], qT.t[:, 0:NCTX], start=True, stop=True, r=[kT, qT], w=[ps])
                    k.op("act", "activation", PTc.t[:, cc, 0:NCTX], ps.t[:, 0:NCTX], AF.Exp, scale=0.125, r=[ps], w=[PTc])
                return c
            r0 = b * 8
            rows = list(range(r0, r0 + 8))
            Rlo = rs(r0); Rhi = rs(r0 + 7) + 7
            PT = c["PT"] = PTr.next()
            q0 = NCTX + r0 * 64
            for cc in range(2):
                ps = k.next_psum()
                k.op("pe", "matmul", ps.t[:, :], kT.t[:, cc * 128:(cc + 1) * 128], qT.t[:, q0:q0 + 512], start=True, stop=True, r=[kT, qT], w=[ps])
                k.op("act", "activation", PTc.t[:, cc, :], ps.t[:, :], AF.Exp, scale=0.125, r=[ps], w=[PTc])
            for R in range(Rlo, Rhi + 1):
                vr = [r for r in rows if rs(r) <= R <= rs(r) + 7]
                ra, rb = vr[0], vr[-1]
                nr = rb - ra + 1
                n = nr * 64
                s0 = 7 - R + ra
                ps = k.next_psum()
                k.op("pe", "matmul", ps.t[0:64, 0:n], kT.t[:, NCTX + R * 64:NCTX + (R + 1) * 64], qT.t[:, NCTX + ra * 64:NCTX + (rb + 1) * 64],
                     start=True, stop=True, r=[kT, qT], w=[ps])
                tm = tmpr.next()
                k.op("dve", "scalar_tensor_tensor", tm.t[0:64, 0:n], ps.t[0:64, 0:n], 0.125, tbl.t[:, s0:s0 + nr, :].rearrange("p a b -> p (a b)"),
                     ALU.mult, ALU.add, r=[ps, tbl], w=[tm])
                o0 = (ra - r0) * 64
                k.op("act", "activation", PT.t[:, R - Rlo, o0:o0 + n], tm.t[0:64, 0:n], AF.Exp, r=[tm], w=[PT])
            for pi in range(4):
                pa_, pb2 = r0 + 2 * pi, r0 + 2 * pi + 1
                for R in range(rs(pa_), rs(pb2) + 8):
                    for row in (pa_, pb2):
                        if not (rs(row) <= R <= rs(row) + 7):
                            ri = row - r0
                            k.op("pool", "tensor_copy", PT.t[:, R - Rlo, ri * 64:(ri + 1) * 64], zer.t[0:64, 0:64], r=[zer], w=[PT])
            return c

        def stageV(c):
            h, b = c["item"]
            H = c["H"]
            va, vc, yct = H["va"], H["vc"], H["yct"]
            PTc = c["PTc"]
            if b == "c":
                ps2 = k.next_psum()
                for qc in range(2):
                    for cc in range(2):
                        k.op("pe", "matmul", ps2.t[:, qc * 66:(qc + 1) * 66], PTc.t[:, cc, qc * 128:(qc + 1) * 128], vc.t[:, cc, :],
                             start=(cc == 0), stop=(cc == 1), r=[PTc, vc], w=[ps2])
                rec = recr.next(); yb = yblk.next()
                p2v = ps2.t[:, 0:132].rearrange("p (a b) -> p a b", b=66)
                k.op("dve", "reciprocal", rec.t[:, 0:2], p2v[:, :, 64], r=[ps2], w=[rec])
                k.op("dve", "tensor_tensor", yb.t[:, 0:2, :], p2v[:, :, 0:64], rec.t[:, 0:2].unsqueeze(2).broadcast_to([128, 2, 64]), ALU.mult,
                     r=[ps2, rec], w=[yb])
                pst = k.next_psum()
                for qc in range(2):
                    k.op("pe", "transpose", pst.t[0:64, qc * 128:(qc + 1) * 128], yb.t[:, qc, :], C["ident"].t[:, :], r=[yb, C["ident"]], w=[pst])
                k.op("act", "activation", yct.t[:, 0:NCTX], pst.t[0:64, 0:NCTX], AF.Copy, r=[pst], w=[yct])
                return
            PT = c["PT"]
            r0 = b * 8
            Rlo = rs(r0)
            q0 = NCTX + r0 * 64
            pp = k.next_psum()
            for pi in range(4):
                ra = r0 + 2 * pi
                rb = ra + 1
                off = pi * 66
                U = list(range(rs(ra), rs(rb) + 8))
                for bi, R in enumerate(U):
                    k.op("pe", "matmul", pp.t[:, off:off + 66], PT.t[:, R - Rlo, pi * 128:(pi + 1) * 128], va.t[:, R, :],
                         start=(bi == 0), stop=False, r=[PT, va], w=[pp])
                for cc in range(2):
                    k.op("pe", "matmul", pp.t[:, off:off + 66], PTc.t[:, cc, pi * 128:(pi + 1) * 128], vc.t[:, cc, :],
                         start=False, stop=(cc == 1), r=[PTc, vc], w=[pp])
            rec = recr.next(); yb = yblk.next()
            pv = pp.t[:, 0:264].rearrange("p (a b) -> p a b", b=66)
            k.op("dve", "reciprocal", rec.t[:, 0:4], pv[:, :, 64], r=[pp], w=[rec])
            k.op("dve", "tensor_tensor", yb.t[:, 0:4, :], pv[:, :, 0:64], rec.t[:, 0:4].unsqueeze(2).broadcast_to([128, 4, 64]), ALU.mult,
                 r=[pp, rec], w=[yb])
            pst = k.next_psum()
            for pi in range(4):
                k.op("pe", "transpose", pst.t[0:64, pi * 128:(pi + 1) * 128], yb.t[:, pi, :], C["ident"].t[:, :], r=[yb, C["ident"]], w=[pst])
            k.op("act", "activation", yct.t[:, q0:q0 + 512], pst.t[0:64, :], AF.Copy, r=[pst], w=[yct])
            if b == 7:
                k.dma("sp", S["YCT"][h * 64:(h + 1) * 64, :], yct.t[:, :], r=[yct])

        items = [(h, b) for h in range(16) for b in (["c"] + list(range(8)))]
        pend = stageS(items[0])
        for i in range(len(items)):
            nxt = stageS(items[i + 1]) if i + 1 < len(items) else None
            stageV(pend)
            pend = nxt


def phase_merge(P, l):
    k = P.k
    S = P.scr
    I = P.inp
    srcs = [(S["YAT"], I["w_branch_lru"][l]), (S["YBT"], I["w_branch_ssd"][l]), (S["YCT"], I["w_branch_na"][l])]
    with k.phase() as ph:
        wring = Ring(ph, 2, [128, 8, 512], F32R, "wbr")
        xbr = Ring(ph, 2, [128, 8, 1024], F32R, "xT")
        acc = ph.sb([128, 16, 1024], F32, "acc")
        accb = [[Buf(acc.t) for _ in range(2)] for _ in range(16)]
        gring = Ring(ph, 8, [128, 512], F32, "g")
        tring = Ring(ph, 3, [128, 512], F32, "tmp")
        jobs = [(tb0, br) for tb0 in range(0, T, 1024) for br in range(3)]
        xq = []

        def ldx(j):
            if j < len(jobs):
                tb_, br_ = jobs[j]
                xb_ = xbr.next()
                load_xT(k, xb_, srcs[br_][0], 8, tb_, min(1024, T - tb_))
                xq.append(xb_)

        ldx(0)
        for ji, (tb0, br) in enumerate(jobs):
            nt = min(1024, T - tb0)
            subs = tok_subs(nt)
            if True:
                ldx(ji + 1)
                xb = xq.pop(0)
                seq = [(c0 + cc, t0, n) for c0 in range(0, D, 512) for cc in range(0, 512, 128) for (t0, n) in subs["fm"]]
                gq = []

                def ldg(i, br=br, seq=seq, gq=gq, tb0=tb0):
                    if i < len(seq):
                        c, t0, n = seq[i]
                        g = gring.next()
                        k.dma("sp", g.t[:, 0:n], S["GT"][br * D + c:br * D + c + 128, tb0 + t0:tb0 + t0 + n], w=[g])
                        gq.append(g)

                for i0 in range(5):
                    ldg(i0)
                cnt = [0]

                def ev(ps, m, n, c, t0, br=br, gq=gq, cnt=cnt, ldg=ldg, tb0=tb0):
                    ldg(cnt[0] + 5)
                    cnt[0] += 1
                    g = gq.pop(0)
                    av = acc.t[:, c // 128, t0:t0 + n]
                    ab = accb[c // 128][t0 // 512]
                    if br == 0:
                        k.op("dve", "tensor_tensor", av, ps.t[:, 0:n], g.t[:, 0:n], ALU.mult, r=[ps, g], w=[ab])
                    else:
                        tm = tring.next()
                        k.op("dve", "tensor_tensor", tm.t[:, 0:n], ps.t[:, 0:n], g.t[:, 0:n], ALU.mult, r=[ps, g], w=[tm])
                        k.op("pool", "tensor_tensor", av, av, tm.t[:, 0:n], ALU.add, r=[ab, tm], w=[ab])
                        if br == 2:
                            k.dma("sp", S["YMT"][c:c + 128, tb0 + t0:tb0 + t0 + n], av, r=[ab])

                blocks = [(c0, 512, "fm", ev) for c0 in range(0, D, 512)]
                gemm(k, xb, 8, subs, srcs[br][1], blocks, wring)


def load_mod_bcast(k, P, tiles, chunk):
    for s in range(2):
        k.dma("sp", tiles[s].t[:, :], P.scr["MOD"][s, chunk * D:(chunk + 1) * D].partition_broadcast(128), w=[tiles[s]])


def phase_wout(P, l, hsrc):
    k = P.k
    S = P.scr
    I = P.inp
    with k.phase() as ph:
        wring = Ring(ph, 2, [128, 16, 512], F32R, "wout")
        xb = ph.sb([128, 16, 1024], F32R, "xT")
        g1 = [ph.sb([128, D], F32, "g1") for _ in range(2)]
        load_mod_bcast(k, P, g1, 2)
        hring = Ring(ph, 4, [128, 512], F32, "h")
        tring = Ring(ph, 3, [128, 512], F32, "tmp")
        for tb0 in range(0, T, 1024):
            nt = min(1024, T - tb0)
            subs = tok_subs(nt)
            load_xT(k, xb, S["YMT"], 16, tb0, nt)
            seq = [(c0, t0, n) for c0 in range(0, D, 512) for (t0, n) in subs["tm"]]
            hq = []

            def ldh(i, seq=seq, hq=hq, tb0=tb0):
                if i < len(seq):
                    c, t0, n = seq[i]
                    h = hring.next()
                    k.dma("sp", h.t[0:n, :], hsrc[tb0 + t0:tb0 + t0 + n, c:c + 512], w=[h])
                    hq.append(h)

            ldh(0); ldh(1)
            cnt = [0]

            def ev(ps, n, ncw, c, t0, hq=hq, cnt=cnt, ldh=ldh, tb0=tb0):
                ldh(cnt[0] + 2)
                cnt[0] += 1
                h = hq.pop(0)
                s = 1 if (tb0 + t0) < NCTX else 0
                tm = tring.next()
                k.op("dve", "tensor_tensor", tm.t[0:n, :], ps.t[0:n, 0:ncw], g1[s].t[0:n, c:c + ncw], ALU.mult, r=[ps, g1[s]], w=[tm])
                k.op("pool", "tensor_tensor", tm.t[0:n, :], tm.t[0:n, :], h.t[0:n, :], ALU.add, r=[tm, h], w=[tm])
                k.dma("sp", S["H"][tb0 + t0:tb0 + t0 + n, c:c + ncw], tm.t[0:n, :], r=[tm])

            blocks = [(c0, 512, "tm", ev) for c0 in range(0, D, 512)]
            gemm(k, xb, 16, subs, I["w_out"][l], blocks, wring)


CAP_L = 512
CAP_C = 32
NSLOT = CAP_L + CAP_C


def phase_router(P, l):
    k = P.k
    S = P.scr
    I = P.inp
    with k.phase() as ph:
        C = make_consts(k, ph)
        wr = ph.sb([128, 16, 16], F32, "wr")
        k.dma("sp", wr.t[:, :, :], I["w_router"][l].rearrange("(kc p) e -> p kc e", p=128), w=[wr])
        affT = ph.sb([16, T], F32, "affT")
        work = ph.sb([16, T], F32, "work")
        mask = ph.sb([16, T], F32, "mask")
        ones16 = ph.sb([16, T], F32, "ones16")
        scan = ph.sb([16, T], F32, "scan")
        k.op("pool", "memset", ones16.t[:, :], 1.0, w=[ones16])
        xr = Ring(ph, 2, [128, 16, 512], F32, "vT")
        sm = Ring(ph, 4, [128, 64], F32, "rsm")
        uv = S["UT"].rearrange("(kc p) t -> p kc t", p=128)
        for tb0 in range(0, T, 512):
            nt = min(512, T - tb0)
            xb = xr.next()
            k.dma("sp", xb.t[:, :, 0:nt], uv[:, :, tb0:tb0 + nt], w=[xb])
            for t0 in range(0, nt, 128):
                ps = k.next_psum()
                for kc in range(16):
                    k.op("pe", "matmul", ps.t[:, 0:16], xb.t[:, kc, t0:t0 + 128], wr.t[:, kc, :], start=(kc == 0), stop=(kc == 15),
                         r=[xb, wr], w=[ps])
                s = sm.next()
                k.op("dve", "tensor_reduce", s.t[:, 0:1], ps.t[:, 0:16], axis=AXL.X, op=ALU.max, r=[ps], w=[s])
                k.op("dve", "tensor_scalar", s.t[:, 1:2], s.t[:, 0:1], -1.0, None, ALU.mult, r=[s], w=[s])
                k.op("act", "activation", s.t[:, 16:32], ps.t[:, 0:16], AF.Exp, bias=s.t[:, 1:2], accum_out=s.t[:, 2:3], r=[ps, s], w=[s])
                k.op("dve", "reciprocal", s.t[:, 3:4], s.t[:, 2:3], r=[s], w=[s])
                k.op("dve", "tensor_scalar", s.t[:, 32:48], s.t[:, 16:32], s.t[:, 3:4], None, ALU.mult, r=[s], w=[s])
                pt = k.next_psum()
                k.op("pe", "transpose", pt.t[0:16, 0:128], s.t[:, 32:48], C["ident"].t[:, :], r=[s, C["ident"]], w=[pt])
                k.op("act", "activation", affT.t[:, tb0 + t0:tb0 + t0 + 128], pt.t[0:16, 0:128], AF.Copy, r=[pt], w=[affT])
        thr = ph.sb([16, 8], F32, "thr")
        mx = Ring(ph, 2, [16, 8], F32, "mx")
        k.op("dve", "tensor_copy", work.t[:, :], affT.t[:, :], r=[affT], w=[work])
        for (s0, s1, cap, ti) in ((0, NCTX, CAP_C, 0), (NCTX, T, CAP_L, 1)):
            nit = cap // 8
            for it in range(nit):
                m = mx.next()
                k.op("dve", "max", m.t[:, :], work.t[:, s0:s1], r=[work], w=[m])
                if it < nit - 1:
                    k.op("dve", "match_replace", work.t[:, s0:s1], m.t[:, :], work.t[:, s0:s1], -1.0, r=[work, m], w=[work])
                else:
                    k.op("dve", "tensor_copy", thr.t[:, ti:ti + 1], m.t[:, 7:8], r=[m], w=[thr])
            k.op("dve", "tensor_scalar", mask.t[:, s0:s1], affT.t[:, s0:s1], thr.t[:, ti:ti + 1], None, ALU.is_ge, r=[affT, thr], w=[mask])
            k.op("dve", "tensor_tensor_scan", scan.t[:, s0:s1], ones16.t[:, s0:s1], mask.t[:, s0:s1], 0.0, ALU.mult, ALU.add,
                 r=[ones16, mask], w=[scan])
        k.op("dve", "tensor_tensor", scan.t[:, :], scan.t[:, :], mask.t[:, :], ALU.mult, r=[scan, mask], w=[scan])
        k.op("dve", "tensor_scalar", scan.t[:, :], scan.t[:, :], -1.0, None, ALU.add, r=[scan], w=[scan])
        k.op("pool", "tensor_tensor", mask.t[:, :], mask.t[:, :], affT.t[:, :], ALU.mult, r=[mask, affT], w=[mask])
        k.dma("sp", S["POSM"][:, :], scan.t[:, :], r=[scan])
        stg = Ring(ph, 3, [128, 32], F32, "pa")
        for t0 in range(0, T, 128):
            pt = k.next_psum()
            k.op("pe", "transpose", pt.t[:, 0:16], scan.t[:, t0:t0 + 128], C["ident"].t[0:16, 0:16], r=[scan, C["ident"]], w=[pt])
            k.op("pe", "transpose", pt.t[:, 16:32], mask.t[:, t0:t0 + 128], C["ident"].t[0:16, 0:16], r=[mask, C["ident"]], w=[pt])
            st = stg.next()
            k.op("act", "activation", st.t[:, :], pt.t[:, 0:32], AF.Copy, r=[pt], w=[st])
            k.dma("sp", S["PA"][t0:t0 + 128, :], st.t[:, :], r=[st])


def phase_moe1(P, l):
    k = P.k
    S = P.scr
    I = P.inp
    NT = T // 128
    with k.phase() as ph:
        pa = ph.sb([128, NT, 32], F32, "pa")
        k.dma("sp", pa.t[:, :, :], S["PA"].rearrange("(tt p) c -> p tt c", p=128), w=[pa])
        iota = ph.sb([128, 512], F32, "iota")
        k.op("pool", "iota", iota.t[:, :], pattern=[[1, 512]], base=0, channel_multiplier=0, allow_small_or_imprecise_dtypes=True, w=[iota])
        vctx = ph.sb([128, 2, D], F32R, "vctx")
        k.dma("pool", vctx.t[:, :, :], S["VTOK"][0:NCTX, :].rearrange("(tt p) c -> p tt c", p=128), w=[vctx])
        vring = Ring(ph, 3, [128, 1024], F32R, "vtok")
        selr = Ring(ph, 3, [128, 512], F32R, "sel")
        xg = ph.sb([128, 16, NSLOT], F32R, "xg")
        h1s = ph.sb([128, 8, NSLOT], F32, "h1s")
        hid = ph.sb([128, 8, NSLOT], F32R, "hid")
        wring = Ring(ph, 2, [128, 16, 512], F32R, "wexp")
        evs = Evac(k, ph, 4)
        subs = {"fm": [(0, 512), (512, CAP_C)], "tm": [(0, 128), (128, 128), (256, 128), (384, 128), (512, CAP_C)]}
        for e in range(NEXP):
            for half in range(2):
                k.ps_rr = 0
                for tt in range(2, NT):
                    vt = vring.next()
                    k.dma("pool", vt.t[:, :], S["VTOK"][tt * 128:(tt + 1) * 128, half * 1024:(half + 1) * 1024], w=[vt])
                    sel = selr.next()
                    k.op("dve", "tensor_scalar", sel.t[:, :], iota.t[:, :], pa.t[:, tt, e:e + 1], None, ALU.is_equal, r=[iota, pa], w=[sel])
                    for dc in range(8):
                        k.op("pe", "matmul", k.psum[dc].t[:, :], vt.t[:, dc * 128:(dc + 1) * 128], sel.t[:, :], start=(tt == 2), stop=(tt == NT - 1),
                             r=[vt, sel], w=[k.psum[dc]])
                for dc in range(8):
                    if dc % 2 == 0:
                        k.op("act", "activation", xg.t[:, half * 8 + dc, 0:512], k.psum[dc].t[:, :], AF.Copy, r=[k.psum[dc]], w=[xg])
                    else:
                        k.op("dve", "tensor_copy", xg.t[:, half * 8 + dc, 0:512], k.psum[dc].t[:, :], r=[k.psum[dc]], w=[xg])
            selc = [selr.next() for _ in range(2)]
            for tt in range(2):
                k.op("dve", "tensor_scalar", selc[tt].t[:, 0:CAP_C], iota.t[:, 0:CAP_C], pa.t[:, tt, e:e + 1], None, ALU.is_equal,
                     r=[iota, pa], w=[selc[tt]])
            ps = k.next_psum()
            for dc in range(16):
                for tt in range(2):
                    k.op("pe", "matmul", ps.t[:, dc * CAP_C:(dc + 1) * CAP_C], vctx.t[:, tt, dc * 128:(dc + 1) * 128], selc[tt].t[:, 0:CAP_C],
                         start=(tt == 0), stop=(tt == 1), r=[vctx, selc[tt]], w=[ps])
            k.op("dve", "tensor_copy", xg.t[:, :, 512:NSLOT], ps.t[:, :].rearrange("p (a b) -> p a b", b=CAP_C), r=[ps], w=[xg])

            def ev1(ps, m, n, c, t0):
                k.op("act", "activation", h1s.t[:, c // 128, t0:t0 + n], ps.t[:, 0:n], AF.Silu, r=[ps], w=[h1s])

            def ev3(ps, m, n, c, t0):
                k.op("dve", "tensor_tensor", hid.t[:, c // 128, t0:t0 + n], ps.t[:, 0:n], h1s.t[:, c // 128, t0:t0 + n], ALU.mult, r=[ps, h1s], w=[hid])

            gemm(k, xg, 16, subs, I["w1"][l, e], [(c0, 512, "fm", ev1) for c0 in range(0, EFF, 512)], wring)
            gemm(k, xg, 16, subs, I["w3"][l, e], [(c0, 512, "fm", ev3) for c0 in range(0, EFF, 512)], wring)
            gemm(k, hid, 8, subs, I["w2"][l, e], [(c0, 512, "tm", evs.tm(S["YE"][e], 0, None, 0)) for c0 in range(0, D, 512)], wring)


def phase_moe2(P, l):
    k = P.k
    S = P.scr
    NT = T // 128
    with k.phase() as ph:
        pa = ph.sb([128, NT, 32], F32, "pa")
        k.dma("sp", pa.t[:, :, :], S["PA"].rearrange("(tt p) c -> p tt c", p=128), w=[pa])
        posm = ph.sb([16, T], F32, "posm")
        k.dma("sp", posm.t[:, :], S["POSM"][:, :], w=[posm])
        iop = ph.sb([128, 4], F32, "iop")
        k.op("pool", "iota", iop.t[:, :], pattern=[[128, 4]], base=0, channel_multiplier=1, allow_small_or_imprecise_dtypes=True, w=[iop])
        sele = ph.sb([16, 16, 128], F32, "sele")
        k.op("pool", "memset", sele.t[:, :, :], 1.0, w=[sele])
        k.op("pool", "affine_select", sele.t[:, :, :], sele.t[:, :, :], pattern=[[-1, 16], [0, 128]], compare_op=ALU.is_equal, fill=0.0,
             base=0, channel_multiplier=1, r=[sele], w=[sele])
        g2 = [ph.sb([128, D], F32, "g2") for _ in range(2)]
        load_mod_bcast(k, P, g2, 5)
        acc = ph.sb([128, 4, D], F32, "acc")
        yer = Ring(ph, 2, [128, 5, D], F32R, "ye")
        selT = Ring(ph, 2, [128, 4, 512], F32R, "selT")
        hring = Ring(ph, 2, [128, D], F32, "h")
        blocks = [(0, 2, True)] + [(tt, 4, False) for tt in range(2, NT, 4)]
        for (tt0, ntile, is_ctx) in blocks:
            nt = ntile * 128
            tb0 = tt0 * 128
            for e in range(NEXP):
                ye = yer.next()
                if is_ctx:
                    k.dma("pool", ye.t[0:CAP_C, 4, :], S["YE"][e, CAP_L:NSLOT, :], w=[ye])
                else:
                    k.dma("pool", ye.t[:, 0:4, :], S["YE"][e, 0:CAP_L, :].rearrange("(sc p) c -> p sc c", p=128), w=[ye])
                pb = k.next_psum()
                k.op("pe", "matmul", pb.t[:, 0:nt], sele.t[:, e, :], posm.t[:, tb0:tb0 + nt], start=True, stop=True, r=[sele, posm], w=[pb])
                st = selT.next()
                if is_ctx:
                    k.op("dve", "tensor_scalar", st.t[0:CAP_C, 0, 0:nt], pb.t[0:CAP_C, 0:nt], iop.t[0:CAP_C, 0:1], None, ALU.is_equal, r=[pb, iop], w=[st])
                else:
                    for sc in range(4):
                        k.op("dve", "tensor_scalar", st.t[:, sc, 0:nt], pb.t[:, 0:nt], iop.t[:, sc:sc + 1], None, ALU.is_equal, r=[pb, iop], w=[st])
                for ti in range(ntile):
                    ob_, oap = k.psum_group(4)
                    for cb in range(4):
                        if is_ctx:
                            k.op("pe", "matmul", oap[:, cb * 512:(cb + 1) * 512], st.t[0:CAP_C, 0, ti * 128:(ti + 1) * 128], ye.t[0:CAP_C, 4, cb * 512:(cb + 1) * 512],
                                 start=True, stop=True, r=[st, ye], w=[ob_[cb]])
                        else:
                            for sc in range(4):
                                k.op("pe", "matmul", oap[:, cb * 512:(cb + 1) * 512], st.t[:, sc, ti * 128:(ti + 1) * 128], ye.t[:, sc, cb * 512:(cb + 1) * 512],
                                     start=(sc == 0), stop=(sc == 3), r=[st, ye], w=[ob_[cb]])
                    gate = pa.t[:, tt0 + ti, 16 + e:17 + e]
                    if e == 0:
                        k.op("dve", "tensor_scalar", acc.t[:, ti, :], oap, gate, None, ALU.mult, r=ob_ + [pa], w=[acc])
                    else:
                        k.op("dve", "scalar_tensor_tensor", acc.t[:, ti, :], oap, gate, acc.t[:, ti, :], ALU.mult, ALU.add, r=ob_ + [pa, acc], w=[acc])
            for ti in range(ntile):
                t0 = tb0 + ti * 128
                s = 1 if is_ctx else 0
                h = hring.next()
                k.dma("sp", h.t[:, :], S["H"][t0:t0 + 128, :], w=[h])
                k.op("pool", "tensor_tensor", acc.t[:, ti, :], acc.t[:, ti, :], g2[s].t[:, :], ALU.mult, r=[acc, g2[s]], w=[acc])
                k.op("dve", "tensor_tensor", h.t[:, :], h.t[:, :], acc.t[:, ti, :], ALU.add, r=[h, acc], w=[h])
                k.dma("sp", S["H"][t0:t0 + 128, :], h.t[:, :], r=[h])


def phase_final(P, out_ap):
    k = P.k
    S = P.scr
    with k.phase() as ph:
        gb = ph.sb([128, D], F32, "gb")
        k.dma("sp", gb.t[:, :], P.inp["norm_final"][0, :].partition_broadcast(128), w=[gb])
        hring = Ring(ph, 3, [128, D], F32, "h")
        junk = ph.sb([128, D], F32, "junk")
        stat = Ring(ph, 4, [128, 4], F32, "stat")
        for t0 in range(NCTX, T, 128):
            hb = hring.next()
            k.dma("sp", hb.t[:, :], S["H"][t0:t0 + 128, :], w=[hb])
            st = stat.next()
            k.op("dve", "scalar_tensor_tensor", junk.t[:, :], hb.t[:, :], 1.0, hb.t[:, :], ALU.mult, ALU.mult, accum_out=st.t[:, 0:1],
                 r=[hb], w=[junk, st])
            k.op("act", "activation", st.t[:, 1:2], st.t[:, 0:1], AF.Ln, scale=1.0 / D, bias=EPS, r=[st], w=[st])
            k.op("act", "activation", st.t[:, 3:4], st.t[:, 1:2], AF.Exp, scale=-0.5, r=[st], w=[st])
            k.op("dve", "scalar_tensor_tensor", hb.t[:, :], hb.t[:, :], st.t[:, 3:4], gb.t[:, :], ALU.mult, ALU.mult, r=[hb, st, gb], w=[hb])
            k.dma("sp", out_ap[t0 - NCTX:t0 - NCTX + 128, :], hb.t[:, :], r=[hb])


ALL_WEIGHTS = ["w_ada", "b_ada", "norm_mix", "norm_ffn", "w_in", "lru_conv_w", "lru_conv_b", "lru_w_r", "lru_b_r", "lru_w_i", "lru_b_i",
               "lru_lambda", "ssd_conv_w", "ssd_conv_b", "ssd_a_log", "ssd_dt_bias", "ssd_d", "ssd_norm", "rpbx",
               "w_branch_lru", "w_branch_ssd", "w_branch_na", "w_out", "w_router", "w1", "w3", "w2", "norm_final"]


def build_layer(P, l):
    S = P.scr
    hsrc = P.inp["hin"] if l == 0 else S["H"]
    phase_ada(P, l)
    phase_norm(P, l, 0, hsrc, P.inp["norm_mix"][l:l + 1, :], S["UT"])
    phase_win(P, l)
    phase_lru(P, l)
    phase_ssd_prep(P, l)
    phase_ssd(P, l)
    phase_na(P, l)
    phase_merge(P, l)
    phase_wout(P, l, hsrc)
    if "HMID" in P.dbg and l == 0:
        hm = P.nc.dram_tensor("HMID", [T, D], F32, kind="ExternalOutput").ap()
        P.k.dma("sp", hm[:, :], S["H"][:, :])
        P.k.barrier()
    phase_norm(P, l, 1, S["H"], P.inp["norm_ffn"][l:l + 1, :], S["UT"], dst_tok=S["VTOK"])
    phase_router(P, l)
    if P.old_moe:
        phase_moe1(P, l)
        phase_moe2(P, l)
    else:
        phase_moe(P, l)


def host_inputs(inputs, b, nl=None):
    nl = nl or NL
    rc, rs_ = rope_tables()
    rpbx, negmask = na_tables(np.asarray(inputs["na_rpb"][:nl]))
    im = {"hin": np.ascontiguousarray(np.concatenate([inputs["ctx"][b], inputs["x"][b]], 0)),
          "cvec": np.ascontiguousarray(np.stack([inputs["c"][b], inputs["c_ctx"]], 0)),
          "ropec": rc, "ropes": rs_, "negmask": negmask, "rpbx": rpbx}
    for n in ALL_WEIGHTS:
        if n == "rpbx":
            continue
        if n == "norm_final":
            im[n] = np.ascontiguousarray(np.asarray(inputs[n]).reshape(1, D))
        else:
            im[n] = np.ascontiguousarray(np.asarray(inputs[n][:nl]))
    return im


def build_program():
    P = Prog()
    declare(P, ALL_WEIGHTS)
    out = P.dout("out", [NLAT, D])
    for l in range(DEPTH):
        build_layer(P, l)
    phase_final(P, out)
    P.k.barrier()
    return P


def kernel(**inputs):
    inputs = {n: np.asarray(v) for n, v in inputs.items()}
    B = inputs["x"].shape[0]
    P = build_program()
    ims = [host_inputs(inputs, b, DEPTH) for b in range(B)]
    res = run_bass_kernel_spmd(P.nc, ims, core_ids=list(range(B)))
    return np.stack([np.asarray(res.results[b]["out"]) for b in range(B)], 0).astype(np.float32)


I32 = mybir.dt.int32


def idma(k, out, in_, r=(), w=(), out_idx=None, in_idx=None, **kw):
    q = "pool"
    lanes = k.lanes[q]
    i = k.lane_rr[q]
    k.lane_rr[q] = (i + 1) % len(lanes)
    lane = lanes[i]
    key = "d_%s%d" % (q, i)
    k._deps(q, r, w)
    k._wait(q, (key, lane[0], lane[1] * 16))
    ins = k.eng[q].indirect_dma_start(
        out=out, out_offset=(bass.IndirectOffsetOnAxis(ap=out_idx, axis=0) if out_idx is not None else None),
        in_=in_, in_offset=(bass.IndirectOffsetOnAxis(ap=in_idx, axis=0) if in_idx is not None else None), **kw)
    lane[1] += 1
    ins.then_inc(lane[0], 16)
    tok = (key, lane[0], lane[1] * 16)
    k._mark(tok, r, w)
    k.n_ins += 1
    return tok


def phase_moe(P, l):
    k = P.k
    S = P.scr
    I = P.inp
    NT = T // 128
    with k.phase() as ph:
        C = make_consts(k, ph)
        pa = ph.sb([128, NT, 32], F32, "pa")
        k.dma("sp", pa.t[:, :, :], S["PA"].rearrange("(tt p) c -> p tt c", p=128), w=[pa])
        iota = ph.sb([128, 512], F32, "iota")
        k.op("pool", "iota", iota.t[:, :], pattern=[[1, 512]], base=0, channel_multiplier=0, allow_small_or_imprecise_dtypes=True, w=[iota])
        tg = ph.sb([128, NT, 16, 4], F32R, "tg")
        tid = ph.sb([128, NT, 16], F32, "tid")
        k.op("pool", "iota", tid.t[:, :, :], pattern=[[1, NT], [0, 16]], base=0, channel_multiplier=0, allow_small_or_imprecise_dtypes=True, w=[tid])
        k.op("dve", "tensor_copy", tg.t[:, :, :, 0], tid.t[:, :, :], r=[tid], w=[tg])
        tid2 = ph.sb([128, NT, 16], F32, "tid2")
        k.op("pool", "iota", tid2.t[:, :, :], pattern=[[0, NT], [0, 16]], base=0, channel_multiplier=1, allow_small_or_imprecise_dtypes=True, w=[tid2])
        k.op("dve", "tensor_copy", tg.t[:, :, :, 1], tid2.t[:, :, :], r=[tid2], w=[tg])
        k.op("dve", "tensor_copy", tg.t[:, :, :, 2], pa.t[:, :, 16:32], r=[pa], w=[tg])
        k.op("pool", "memset", tid2.t[:, :, :], 0.0, r=[tid2], w=[tid2])
        k.op("dve", "tensor_copy", tg.t[:, :, :, 3], tid2.t[:, :, :], r=[tid2], w=[tg])
        g2 = [ph.sb([128, D], F32, "g2") for _ in range(2)]
        load_mod_bcast(k, P, g2, 5)
        selr = Ring(ph, 2, [128, 512], F32R, "sel")
        idxf = ph.sb([128, 8], F32, "idxf")
        raw4 = ph.sb([128, 5, 4], F32, "raw4")
        idxr = Ring(ph, 2, [128, 8], I32, "idx")
        gater = Ring(ph, 2, [128, 8], F32, "gate")
        xgr = Ring(ph, 3, [128, D], F32, "xgtok")
        yew = ph.sb([128, 5, D], F32, "yew")
        xg = ph.sb([128, 16, NSLOT], F32R, "xg")
        h1s = ph.sb([128, 8, NSLOT], F32, "h1s")
        hid = ph.sb([128, 8, NSLOT], F32R, "hid")
        wring = Ring(ph, 2, [128, 16, 256], F32R, "wexp")
        Hbuf = Buf()
        subs = {"fm": [(0, NSLOT // 2), (NSLOT // 2, NSLOT // 2)], "tm": [(0, 128), (128, 128), (256, 128), (384, 128), (512, CAP_C)]}
        NR = [128, 128, 128, 128, CAP_C]
        state = {}

        def idx_gather(e):
            idx = idxr.next(); gate = gater.next()
            k.ps_rr = 0
            banks = k.psum[0:5]
            for tt in range(NT):
                sel = selr.next()
                if tt < 2:
                    k.op("dve", "tensor_scalar", sel.t[:, 0:CAP_C], iota.t[:, 0:CAP_C], pa.t[:, tt, e:e + 1], None, ALU.is_equal, r=[iota, pa], w=[sel])
                    k.op("pe", "matmul", banks[4].t[0:CAP_C, 0:4], sel.t[:, 0:CAP_C], tg.t[:, tt, e, :], start=(tt == 0), stop=(tt == 1),
                         r=[sel, tg], w=[banks[4]])
                else:
                    k.op("dve", "tensor_scalar", sel.t[:, :], iota.t[:, :], pa.t[:, tt, e:e + 1], None, ALU.is_equal, r=[iota, pa], w=[sel])
                    for sc in range(4):
                        k.op("pe", "matmul", banks[sc].t[:, 0:4], sel.t[:, sc * 128:(sc + 1) * 128], tg.t[:, tt, e, :], start=(tt == 2), stop=(tt == NT - 1),
                             r=[sel, tg], w=[banks[sc]])
            for sc in range(5):
                n = NR[sc]
                k.op("dve", "tensor_copy", raw4.t[0:n, sc, :], banks[sc].t[0:n, 0:4], r=[banks[sc]], w=[raw4])
                k.op("dve", "scalar_tensor_tensor", idxf.t[0:n, sc:sc + 1], raw4.t[0:n, sc, 0:1], 128.0, raw4.t[0:n, sc, 1:2], ALU.mult, ALU.add,
                     r=[raw4], w=[idxf])
                k.op("dve", "tensor_copy", idx.t[0:n, sc:sc + 1], idxf.t[0:n, sc:sc + 1], r=[idxf], w=[idx])
                k.op("dve", "tensor_copy", gate.t[0:n, sc:sc + 1], raw4.t[0:n, sc, 2:3], r=[raw4], w=[gate])
            k.ps_rr = 5
            chunks = []
            for sc in range(3):
                chunks.append(gather_chunk(idx, sc))
            state[e] = (idx, gate, chunks)

        def gather_chunk(idx, sc):
            n = NR[sc]
            xt = xgr.next()
            idma(k, xt.t[0:n, :], S["VTOK"][:, :], r=[idx], w=[xt], in_idx=idx.t[0:n, sc:sc + 1])
            return xt

        def transposes(e, idx, chunks):
            flip = 0
            for sc in range(5):
                xt = chunks[sc]
                if sc < 4:
                    for dc0 in range(0, 16, 4):
                        ps = k.next_psum()
                        for q in range(4):
                            dc = dc0 + q
                            k.op("pe", "transpose", ps.t[:, q * 128:(q + 1) * 128], xt.t[:, dc * 128:(dc + 1) * 128], C["ident"].t[:, :],
                                 r=[xt, C["ident"]], w=[ps])
                        outv = xg.t[:, dc0:dc0 + 4, sc * 128:(sc + 1) * 128]
                        inv = ps.t[:, :].rearrange("p (q t) -> p q t", q=4)
                        flip ^= 1
                        if flip:
                            k.op("act", "activation", outv, inv, AF.Copy, r=[ps], w=[xg])
                        else:
                            k.op("dve", "tensor_copy", outv, inv, r=[ps], w=[xg])
                else:
                    ps = k.next_psum()
                    for dc in range(16):
                        k.op("pe", "transpose", ps.t[:, dc * CAP_C:(dc + 1) * CAP_C], xt.t[0:CAP_C, dc * 128:(dc + 1) * 128], C["ident"].t[0:CAP_C, 0:CAP_C],
                             r=[xt, C["ident"]], w=[ps])
                    k.op("dve", "tensor_copy", xg.t[:, :, CAP_L:NSLOT], ps.t[:, :].rearrange("p (a b) -> p a b", b=CAP_C), r=[ps], w=[xg])
                if sc + 3 < 5:
                    chunks.append(gather_chunk(idx, sc + 3))

        idx_gather(0)
        for e in range(NEXP):
            idx, gate, chunks = state.pop(e)
            transposes(e, idx, chunks)
            def ev1(ps, m, n, c, t0):
                k.op("act", "activation", h1s.t[0:m, c // 128, t0:t0 + n], ps.t[0:m, 0:n], AF.Silu, r=[ps], w=[h1s])

            def ev3(ps, m, n, c, t0):
                k.op("dve", "tensor_tensor", hid.t[0:m, c // 128, t0:t0 + n], ps.t[0:m, 0:n], h1s.t[0:m, c // 128, t0:t0 + n], ALU.mult, r=[ps, h1s], w=[hid])

            def ev2(ps, n, ncw, c, t0, gate=gate):
                sc = t0 // 128
                s = 1 if sc == 4 else 0
                k.op("dve", "scalar_tensor_tensor", yew.t[0:n, sc, c:c + ncw], ps.t[0:n, 0:ncw], gate.t[0:n, sc:sc + 1], g2[s].t[0:n, c:c + ncw],
                     ALU.mult, ALU.mult, r=[ps, gate, g2[s]], w=[yew])

            gemm(k, xg, 16, subs, I["w1"][l, e], [(c0, 256, "fm", ev1) for c0 in range(0, EFF, 256)], wring)
            if e + 1 < NEXP:
                idx_gather(e + 1)
            gemm(k, xg, 16, subs, I["w3"][l, e], [(c0, 256, "fm", ev3) for c0 in range(0, EFF, 256)], wring)
            gemm(k, hid, 8, subs, I["w2"][l, e], [(c0, 256, "tm", ev2) for c0 in range(0, D, 256)], wring)
            for sc in range(5):
                n = NR[sc]
                idma(k, S["H"][:, :], yew.t[0:n, sc, :], r=[yew, idx], w=[Hbuf], out_idx=idx.t[0:n, sc:sc + 1], compute_op=ALU.add)
```
